# Optimizing a Trainium2 kernel written in Bass

```python
import jax, jax.numpy as jnp
from jax import lax
import numpy as np

D_MODEL = 1024
BATCH = 4
SEQ = 8192
DEPTH = 2

ATTN_HEADS = 8
ATTN_HEAD_DIM = 128
ATTN_WIDTH = ATTN_HEADS * ATTN_HEAD_DIM
MOBA_BLOCK = 256
MOBA_TOPK = 3
MOBA_QUERY_BLOCK = 32
SSM_HEADS = 16
SSM_HEAD_DIM = 64
SSM_WIDTH = SSM_HEADS * SSM_HEAD_DIM
SSM_GROUPS = 2
SSM_STATE = 128
SSM_CONV = 4
SSM_CHUNK = 128
SSM_CONV_CH = SSM_WIDTH + 2 * SSM_GROUPS * SSM_STATE
SGU_GROUPS = 8
SGU_GROUP_DIM = 128
SGU_WIDTH = SGU_GROUPS * SGU_GROUP_DIM
SGU_CHUNK = 128
N_BRANCHES = 3
FFN_HIDDEN = 2816
NORM_EPS = 1e-6
IN_SIZES = (ATTN_WIDTH, ATTN_WIDTH, ATTN_WIDTH, SSM_WIDTH, SSM_CONV_CH, SSM_HEADS,
            SGU_WIDTH, SGU_WIDTH, N_BRANCHES * D_MODEL)
IN_COLS = sum(IN_SIZES)

kernel_name = "hybrid_moba_ssd_gmlp_macaron_block"

F32 = jnp.float32


def rms_norm(x, g):
    xf = x.astype(F32)
    y = xf * lax.rsqrt(jnp.mean(xf * xf, axis=-1, keepdims=True) + NORM_EPS)
    return (y * g.astype(F32)).astype(x.dtype)


def layer_norm(x, g, b):
    xf = x.astype(F32)
    mu = jnp.mean(xf, axis=-1, keepdims=True)
    var = jnp.mean(jnp.square(xf - mu), axis=-1, keepdims=True)
    y = (xf - mu) * lax.rsqrt(var + NORM_EPS)
    return (y * g.astype(F32) + b.astype(F32)).astype(x.dtype)


def swiglu_ffn(x, w_gu, w_down):
    gate, up = jnp.split(x @ w_gu, 2, axis=-1)
    return (jax.nn.silu(gate) * up) @ w_down


def alibi_slopes(n_heads):
    return 2.0 ** (-8.0 * (jnp.arange(n_heads, dtype=F32) + 1.0) / n_heads)


def moba_attention(q, k, v):
    b_, s, h, dh = q.shape
    nb = -(-s // MOBA_BLOCK)
    sp = nb * MOBA_BLOCK
    top = min(MOBA_TOPK, nb)
    qt = (q * (dh ** -0.5)).transpose(0, 2, 1, 3)
    pad = ((0, 0), (0, 0), (0, sp - s), (0, 0))
    kt = jnp.pad(k.transpose(0, 2, 1, 3), pad)
    vt = jnp.pad(v.transpose(0, 2, 1, 3), pad)
    k_blocks = kt.reshape(b_, h, nb, MOBA_BLOCK, dh)
    v_blocks = vt.reshape(b_, h, nb, MOBA_BLOCK, dh)
    k_mean = jnp.mean(k_blocks.astype(F32), axis=3)
    slopes = alibi_slopes(h)
    b_ix = jnp.arange(b_)[:, None, None, None]
    h_ix = jnp.arange(h)[None, :, None, None]
    blk_ids = jnp.arange(nb)
    offs = jnp.arange(MOBA_BLOCK)
    qb_len = MOBA_QUERY_BLOCK

    def one_query_block(start):
        qb = lax.dynamic_slice_in_dim(qt, start, qb_len, axis=2)
        t = start + jnp.arange(qb_len)
        own = start // MOBA_BLOCK
        gate = jnp.einsum('bhqd,bhnd->bhqn', qb.astype(F32), k_mean)
        gate = jnp.where(blk_ids < own, gate, -jnp.inf)
        _, sel = lax.top_k(gate, top)
        sel_valid = jnp.arange(top) < own
        k_sel = k_blocks[b_ix, h_ix, sel]
        v_sel = v_blocks[b_ix, h_ix, sel]
        s_sel = jnp.einsum('bhqd,bhqnkd->bhqnk', qb, k_sel).astype(F32)
        dist_sel = (t[:, None, None] - (sel[..., None] * MOBA_BLOCK + offs)).astype(F32)
        s_sel = s_sel - slopes[:, None, None, None] * dist_sel
        s_sel = jnp.where(sel_valid[:, None], s_sel, -jnp.inf)
        k_own = lax.dynamic_slice_in_dim(kt, own * MOBA_BLOCK, MOBA_BLOCK, axis=2)
        v_own = lax.dynamic_slice_in_dim(vt, own * MOBA_BLOCK, MOBA_BLOCK, axis=2)
        dist_own = t[:, None] - (own * MOBA_BLOCK + offs)[None, :]
        s_own = jnp.einsum('bhqd,bhkd->bhqk', qb, k_own).astype(F32)
        s_own = jnp.where(dist_own >= 0,
                          s_own - slopes[:, None, None] * dist_own.astype(F32), -jnp.inf)
        scores = jnp.concatenate(
            [s_own, s_sel.reshape(b_, h, qb_len, top * MOBA_BLOCK)], axis=-1)
        p = jax.nn.softmax(scores, axis=-1).astype(vt.dtype)
        p_own = p[..., :MOBA_BLOCK]
        p_sel = p[..., MOBA_BLOCK:].reshape(b_, h, qb_len, top, MOBA_BLOCK)
        return (jnp.einsum('bhqk,bhkd->bhqd', p_own, v_own)
                + jnp.einsum('bhqnk,bhqnkd->bhqd', p_sel, v_sel))

    starts = jnp.arange(0, s, qb_len)
    out = lax.map(one_query_block, starts)
    return out.transpose(1, 0, 3, 2, 4).reshape(b_, s, h * dh)


def causal_depthwise_conv(x, w, b):
    k = w.shape[0]
    y = lax.conv_general_dilated(x, w[:, None, :], window_strides=(1,),
                                 padding=[(k - 1, 0)],
                                 dimension_numbers=('NWC', 'WIO', 'NWC'),
                                 feature_group_count=x.shape[-1])
    return y + b


def ssd_scan(x, dt, a, bm, cm):
    b_, s, h, p = x.shape
    g, n = bm.shape[2], bm.shape[3]
    hg = h // g
    q = SSM_CHUNK
    nc = s // q
    xc = x.astype(F32).reshape(b_, nc, q, g, hg, p)
    dtc = dt.astype(F32).reshape(b_, nc, q, g, hg)
    bc = bm.astype(F32).reshape(b_, nc, q, g, n)
    cc = cm.astype(F32).reshape(b_, nc, q, g, n)
    a_cs = jnp.cumsum(dtc * a.astype(F32).reshape(g, hg), axis=2)
    tri = jnp.tril(jnp.ones((q, q), dtype=bool))[:, :, None, None]
    seg = a_cs[:, :, :, None] - a_cs[:, :, None, :]
    decay = jnp.exp(jnp.where(tri, seg, -jnp.inf))
    cb = jnp.einsum('bcign,bcjgn->bcijg', cc, bc)
    wts = cb[..., None] * decay * dtc[:, :, None]
    y_diag = jnp.einsum('bcijgh,bcjghp->bcighp', wts, xc)
    decay_to_end = jnp.exp(a_cs[:, :, -1:] - a_cs)
    states = jnp.einsum('bcjgn,bcjgh,bcjghp->bcghpn', bc, decay_to_end * dtc, xc)
    chunk_decay = jnp.exp(a_cs[:, :, -1])

    def step(carry, inp):
        st, dec = inp
        return carry * dec[..., None, None] + st, carry

    h0 = jnp.zeros((b_, g, hg, p, n), F32)
    _, h_prev = lax.scan(step, h0, (jnp.swapaxes(states, 0, 1), jnp.swapaxes(chunk_decay, 0, 1)))
    h_prev = jnp.swapaxes(h_prev, 0, 1)
    y_off = jnp.einsum('bcign,bcghpn,bcigh->bcighp', cc, h_prev, jnp.exp(a_cs))
    return (y_diag + y_off).reshape(b_, s, h, p).astype(x.dtype)


def hybrid_mixer(xn, w_in, conv_w, conv_b, dt_bias, a_log, d_skip, ssm_norm_g,
                 sgu_ln_g, sgu_ln_b, sgu_w, sgu_b, p_attn, p_ssm, p_sgu, w_out):
    b_, s, _ = xn.shape
    split_at = np.cumsum(IN_SIZES)[:-1].tolist()
    q, k, v, z, xbc, dt_raw, u, vg, gate_raw = jnp.split(xn @ w_in, split_at, axis=-1)

    y_a = moba_attention(q.reshape(b_, s, ATTN_HEADS, ATTN_HEAD_DIM),
                         k.reshape(b_, s, ATTN_HEADS, ATTN_HEAD_DIM),
                         v.reshape(b_, s, ATTN_HEADS, ATTN_HEAD_DIM))

    xbc = jax.nn.silu(causal_depthwise_conv(xbc, conv_w, conv_b))
    xs, bm, cm = jnp.split(xbc, [SSM_WIDTH, SSM_WIDTH + SSM_GROUPS * SSM_STATE], axis=-1)
    xs = xs.reshape(b_, s, SSM_HEADS, SSM_HEAD_DIM)
    bm = bm.reshape(b_, s, SSM_GROUPS, SSM_STATE)
    cm = cm.reshape(b_, s, SSM_GROUPS, SSM_STATE)
    dt = jax.nn.softplus(dt_raw.astype(F32) + dt_bias.astype(F32))
    a = -jnp.exp(a_log.astype(F32))
    y_s = ssd_scan(xs, dt, a, bm, cm) + d_skip[:, None] * xs
    y_s = rms_norm(y_s.reshape(b_, s, SSM_WIDTH) * jax.nn.silu(z), ssm_norm_g)

    u = jax.nn.gelu(u)
    vg = layer_norm(jax.nn.gelu(vg), sgu_ln_g, sgu_ln_b)
    vg = vg.reshape(b_, s // SGU_CHUNK, SGU_CHUNK, SGU_GROUPS, SGU_GROUP_DIM)
    w_sp = jnp.tril(sgu_w)
    sv = jnp.einsum('gij,bcjgd->bcigd', w_sp, vg) + sgu_b.T[None, None, :, :, None]
    y_c = u * sv.reshape(b_, s, SGU_WIDTH)

    gates = jax.nn.sigmoid(gate_raw).reshape(b_, s, N_BRANCHES, D_MODEL)
    merged = (gates[:, :, 0] * (y_a @ p_attn)
              + gates[:, :, 1] * (y_s @ p_ssm)
              + gates[:, :, 2] * (y_c @ p_sgu))
    return merged @ w_out


def setup_inputs(seed: int = 0) -> dict:
    key = jax.random.key(seed)
    ks = jax.random.split(key, 32)
    L = DEPTH

    def nrm(k, shape, scale):
        return jax.random.normal(k, shape, F32) * scale

    def gain(k, shape):
        return 1.0 + 0.05 * jax.random.normal(k, shape, F32)

    dt0 = jnp.exp(jax.random.uniform(ks[9], (L, SSM_HEADS), F32)
                  * (jnp.log(0.1) - jnp.log(0.001)) + jnp.log(0.001))
    return {
        "x": jax.random.normal(ks[0], (BATCH, SEQ, D_MODEL), F32),
        "ffn1_pre_g": gain(ks[1], (L, D_MODEL)),
        "ffn1_w_gu": nrm(ks[2], (L, D_MODEL, 2 * FFN_HIDDEN), D_MODEL ** -0.5),
        "ffn1_w_down": nrm(ks[3], (L, FFN_HIDDEN, D_MODEL), FFN_HIDDEN ** -0.5),
        "ffn1_post_g": gain(ks[4], (L, D_MODEL)),
        "mix_pre_g": gain(ks[5], (L, D_MODEL)),
        "w_in": nrm(ks[6], (L, D_MODEL, IN_COLS), D_MODEL ** -0.5),
        "conv_w": nrm(ks[7], (L, SSM_CONV, SSM_CONV_CH), SSM_CONV ** -0.5),
        "conv_b": nrm(ks[8], (L, SSM_CONV_CH), 0.02),
        "dt_bias": dt0 + jnp.log(-jnp.expm1(-dt0)),
        "a_log": jnp.log(jax.random.uniform(ks[10], (L, SSM_HEADS), F32, 1.0, 16.0)),
        "d_skip": gain(ks[11], (L, SSM_HEADS)),
        "ssm_norm_g": gain(ks[12], (L, SSM_WIDTH)),
        "sgu_ln_g": gain(ks[13], (L, SGU_WIDTH)),
        "sgu_ln_b": nrm(ks[14], (L, SGU_WIDTH), 0.02),
        "sgu_w": nrm(ks[15], (L, SGU_GROUPS, SGU_CHUNK, SGU_CHUNK), SGU_CHUNK ** -0.5),
        "sgu_b": gain(ks[16], (L, SGU_GROUPS, SGU_CHUNK)),
        "p_attn": nrm(ks[17], (L, ATTN_WIDTH, D_MODEL), ATTN_WIDTH ** -0.5),
        "p_ssm": nrm(ks[18], (L, SSM_WIDTH, D_MODEL), SSM_WIDTH ** -0.5),
        "p_sgu": nrm(ks[19], (L, SGU_WIDTH, D_MODEL), SGU_WIDTH ** -0.5),
        "w_out": nrm(ks[20], (L, D_MODEL, D_MODEL), D_MODEL ** -0.5),
        "mix_post_g": gain(ks[21], (L, D_MODEL)),
        "ffn2_pre_g": gain(ks[22], (L, D_MODEL)),
        "ffn2_w_gu": nrm(ks[23], (L, D_MODEL, 2 * FFN_HIDDEN), D_MODEL ** -0.5),
        "ffn2_w_down": nrm(ks[24], (L, FFN_HIDDEN, D_MODEL), FFN_HIDDEN ** -0.5),
        "ffn2_post_g": gain(ks[25], (L, D_MODEL)),
    }


def reference(x, ffn1_pre_g, ffn1_w_gu, ffn1_w_down, ffn1_post_g, mix_pre_g, w_in,
              conv_w, conv_b, dt_bias, a_log, d_skip, ssm_norm_g, sgu_ln_g, sgu_ln_b,
              sgu_w, sgu_b, p_attn, p_ssm, p_sgu, w_out, mix_post_g, ffn2_pre_g,
              ffn2_w_gu, ffn2_w_down, ffn2_post_g):
    h = x
    for i in range(DEPTH):
        f1 = swiglu_ffn(rms_norm(h, ffn1_pre_g[i]), ffn1_w_gu[i], ffn1_w_down[i])
        h = h + 0.5 * rms_norm(f1, ffn1_post_g[i])
        m = hybrid_mixer(rms_norm(h, mix_pre_g[i]), w_in[i], conv_w[i], conv_b[i],
                         dt_bias[i], a_log[i], d_skip[i], ssm_norm_g[i], sgu_ln_g[i],
                         sgu_ln_b[i], sgu_w[i], sgu_b[i], p_attn[i], p_ssm[i], p_sgu[i],
                         w_out[i])
        h = h + rms_norm(m, mix_post_g[i])
        f2 = swiglu_ffn(rms_norm(h, ffn2_pre_g[i]), ffn2_w_gu[i], ffn2_w_down[i])
        h = h + 0.5 * rms_norm(f2, ffn2_post_g[i])
    return h
```

```python
import numpy as np
from contextlib import ExitStack
import concourse.bass as bass
import concourse.mybir as mybir
from concourse.bass_utils import run_bass_kernel_spmd

F32 = mybir.dt.float32
BF16 = mybir.dt.bfloat16
AF = mybir.ActivationFunctionType
ALU = mybir.AluOpType
AX = mybir.AxisListType

D = 1024
NL = 2
FH = 2816
NJ = FH // 128
INC = 10768
TS = 512
EPS = 1e-6
NEG = -30000.0
OQ, OK_, OV, OZ, OX, ODT, OU, OVG, OG = 0, 1024, 2048, 3072, 4096, 5632, 5648, 6672, 7696

EPOCH = 30000
DEPOCH = 1800
KSLOT = 8


class Buf:
    __slots__ = ("w", "r", "name")

    def __init__(self, name=""):
        self.w = None
        self.r = {}
        self.name = name


def bufs(n, name=""):
    return [Buf(f"{name}{i}") for i in range(n)]


class _Op:
    __slots__ = ("fn", "kind", "waits", "signal", "sigval", "slot", "use")

    def __init__(self, fn, kind):
        self.fn = fn
        self.kind = kind
        self.waits = []
        self.signal = False
        self.sigval = 0
        self.slot = -1
        self.use = 0


class _Eng:
    def __init__(self, name):
        self.name = name
        self.ops = []
        self.seen = {}
        self.ndma = 0


class Sched:
    ENGS = ("pe", "act", "dve", "pool", "sp")

    def __init__(self, nc):
        self.nc = nc
        self.e = {n: _Eng(n) for n in self.ENGS}

    def op(self, eng, fn, reads=(), writes=(), kind="c"):
        e = self.e[eng]
        o = _Op(fn, kind)
        idx = len(e.ops)
        deps = {}

        def add(ev, hard):
            if ev is None:
                return
            k, v = ev
            if (not hard) and kind == "c" and k == ("c", eng):
                return
            if deps.get(k, -1) < v:
                deps[k] = v

        for b in reads:
            add(b.w, True)
        for b in writes:
            add(b.w, eng != "pe")
            for k, v in b.r.items():
                add((k, v), False)
        if kind == "d":
            d = e.ndma
            e.ndma += 1
            o.slot = d % KSLOT
            o.use = d // KSLOT + 1
            if o.use > 1:
                add((("d", eng, o.slot), o.use - 1), True)
            ev = (("d", eng, o.slot), o.use)
        else:
            ev = (("c", eng), idx)
        for k, v in deps.items():
            if e.seen.get(k, -1) >= v:
                continue
            e.seen[k] = v
            o.waits.append((k, v))
            if k[0] == "c":
                self.e[k[1]].ops[v].signal = True
        for b in reads:
            k, v = ev
            if b.r.get(k, -1) < v:
                b.r[k] = v
        for b in writes:
            b.w = ev
            b.r = {}
        e.ops.append(o)
        return ev

    def barrier(self):
        evs = []
        for n, e in self.e.items():
            for i in range(len(e.ops) - 1, -1, -1):
                if e.ops[i].kind == "c":
                    evs.append((("c", n), i))
                    break
            d = e.ndma
            for s in range(min(KSLOT, d)):
                evs.append((("d", n, s), (d - 1 - s) // KSLOT + 1))
        for n in self.ENGS:
            e = self.e[n]
            o = _Op(None, "n")
            for k, v in evs:
                if k == ("c", n):
                    continue
                if e.seen.get(k, -1) >= v:
                    continue
                e.seen[k] = v
                o.waits.append((k, v))
                if k[0] == "c":
                    self.e[k[1]].ops[v].signal = True
            if o.waits:
                e.ops.append(o)

    def emit(self):
        nc = self.nc
        for e in self.e.values():
            c = 0
            for o in e.ops:
                if o.kind == "c" and o.signal:
                    c += 1
                    o.sigval = c
            e.nsig = c
        with ExitStack() as st:
            sems = {}

            def sem(key):
                if key not in sems:
                    sems[key] = st.enter_context(nc.semaphore("s_" + "_".join(str(x) for x in key)))
                return sems[key]

            for n, e in self.e.items():
                for ep in range((e.nsig + EPOCH - 1) // EPOCH):
                    sem(("c", n, ep))
                if e.ndma:
                    maxuse = (e.ndma - 1) // KSLOT + 1
                    for s in range(min(KSLOT, e.ndma)):
                        for ep in range((maxuse + DEPOCH - 1) // DEPOCH):
                            sem(("d", n, s, ep))

            def resolve(k, v):
                if k[0] == "c":
                    sv = self.e[k[1]].ops[v].sigval
                    assert sv > 0
                    return sem(("c", k[1], (sv - 1) // EPOCH)), (sv - 1) % EPOCH + 1
                return sem(("d", k[1], k[2], (v - 1) // DEPOCH)), 16 * ((v - 1) % DEPOCH + 1)

            block = st.enter_context(nc.Block())
            regs = {"pe": block.tensor, "act": block.scalar, "dve": block.vector,
                    "pool": block.gpsimd, "sp": block.sync}
            for n in self.ENGS:
                e = self.e[n]
                if not e.ops:
                    continue

                def body(eng, e=e, n=n):
                    for o in e.ops:
                        for k, v in o.waits:
                            s, val = resolve(k, v)
                            eng.wait_ge(s, val)
                        if o.fn is None:
                            continue
                        ins = o.fn(eng)
                        if o.kind == "d":
                            ins.then_inc(sem(("d", n, o.slot, (o.use - 1) // DEPOCH)), 16)
                        elif o.signal:
                            ins.then_inc(sem(("c", n, (o.sigval - 1) // EPOCH)), 1)

                regs[n](body)

    def stats(self):
        return {n: (len(e.ops), sum(len(o.waits) for o in e.ops), e.ndma) for n, e in self.e.items()}


class Arena:
    def __init__(self, nc, nfloats):
        self.t = nc.alloc_sbuf_tensor("arena", [128, nfloats], F32)
        self.n = nfloats
        self.p = 0
        self.base = 0

    def f32(self, n):
        n8 = (n + 7) // 8 * 8
        assert self.p + n8 <= self.n, f"arena overflow {self.p}+{n8}>{self.n}"
        a = self.t[:, self.p:self.p + n]
        self.p += n8
        return a

    def bf16(self, n):
        nf = (n + 1) // 2
        n8 = (nf + 7) // 8 * 8
        assert self.p + n8 <= self.n, f"arena overflow {self.p}+{n8}>{self.n}"
        a = self.t[:, self.p:self.p + nf].bitcast(BF16)
        self.p += n8
        return a

    def keep(self):
        self.base = self.p

    def reset(self):
        self.p = self.base


def r3(ap, a):
    return ap.rearrange("p (a b) -> p a b", a=a)


class Prog:
    def __init__(self, S, nl=NL, dbg=(), stop_after=None):
        self.S = S
        self.nl = nl
        self.NT = S // TS
        self.NB = S // 256
        self.dbg = set(dbg)
        self.stop_after = stop_after
        nc = self.nc = bass.Bass("TRN2", target_bir_lowering=False)
        self.sch = Sched(nc)
        self.ar = Arena(nc, 52736)
        self.dram = {}
        self.dbuf = {}
        self.ps = [nc.alloc_psum_tensor(f"ps{i}", [128, 512], F32)[:] for i in range(8)]
        self.psb = bufs(8, "ps")
        self.rr = 0

    def din(self, name, shape, dt=F32):
        t = self.nc.dram_tensor(name, list(shape), dt, kind="ExternalInput").ap()
        self.dram[name] = t
        return t

    def dscr(self, name, shape, dt, out=False):
        kind = "ExternalOutput" if (out or name in self.dbg) else "Internal"
        t = self.nc.dram_tensor(name, list(shape), dt, kind=kind).ap()
        self.dram[name] = t
        return t

    def db(self, key):
        if key not in self.dbuf:
            self.dbuf[key] = Buf(str(key))
        return self.dbuf[key]

    def dma(self, out, in_, reads, writes, eng="sp"):
        self.sch.op(eng, lambda e: e.dma_start(out=out, in_=in_), reads, writes, kind="d")

    def mm(self, out, lhsT, rhs, start, stop, reads, writes, **kw):
        self.sch.op("pe", lambda e: e.matmul(out, lhsT, rhs, start=start, stop=stop, **kw), reads, writes)

    def tr(self, out, in_, ident, reads, writes):
        self.sch.op("pe", lambda e: e.transpose(out, in_, ident), reads, writes)

    def act(self, out, in_, func, reads, writes, bias=None, scale=None, accum=None):
        kw = {}
        if bias is not None:
            kw["bias"] = bias
        if scale is not None:
            kw["scale"] = scale
        if accum is not None:
            kw["accum_out"] = accum
        self.sch.op("act", lambda e: e.activation(out=out, in_=in_, func=func, **kw), reads, writes)

    def tt(self, eng, out, in0, in1, op, reads, writes):
        self.sch.op(eng, lambda e: e.tensor_tensor(out=out, in0=in0, in1=in1, op=op), reads, writes)

    def tsc(self, eng, out, in0, s1, s2, op0, op1, reads, writes):
        if s2 is None:
            self.sch.op(eng, lambda e: e.tensor_scalar(out=out, in0=in0, scalar1=s1, scalar2=None, op0=op0), reads, writes)
        else:
            self.sch.op(eng, lambda e: e.tensor_scalar(out=out, in0=in0, scalar1=s1, scalar2=s2, op0=op0, op1=op1), reads, writes)

    def stt(self, eng, out, in0, scalar, in1, op0, op1, reads, writes):
        self.sch.op(eng, lambda e: e.scalar_tensor_tensor(out=out, in0=in0, scalar=scalar, in1=in1, op0=op0, op1=op1), reads, writes)

    def cp(self, eng, out, in_, reads, writes):
        if eng == "act":
            self.act(out, in_, AF.Copy, reads, writes)
        else:
            self.sch.op(eng, lambda e: e.tensor_copy(out=out, in_=in_), reads, writes)

    def memset(self, eng, ap, val, writes):
        self.sch.op(eng, lambda e: e.memset(ap, val), (), writes)

    def recip(self, out, in_, reads, writes):
        self.sch.op("dve", lambda e: e.reciprocal(out=out, in_=in_), reads, writes)

    def stage_begin(self):
        self.sch.barrier()
        self.ar.reset()

    def declare(self):
        S, nl = self.S, self.nl
        self.xT = self.din("xT", [D, S])
        self.cst = self.din("cst", [128, 512])
        self.selb = self.din("selb", [35, self.S])
        self.arow = self.din("arow", [8, 3, 512])
        self.selh = self.din("selh", [16, 16 * 128])
        self.W = []
        for l in range(nl):
            w = {}
            w["gu1"] = self.din(f"gu1_{l}", [D, 2 * FH])
            w["dn1"] = self.din(f"dn1_{l}", [FH, D])
            w["win"] = self.din(f"win_{l}", [D, INC])
            w["pa"] = self.din(f"pa_{l}", [D, D])
            w["pss"] = self.din(f"pss_{l}", [D, D])
            w["pc"] = self.din(f"pc_{l}", [D, D])
            w["wo"] = self.din(f"wo_{l}", [D, D])
            w["gu2"] = self.din(f"gu2_{l}", [D, 2 * FH])
            w["dn2"] = self.din(f"dn2_{l}", [FH, D])
            w["vec"] = self.din(f"vec_{l}", [128, 128])
            w["rows"] = self.din(f"rows_{l}", [1, 3 * 1024 + 32])
            w["sguwT"] = self.din(f"sguwT_{l}", [128, 8 * 128])
            self.W.append(w)
        self.WB = []
        for l in range(nl):
            wb = {}
            for k, shp in (("gu1", [D, 2 * FH]), ("dn1", [FH, D]), ("win", [D, INC]), ("pa", [D, D]),
                           ("pss", [D, D]), ("pc", [D, D]), ("wo", [D, D]), ("gu2", [D, 2 * FH]), ("dn2", [FH, D])):
                wb[k] = self.dscr(f"wb_{k}_{l}", shp, BF16)
            self.WB.append(wb)
        self.hT = self.dscr("hT", [D, S], F32)
        self.outT = self.dscr("outT", [D, S], F32, out=True)
        self.QT = self.dscr("QT", [D, S], BF16)
        self.KT = self.dscr("KT", [D, S], BF16)
        self.V = self.dscr("V", [S, D], BF16)
        self.ZS = self.dscr("ZS", [D, S], BF16)
        self.XC = self.dscr("XC", [1536, S], BF16)
        self.DT = self.dscr("DT", [S, 32], F32)
        self.GT = self.dscr("GT", [3 * D, S], BF16)
        self.YC = self.dscr("YC", [D, S], BF16)
        self.YA = self.dscr("YA", [D, S], BF16)
        self.YS = self.dscr("YS", [D, S], BF16)
        self.KM = self.dscr("KM", [128, 8 * self.NB], F32)

    def setup_consts(self):
        ar = self.ar
        self.c_f = ar.f32(512)
        self.b_cf = Buf("cst")
        self.dma(self.c_f, self.cst, [], [self.b_cf])
        self.ident_f = self.c_f[:, 0:128]
        self.U_f = self.c_f[:, 128:256]
        self.negm_f = self.c_f[:, 256:384]
        self.c_b = ar.bf16(384)
        self.b_cb = Buf("cstb")
        self.cp("dve", self.c_b, self.c_f[:, 0:384], [self.b_cf], [self.b_cb])
        self.ident_b = self.c_b[:, 0:128]
        self.U_b = self.c_b[:, 128:256]
        self.ones_b = ar.bf16(128)
        self.ones_f = ar.f32(128)
        self.b_ones = Buf("ones")
        self.memset("pool", self.ones_b, 1.0, [self.b_ones])
        self.memset("pool", self.ones_f, 1.0, [self.b_ones])
        self.vec = []
        self.b_vec = []
        for l in range(self.nl):
            v = ar.f32(128)
            b = Buf(f"vec{l}")
            self.dma(v, self.W[l]["vec"], [], [b])
            self.vec.append(v)
            self.b_vec.append(b)
        ar.keep()

    def vcol(self, l, name, c):
        off = {"g_f1pre": 0, "g_f1post": 8, "g_mpre": 16, "g_mpost": 24, "g_f2pre": 32, "g_f2post": 40,
               "g_ssm": 48, "conv_w": 56, "conv_b": 104, "dskip": 116}[name]
        return self.vec[l][:, off + c: off + c + 1]

    def stage_convert(self):
        self.stage_begin()
        ar = self.ar
        NBUF = 4
        CW = 2048
        src = [ar.f32(CW) for _ in range(NBUF)]
        dst = [ar.bf16(CW) for _ in range(NBUF)]
        bs = bufs(NBUF, "cvs")
        bd = bufs(NBUF, "cvd")
        engs = ["act", "dve", "pool"]
        i = 0
        for l in range(self.nl):
            for k, wsrc in self.W[l].items():
                if k in ("vec", "rows", "sguwT"):
                    continue
                wdst = self.WB[l][k]
                K, N = wsrc.shape
                for kc in range(K // 128):
                    for c0 in range(0, N, CW):
                        cw = min(CW, N - c0)
                        import os
                        if i >= int(os.environ.get("CVLIM", "1000000")):
                            continue
                        b = i % NBUF
                        self.dma(src[b][:, 0:cw], wsrc[kc * 128:(kc + 1) * 128, c0:c0 + cw], [], [bs[b]])
                        self.cp(engs[i % 3], dst[b][:, 0:cw], src[b][:, 0:cw], [bs[b]], [bd[b]])
                        self.dma(wdst[kc * 128:(kc + 1) * 128, c0:c0 + cw], dst[b][:, 0:cw], [bd[b]],
                                 [self.db(("wb", l, k))], eng=("pool" if i % 2 == 0 else "act"))
                        i += 1

    def rms_stats(self, src_f, b_src, rstd, b_rstd, sq, b_sq, psi):
        ps, pb = self.ps[psi], self.psb[psi]
        for c in range(8):
            j = c % 2
            self.act(sq[j], src_f[:, c, :], AF.Square, [b_src[c]], [b_sq[j]], scale=1.0 / 32.0)
            self.mm(ps, self.ones_b, sq[j], c == 0, c == 7, [self.b_ones, b_sq[j]], [pb])
        self.act(rstd, ps, AF.Sqrt, [pb, self.b_eps], [b_rstd], bias=self.eps_col, scale=1.0)
        self.recip(rstd, rstd, [b_rstd], [b_rstd])

    def alloc_norm_tmp(self):
        ar = self.ar
        self.sq = [ar.bf16(512) for _ in range(2)]
        self.b_sq = bufs(2, "sq")
        self.rstd = [ar.f32(512) for _ in range(2)]
        self.b_rstd = bufs(2, "rstd")
        self.eps_col = ar.f32(8)[:, 0:1]
        self.b_eps = Buf("eps")
        self.memset("pool", self.eps_col, EPS, [self.b_eps])

    def prenorm(self, l, gname, hres, b_h, xn, b_xn, k):
        self.rms_stats(hres, b_h, self.rstd[k], self.b_rstd[k], self.sq, self.b_sq, 7)
        for c in range(8):
            self.stt("dve", xn[:, c, :], hres[:, c, :], self.vcol(l, gname, c), self.rstd[k],
                     ALU.mult, ALU.mult, [b_h[c], self.b_rstd[k], self.b_vec[l]], [b_xn[c]])

    def postnorm_res(self, l, gname, f, b_f, hres, b_h, coef, k):
        self.rms_stats(f, b_f, self.rstd[k], self.b_rstd[k], self.sq, self.b_sq, 7)
        for c in range(8):
            eng = "pool"
            self.stt("dve", f[:, c, :], f[:, c, :], self.gc[l][gname][:, c:c + 1], self.rstd[k], ALU.mult, ALU.mult,
                     [b_f[c], self.b_rstd[k], self.b_gc], [b_f[c]])
            self.tt(eng, hres[:, c, :], hres[:, c, :], f[:, c, :], ALU.add, [b_f[c], b_h[c]], [b_h[c]])

    def setup_gc(self):
        ar = self.ar
        self.gc = []
        self.b_gc = Buf("gc")
        for l in range(self.nl):
            t = ar.f32(24)
            d = {}
            for i, (nm, coef) in enumerate((("g_f1post", 0.5), ("g_mpost", 1.0), ("g_f2post", 0.5))):
                off = {"g_f1post": 8, "g_mpost": 24, "g_f2post": 40}[nm]
                self.tsc("dve", t[:, i * 8:(i + 1) * 8], self.vec[l][:, off:off + 8], coef, None, ALU.mult, None,
                         [self.b_vec[l]], [self.b_gc])
                d[nm] = t[:, i * 8:(i + 1) * 8]
            self.gc.append(d)
        ar.keep()

    def stage_ffn(self, l, which, src, dst, src_key, dst_key):
        self.stage_begin()
        ar = self.ar
        NT = self.NT
        wgu = self.WB[l]["gu1" if which == 1 else "gu2"]
        wdn = self.WB[l]["dn1" if which == 1 else "dn2"]
        bw_gu = self.db(("wb", l, "gu1" if which == 1 else "gu2"))
        bw_dn = self.db(("wb", l, "dn1" if which == 1 else "dn2"))
        gpre = "g_f1pre" if which == 1 else "g_f2pre"
        gpost = "g_f1post" if which == 1 else "g_f2post"
        self.alloc_norm_tmp()
        hres = [r3(ar.f32(8 * TS), 8) for _ in range(2)]
        b_h = [bufs(8, f"hres{i}_") for i in range(2)]
        xn = r3(ar.bf16(8 * TS), 8)
        b_xn = bufs(8, "xn")
        HT = r3(ar.bf16(NJ * TS), NJ)
        b_H = bufs(NJ, "H")
        f = r3(ar.f32(8 * TS), 8)
        b_f = bufs(8, "f")
        sg = [ar.f32(TS) for _ in range(2)]
        b_sg = bufs(2, "sg")
        JG = 2
        NG = NJ // JG
        wg = [r3(ar.bf16(8 * 2 * JG * 128), 8) for _ in range(3)]
        b_wg = bufs(3, "wg")
        CG = 2
        wd = [r3(ar.bf16(NJ * CG * 128), NJ) for _ in range(2)]
        b_wd = bufs(2, "wd")
        srcv = src.rearrange("(c p) s -> p c s", p=128)
        dstv = dst.rearrange("(c p) s -> p c s", p=128)
        wguv = wgu.rearrange("(kc p) n -> p kc n", p=128)
        wdnv = wdn.rearrange("(j p) n -> p j n", p=128)
        gi = 0
        di = 0
        self.dma(hres[0], srcv[:, :, 0:TS], [self.db((src_key, 0))], b_h[0])
        self.prenorm(l, gpre, hres[0], b_h[0], xn, b_xn, 0)
        for t in range(NT):
            k = t % 2
            t0 = t * TS
            if t + 1 < NT:
                self.dma(hres[1 - k], srcv[:, :, t0 + TS:t0 + 2 * TS], [self.db((src_key, t + 1))], b_h[1 - k])
            for g in range(NG):
                wb_ = gi % 3
                gi += 1
                j0 = g * JG
                self.dma(wg[wb_][:, :, 0:JG * 128], wguv[:, :, j0 * 128:(j0 + JG) * 128], [bw_gu], [b_wg[wb_]])
                self.dma(wg[wb_][:, :, JG * 128:2 * JG * 128], wguv[:, :, FH + j0 * 128:FH + (j0 + JG) * 128], [bw_gu], [b_wg[wb_]])
                for jj in range(JG):
                    j = j0 + jj
                    pg = (2 * (j % 2))
                    pu = pg + 1
                    for kc in range(8):
                        self.mm(self.ps[pg], wg[wb_][:, kc, jj * 128:(jj + 1) * 128], xn[:, kc, :], kc == 0, kc == 7,
                                [b_wg[wb_], b_xn[kc]], [self.psb[pg]])
                    for kc in range(8):
                        self.mm(self.ps[pu], wg[wb_][:, kc, JG * 128 + jj * 128:JG * 128 + (jj + 1) * 128], xn[:, kc, :],
                                kc == 0, kc == 7, [b_wg[wb_], b_xn[kc]], [self.psb[pu]])
                    s = j % 2
                    self.act(sg[s], self.ps[pg], AF.Silu, [self.psb[pg]], [b_sg[s]])
                    self.tt("dve", HT[:, j, :], sg[s], self.ps[pu], ALU.mult, [b_sg[s], self.psb[pu]], [b_H[j]])
            if t + 1 < NT:
                self.prenorm(l, gpre, hres[1 - k], b_h[1 - k], xn, b_xn, 1 - k)
            for cg in range(8 // CG):
                wb_ = di % 2
                di += 1
                self.dma(wd[wb_], wdnv[:, :, cg * CG * 128:(cg + 1) * CG * 128], [bw_dn], [b_wd[wb_]])
                for cc in range(CG):
                    c = cg * CG + cc
                    pd = 4 + (c % 2)
                    for j in range(NJ):
                        self.mm(self.ps[pd], wd[wb_][:, j, cc * 128:(cc + 1) * 128], HT[:, j, :], j == 0, j == NJ - 1,
                                [b_wd[wb_], b_H[j]], [self.psb[pd]])
                    self.cp("act", f[:, c, :], self.ps[pd], [self.psb[pd]], [b_f[c]])
            self.postnorm_res(l, gpost, f, b_f, hres[k], b_h[k], None, k)
            self.dma(dstv[:, :, t0:t0 + TS], hres[k], b_h[k], [self.db((dst_key, t))], eng="pool")

    def build(self):
        self.declare()
        self.setup_consts()
        self.setup_gc()
        self.stage_convert()
        stages = []
        for l in range(self.nl):
            src = self.xT if l == 0 else self.hT
            stages.append(("ffn1", lambda l=l, src=src: self.stage_ffn(l, 1, src, self.hT, "x" if l == 0 else "h", "h")))
            stages.append(("proj", lambda l=l: self.stage_proj(l)))
            stages.append(("attn", lambda l=l: self.stage_attn(l)))
            stages.append(("ssd", lambda l=l: self.stage_ssd(l)))
            stages.append(("merge", lambda l=l: self.stage_merge(l)))
            last = (l == self.nl - 1)
            stages.append(("ffn2", lambda l=l, last=last: self.stage_ffn(l, 2, self.hT, self.outT if last else self.hT, "h",
                                                                          "o" if last else "h")))
        for i, (nm, fn) in enumerate(stages):
            if self.stop_after is not None and i > self.stop_after:
                break
            fn()
        self.sch.barrier()
        self.sch.emit()
        return self.nc


def host_consts(S):
    NB = S // 256
    cst = np.zeros((128, 512), np.float32)
    cst[:, 0:128] = np.eye(128, dtype=np.float32)
    t = np.arange(128)
    cst[:, 128:256] = (t[:, None] <= t[None, :]).astype(np.float32)
    cst[:, 256:384] = np.where(t[:, None] <= t[None, :], 0.0, NEG)
    cst[:, 384:512] = t[None, :].astype(np.float32)
    NKT = S // 128
    selb = np.zeros((35, NKT * 128), np.float32)
    for kt in range(NKT):
        selb[kt // 2, kt * 128:(kt + 1) * 128] = -NEG
        selb[34, kt * 128:(kt + 1) * 128] = kt
    selb[32, :] = 1.0
    selb[33, :] = np.tile(t, NKT).astype(np.float32)
    slopes = 2.0 ** (-8.0 * (np.arange(8) + 1.0) / 8)
    arow = np.zeros((8, 3, 512), np.float32)
    for h in range(8):
        arow[h, 0, :] = -slopes[h] * np.arange(512)
        arow[h, 1, :] = slopes[h]
        arow[h, 2, :] = slopes[h] * 128.0
    selh = np.zeros((16, 16 * 128), np.float32)
    for h in range(16):
        selh[h, h * 128:(h + 1) * 128] = 1.0
    return {"cst": cst, "selb": selb, "arow": arow, "selh": selh}


def host_layer_inputs(inp, l):
    def colv(v):
        return np.ascontiguousarray(v.reshape(-1, 128).T)
    vec = np.zeros((128, 128), np.float32)
    for i, nm in enumerate(("ffn1_pre_g", "ffn1_post_g", "mix_pre_g", "mix_post_g", "ffn2_pre_g", "ffn2_post_g")):
        vec[:, i * 8:(i + 1) * 8] = colv(inp[nm][l])
    vec[:, 48:56] = colv(inp["ssm_norm_g"][l])
    cw = inp["conv_w"][l]
    vec[:, 56:104] = cw.T.reshape(12, 128, 4).transpose(1, 0, 2).reshape(128, 48)
    vec[:, 104:116] = colv(inp["conv_b"][l])
    vec[:, 116:124] = colv(np.repeat(inp["d_skip"][l], 64))
    rows = np.concatenate([inp["sgu_ln_g"][l], inp["sgu_ln_b"][l], inp["sgu_b"][l].reshape(-1),
                           inp["dt_bias"][l], inp["a_log"][l]])[None, :].astype(np.float32)
    sguwT = np.ascontiguousarray(inp["sgu_w"][l].transpose(2, 0, 1).reshape(128, 1024))
    d = {
        f"gu1_{l}": inp["ffn1_w_gu"][l], f"dn1_{l}": inp["ffn1_w_down"][l], f"win_{l}": inp["w_in"][l],
        f"pa_{l}": inp["p_attn"][l], f"pss_{l}": inp["p_ssm"][l], f"pc_{l}": inp["p_sgu"][l], f"wo_{l}": inp["w_out"][l],
        f"gu2_{l}": inp["ffn2_w_gu"][l], f"dn2_{l}": inp["ffn2_w_down"][l],
        f"vec_{l}": vec, f"rows_{l}": rows, f"sguwT_{l}": sguwT,
    }
    return {k: np.ascontiguousarray(v, dtype=np.float32) for k, v in d.items()}


_CACHE = {}


def kernel(**inputs):
    inp = {k: np.asarray(v) for k, v in inputs.items()}
    x = inp["x"]
    B, S, _ = x.shape
    key = (S,)
    if key not in _CACHE:
        _CACHE[key] = Prog(S).build()
    nc = _CACHE[key]
    shared = host_consts(S)
    for l in range(NL):
        shared.update(host_layer_inputs(inp, l))
    workers = [0, 1, 4, 5]
    big = [k for k, v in shared.items() if v.size >= (1 << 20)]
    zeros = dict(shared)
    for k in big:
        zeros[k] = np.zeros_like(shared[k])
    xz = np.zeros((D, S), np.float32)
    in_maps = []
    for c in range(8):
        if c in workers:
            m = dict(shared)
            m["xT"] = np.ascontiguousarray(x[workers.index(c)].T)
        else:
            m = dict(zeros)
            m["xT"] = xz
        in_maps.append(m)
    res = run_bass_kernel_spmd(nc, in_maps, core_ids=list(range(8)))
    out = np.stack([np.ascontiguousarray(np.asarray(res.results[workers[b]]["outT"]).T) for b in range(B)], axis=0)
    return out.astype(np.float32)


def stage_proj(self, l):
    self.stage_begin()
    ar = self.ar
    NT, NB, S = self.NT, self.NB, self.S
    winv = self.WB[l]["win"].rearrange("(kc p) n -> p kc n", p=128)
    bw = self.db(("wb", l, "win"))
    self.alloc_norm_tmp()
    self.alloc_gelu()
    hres = r3(ar.f32(8 * TS), 8)
    b_h = bufs(8, "ph")
    xn = r3(ar.bf16(8 * TS), 8)
    b_xn = bufs(8, "pxn")
    NWB = 4
    wblk = [r3(ar.bf16(8 * 512), 8) for _ in range(NWB)]
    b_wblk = bufs(NWB, "wblk")
    ob = [r3(ar.bf16(8 * TS), 8) for _ in range(3)]
    b_ob = bufs(3, "ob")
    gu = r3(ar.bf16(8 * TS), 8)
    b_gu = Buf("gu")
    xbc = r3(ar.f32(12 * (TS + 8)), 12)
    b_xbc = bufs(12, "xbc")
    cacc = [ar.f32(TS) for _ in range(2)]
    b_cacc = bufs(2, "cacc")
    xco = r3(ar.bf16(12 * TS), 12)
    b_xco = Buf("xco")
    vt = r3(ar.bf16(4 * 1024), 4)
    b_vt = Buf("vt")
    vgf = [ar.f32(1024) for _ in range(2)]
    b_vgf = bufs(2, "vgf")
    vgt = [ar.f32(1024) for _ in range(2)]
    b_vgt = bufs(2, "vgt")
    vgn = [ar.bf16(1024) for _ in range(2)]
    b_vgn = bufs(2, "vgn")
    rows = ar.f32(3 * 1024 + 32)
    b_rows = Buf("rows")
    self.dma(rows, self.W[l]["rows"].to_broadcast([128, 3 * 1024 + 32]), [], [b_rows])
    lng, lnb, sgub = rows[:, 0:1024], rows[:, 1024:2048], rows[:, 2048:3072]
    dtb, alog = rows[:, 3072:3088], rows[:, 3088:3104]
    abc = ar.f32(16)
    b_abc = Buf("abc")
    self.act(abc, alog, AF.Exp, [b_rows], [b_abc])
    self.tsc("dve", abc, abc, -1.0, None, ALU.mult, None, [b_abc], [b_abc])
    sg_all = ar.f32(1024)
    sgt = [sg_all[:, 0:512], sg_all[:, 512:1024]]
    b_sgt = bufs(2, "sgt")
    self.dma(sg_all, self.W[l]["sguwT"], [], b_sgt)
    wsT = r3(ar.bf16(1024), 8)
    b_wsT = Buf("wsT")
    self.tt("dve", wsT, r3(sg_all, 8), self.U_f[:, None, :].to_broadcast([128, 8, 128]), ALU.mult, b_sgt + [self.b_cf], [b_wsT])
    st = ar.f32(16)
    b_st = bufs(2, "st")
    st2 = [ar.f32(8) for _ in range(2)]
    yc = r3(ar.bf16(8 * TS), 8)
    b_yc = Buf("yc")
    dtt = r3(ar.f32(4 * 32), 4)
    b_dtt = Buf("dtt")
    dtmp = [ar.f32(16) for _ in range(2)]
    b_dtmp = bufs(2, "dtmp")
    km = ar.f32(8 * NB)
    b_km = Buf("km")

    hv = self.hT.rearrange("(c p) s -> p c s", p=128)
    QTv = self.QT.rearrange("(c p) s -> p c s", p=128)
    KTv = self.KT.rearrange("(c p) s -> p c s", p=128)
    ZSv = self.ZS.rearrange("(c p) s -> p c s", p=128)
    XCv = self.XC.rearrange("(c p) s -> p c s", p=128)
    GTv = self.GT.rearrange("(c p) s -> p c s", p=128)
    YCv = self.YC.rearrange("(c p) s -> p c s", p=128)
    Vv = self.V.rearrange("(s p) f -> p s f", p=128)
    DTv = self.DT.rearrange("(s p) c -> p s c", p=128)

    cnt = {"w": 0, "ps": 0, "ob": 0}
    import os
    PL = int(os.environ.get("PROJ_LIM", "99"))

    def load_block(c0, ncols, avoid=()):
        i = cnt["w"] % NWB
        cnt["w"] += 1
        while i in avoid:
            i = cnt["w"] % NWB
            cnt["w"] += 1
        self.dma(wblk[i][:, :, 0:ncols], winv[:, :, c0:c0 + ncols], [bw], [b_wblk[i]])
        return i

    def next_ps():
        i = cnt["ps"] % 6
        cnt["ps"] += 1
        return i

    def fm_segment(c0, nchunks, evac):
        for b0 in range(0, nchunks, 4):
            nb_ = min(4, nchunks - b0)
            wi = load_block(c0 + b0 * 128, nb_ * 128)
            for cc in range(nb_):
                p = next_ps()
                for kc in range(8):
                    self.mm(self.ps[p], wblk[wi][:, kc, cc * 128:(cc + 1) * 128], xn[:, kc, :], kc == 0, kc == 7,
                            [b_wblk[wi], b_xn[kc]], [self.psb[p]])
                evac(b0 + cc, p)

    def tm_segment(c0, ncols, evac):
        for bi, b0 in enumerate(range(0, ncols, 512)):
            w_ = min(512, ncols - b0)
            wi = load_block(c0 + b0, w_)
            for sub in range(4):
                p = next_ps()
                for kc in range(8):
                    self.mm(self.ps[p][:, 0:w_], xn[:, kc, sub * 128:(sub + 1) * 128], wblk[wi][:, kc, 0:w_], kc == 0, kc == 7,
                            [b_wblk[wi], b_xn[kc]], [self.psb[p]])
                evac(bi, sub, p, w_)

    for t in range(NT):
        t0 = t * TS
        self.dma(hres, hv[:, :, t0:t0 + TS], [self.db(("h", t))], b_h)
        self.prenorm(l, "g_mpre", hres, b_h, xn, b_xn, t % 2)
        if t == 0:
            self.memset("pool", xbc[:, :, 0:3], 0.0, b_xbc)
        else:
            self.cp("pool", xbc[:, :, 0:3], xbc[:, :, TS:TS + 3], b_xbc, b_xbc)

        if PL <= 0:
            continue
        oq = cnt["ob"] % 3
        cnt["ob"] += 1

        def ev_q(c, p, oq=oq):
            self.act(ob[oq][:, c, :], self.ps[p], AF.Copy, [self.psb[p]], [b_ob[oq]], scale=128.0 ** -0.5)
        fm_segment(OQ, 8, ev_q)
        self.dma(QTv[:, :, t0:t0 + TS], ob[oq], [b_ob[oq]], [self.db(("QT", t))], eng="act")
        if PL <= 1:
            continue
        okk = cnt["ob"] % 3
        cnt["ob"] += 1

        def ev_k(c, p, okk=okk, t=t):
            for hh in range(2):
                col = c * NB + 2 * t + hh
                self.act(ob[okk][:, c, hh * 256:(hh + 1) * 256], self.ps[p][:, hh * 256:(hh + 1) * 256], AF.Copy,
                         [self.psb[p]], [b_ob[okk], b_km], accum=km[:, col:col + 1])
        fm_segment(OK_, 8, ev_k)
        self.dma(KTv[:, :, t0:t0 + TS], ob[okk], [b_ob[okk]], [self.db(("KT", t))], eng="act")
        if PL <= 2:
            continue

        def ev_v(bi, sub, p, w_):
            self.act(vt[:, sub, bi * 512:(bi + 1) * 512], self.ps[p], AF.Copy, [self.psb[p]], [b_vt])
        tm_segment(OV, 1024, ev_v)
        self.dma(Vv[:, 4 * t:4 * t + 4, :], vt, [b_vt], [self.db(("V", t))], eng="act")
        if PL <= 3:
            continue
        oz = cnt["ob"] % 3
        cnt["ob"] += 1

        def ev_z(c, p, oz=oz):
            self.act(ob[oz][:, c, :], self.ps[p], AF.Silu, [self.psb[p]], [b_ob[oz]])
        fm_segment(OZ, 8, ev_z)
        self.dma(ZSv[:, :, t0:t0 + TS], ob[oz], [b_ob[oz]], [self.db(("ZS", t))], eng="act")
        if PL <= 4:
            continue

        def ev_x(c, p):
            self.cp("dve", xbc[:, c, 3:3 + TS], self.ps[p], [self.psb[p]], [b_xbc[c]])
            a = c % 2
            self.act(cacc[a], xbc[:, c, 3:3 + TS], AF.Identity, [b_xbc[c], self.b_vec[l]], [b_cacc[a]],
                     bias=self.vcol(l, "conv_b", c), scale=self.vcol(l, "conv_w", c * 4 + 3))
            for k in range(3):
                self.stt("dve", cacc[a], xbc[:, c, k:k + TS], self.vcol(l, "conv_w", c * 4 + k), cacc[a], ALU.mult, ALU.add,
                         [b_xbc[c], b_cacc[a], self.b_vec[l]], [b_cacc[a]])
            self.act(xco[:, c, :], cacc[a], AF.Silu, [b_cacc[a]], [b_xco])
        fm_segment(OX, 12, ev_x)
        self.dma(XCv[:, :, t0:t0 + TS], xco, [b_xco], [self.db(("XC", t))], eng="act")
        if PL <= 5:
            continue

        def ev_dt(bi, sub, p, w_):
            a = sub % 2
            self.tt("dve", dtmp[a], self.ps[p][:, 0:16], dtb, ALU.add, [self.psb[p], b_rows], [b_dtmp[a]])
            self.act(dtmp[a], dtmp[a], AF.Exp, [b_dtmp[a]], [b_dtmp[a]])
            self.act(dtt[:, sub, 0:16], dtmp[a], AF.Ln, [b_dtmp[a]], [b_dtt], bias=1.0, scale=1.0)
            self.tt("pool", dtt[:, sub, 16:32], dtt[:, sub, 0:16], abc, ALU.mult, [b_dtt, b_abc], [b_dtt])
        tm_segment(ODT, 16, ev_dt)
        self.dma(DTv[:, 4 * t:4 * t + 4, :], dtt, [b_dtt], [self.db(("DT", t))], eng="pool")
        if PL <= 6:
            continue

        def ev_u(c, p):
            self.gelu(gu[:, c, :], self.ps[p], [self.psb[p]], [b_gu], 512)
        fm_segment(OU, 8, ev_u)
        if PL <= 7:
            continue

        def ev_vg(bi, sub, p, w_):
            a = sub % 2
            self.gelu(vgf[a][:, bi * 512:(bi + 1) * 512], self.ps[p], [self.psb[p]], [b_vgf[a]], 512)
        wi0 = load_block(OVG, 512)
        wi1 = load_block(OVG + 512, 512)
        gstate = {"i": 0, "wi": None, "og": None, "hold": (wi0, wi1)}

        def emit_gate_chunks(n, t0=t0, t=t):
            for _ in range(n):
                gi_ = gstate["i"]
                if gi_ >= 24:
                    return
                gstate["i"] += 1
                gb, c = gi_ // 8, gi_ % 8
                if gi_ % 4 == 0:
                    gstate["wi"] = load_block(OG + gi_ * 128, 512, avoid=gstate["hold"])
                if c == 0:
                    gstate["og"] = cnt["ob"] % 3
                    cnt["ob"] += 1
                wi, og = gstate["wi"], gstate["og"]
                p = next_ps()
                cc = gi_ % 4
                for kc in range(8):
                    self.mm(self.ps[p], wblk[wi][:, kc, cc * 128:(cc + 1) * 128], xn[:, kc, :], kc == 0, kc == 7,
                            [b_wblk[wi], b_xn[kc]], [self.psb[p]])
                self.act(ob[og][:, c, :], self.ps[p], AF.Sigmoid, [self.psb[p]], [b_ob[og]])
                if c == 7:
                    self.dma(GTv[:, gb * 8:(gb + 1) * 8, t0:t0 + TS], ob[og], [b_ob[og]], [self.db(("GT", t))], eng="act")

        def vg_mm(sub):
            for bi, wi in ((0, wi0), (1, wi1)):
                p = next_ps()
                for kc in range(8):
                    self.mm(self.ps[p], xn[:, kc, sub * 128:(sub + 1) * 128], wblk[wi][:, kc, :], kc == 0, kc == 7,
                            [b_wblk[wi], b_xn[kc]], [self.psb[p]])
                ev_vg(bi, sub, p, 512)

        def ln(sub):
            a = sub % 2
            s_ = st2[a]
            self.sch.op("dve", lambda e, a=a, s_=s_: e.tensor_reduce(out=s_[:, 0:1], in_=vgf[a], axis=AX.X, op=ALU.add),
                        [b_vgf[a]], [b_st[a]])
            self.tsc("dve", s_[:, 1:2], s_[:, 0:1], -1.0 / 1024.0, None, ALU.mult, None, [b_st[a]], [b_st[a]])
            self.act(vgt[a], vgf[a], AF.Square, [b_vgf[a], b_st[a]], [b_vgt[a], b_st[a]], bias=s_[:, 1:2], scale=1.0, accum=s_[:, 2:3])
            self.act(s_[:, 3:4], s_[:, 2:3], AF.Sqrt, [b_st[a], self.b_eps], [b_st[a]], bias=self.eps_col, scale=1.0 / 1024.0)
            self.recip(s_[:, 3:4], s_[:, 3:4], [b_st[a]], [b_st[a]])
            self.tsc("dve", vgt[a], vgf[a], s_[:, 1:2], s_[:, 3:4], ALU.add, ALU.mult, [b_vgf[a], b_st[a]], [b_vgt[a]])
            self.tt("pool", vgt[a], vgt[a], lng, ALU.mult, [b_vgt[a], b_rows], [b_vgt[a]])
            self.tt("pool", vgn[a], vgt[a], lnb, ALU.add, [b_vgt[a], b_rows], [b_vgn[a]])

        def sgu(sub):
            a = sub % 2
            for half in range(2):
                pS = 6
                for gg in range(4):
                    g = half * 4 + gg
                    self.mm(self.ps[pS][:, gg * 128:(gg + 1) * 128], vgn[a][:, g * 128:(g + 1) * 128], wsT[:, g, :], True, True,
                            [b_vgn[a], b_wsT], [self.psb[pS]])
                self.tt("dve", sgt[half], self.ps[pS], sgub[:, half * 512:(half + 1) * 512], ALU.add,
                        [self.psb[pS], b_rows], [b_sgt[half]])
                self.tt("pool", yc[:, half * 4:(half + 1) * 4, sub * 128:(sub + 1) * 128], r3(sgt[half], 4),
                        gu[:, half * 4:(half + 1) * 4, sub * 128:(sub + 1) * 128], ALU.mult, [b_sgt[half], b_gu], [b_yc])

        if PL > 8:
            for sub in range(4):
                vg_mm(sub)
                emit_gate_chunks(3)
                ln(sub)
                emit_gate_chunks(3)
                if sub >= 1:
                    sgu(sub - 1)
            gstate["hold"] = ()
            sgu(3)
            emit_gate_chunks(24)
            self.dma(YCv[:, :, t0:t0 + TS], yc, [b_yc], [self.db(("YC", t))], eng="pool")
    self.dma(self.KM, km, [b_km], [self.db("KM")], eng="pool")


def gelu(self, out, in_, reads, writes, n):
    if not hasattr(self, "_gl"):
        raise RuntimeError("gelu tmp not allocated")
    i = self._gl["i"] % 2
    self._gl["i"] += 1
    x, u, bx, bu = self._gl["x"][i][:, 0:n], self._gl["u"][i][:, 0:n], self._gl["bx"][i], self._gl["bu"][i]
    self.cp("act", x, in_, reads, [bx])
    self.tt("pool", u, x, x, ALU.mult, [bx], [bu])
    self.tsc("pool", u, u, 0.044715, 1.0, ALU.mult, ALU.add, [bu], [bu])
    self.tt("pool", u, u, x, ALU.mult, [bu, bx], [bu])
    self.act(u, u, AF.Sigmoid, [bu], [bu], scale=2.0 * 0.7978845608028654)
    self.tt("dve", out, u, x, ALU.mult, [bu, bx], writes)


def alloc_gelu(self):
    ar = self.ar
    self._gl = {"i": 0, "x": [ar.f32(512) for _ in range(2)], "u": [ar.f32(512) for _ in range(2)],
                "bx": bufs(2, "glx"), "bu": bufs(2, "glu")}


Prog.stage_proj = stage_proj
Prog.gelu = gelu
Prog.alloc_gelu = alloc_gelu


def stage_attn(self, l):
    self.stage_begin()
    ar = self.ar
    S, NB = self.S, self.NB
    NQC = S // 512
    NKT = S // 128
    GW = max(NB, 8)
    kmf = ar.f32(8 * NB)
    b_kmf = Buf("kmf")
    self.dma(kmf, self.KM, [self.db("KM")], [b_kmf])
    kmb = ar.bf16(8 * NB)
    b_kmb = Buf("kmb")
    self.cp("dve", kmb, kmf, [b_kmf], [b_kmb])
    self_f = ar.f32(512)
    selb = ar.bf16(S)
    b_self = Buf("self")
    b_selb = Buf("selb")
    for c0 in range(0, S, 512):
        self.dma(self_f[0:35, :], self.selb[0:35, c0:c0 + 512], [], [b_self])
        self.cp("dve", selb[0:35, c0:c0 + 512], self_f[0:35, :], [b_self], [b_selb])
    negtab = ar.f32(2 * GW)
    b_negtab = Buf("negtab")
    self.memset("pool", negtab[:, 0:GW], 0.0, [b_negtab])
    self.memset("pool", negtab[:, GW:2 * GW], -1e30, [b_negtab])
    CM = [[r3(ar.bf16(512), 4) for _ in range(2)] for _ in range(2)]
    b_cm = Buf("cm")
    for blk in range(2):
        for a in range(2):
            m = CM[blk][a]
            self.memset("pool", m, 0.0, [b_cm])
            for rel in range(2):
                s_ = blk * 2 + rel
                if rel < a:
                    self.memset("pool", m[:, s_, :], NEG, [b_cm])
                elif rel == a:
                    self.cp("pool", m[:, s_, :], self.negm_f, [self.b_cf], [b_cm])
    arow_f = ar.f32(512)
    b_arow = Buf("arowf")
    raug = [ar.bf16(512) for _ in range(2)]
    b_raug = bufs(2, "raug")
    for k in range(2):
        self.memset("pool", raug[k][0:35, :], 0.0, [b_raug[k]])
    irow = ar.bf16(512)
    b_irow = Buf("irow")
    KTh = [ar.bf16(S) for _ in range(2)]
    QTh = [ar.bf16(S) for _ in range(2)]
    Vh = [r3(ar.bf16(S), NKT) for _ in range(2)]
    b_kt = bufs(2, "kth")
    b_qt = bufs(2, "qth")
    b_vh = bufs(2, "vh")
    gm = r3(ar.f32(4 * GW), 4)
    b_gm = Buf("gm")
    top8 = r3(ar.f32(32), 4)
    b_top8 = Buf("top8")
    thr = ar.f32(8)
    b_thr = Buf("thr")
    bbqs = [r3(ar.bf16(4 * 32), 4) for _ in range(2)]
    b_bbqs = bufs(2, "bbq")
    b_psT = Buf("psT")
    b_psG = Buf("psG")
    PT = [ar.bf16(512) for _ in range(4)]
    b_pt = bufs(4, "pt")
    rs = [ar.f32(512) for _ in range(2)]
    b_rs = bufs(2, "rs")
    ya = [ar.bf16(512) for _ in range(2)]
    b_ya = bufs(2, "ya")
    for k in range(2):
        self.memset("pool", bbqs[k], 0.0, [b_bbqs[k]])
    psT_b = self.ps[6].bitcast(BF16)
    slopes = [2.0 ** (-8.0 * (h + 1.0) / 8) for h in range(8)]
    QTd = self.QT.rearrange("(c p) s -> p c s", p=128)
    KTd = self.KT.rearrange("(c p) s -> p c s", p=128)
    Vd = self.V.rearrange("(t p) f -> p t f", p=128)
    YAd = self.YA.rearrange("(c p) s -> p c s", p=128)
    allQ = [self.db(("QT", t)) for t in range(self.NT)]
    allK = [self.db(("KT", t)) for t in range(self.NT)]
    allV = [self.db(("V", t)) for t in range(self.NT)]
    cnt = {"pt": 0, "sc": 0}

    def gate_a(h, c, hb, k):
        pG = 6
        for s_ in range(4):
            qs = c * 512 + s_ * 128
            self.mm(self.ps[pG][:, s_ * GW:s_ * GW + NB], QTh[hb][:, qs:qs + 128], kmb[:, h * NB:(h + 1) * NB], True, True,
                    [b_qt[hb], b_kmb], [b_psG])
        for half in range(2):
            own = 2 * c + half
            self.memset("dve", gm[:, 2 * half:2 * half + 2, :], -1e30, [b_gm]) if GW > NB else None
            self.tt("dve", gm[:, 2 * half:2 * half + 2, 0:NB], r3(self.ps[pG][:, 0:4 * GW], 4)[:, 2 * half:2 * half + 2, 0:NB],
                    negtab[:, None, GW - own:GW - own + NB].to_broadcast([128, 2, NB]), ALU.add,
                    [b_psG, b_negtab], [b_gm])
        for s_ in range(4):
            self.sch.op("dve", lambda e, s_=s_: e.max(out=top8[:, s_, :], in_=gm[:, s_, :]), [b_gm], [b_top8])
        self.tsc("dve", thr[:, 0:4], top8[:, :, 2], -1e29, None, ALU.max, None, [b_top8], [b_thr])
        for s_ in range(4):
            own = 2 * c + s_ // 2
            self.tsc("dve", bbqs[k][:, s_, 0:NB], gm[:, s_, 0:NB], thr[:, s_:s_ + 1], 1.0, ALU.is_ge, ALU.subtract,
                     [b_gm, b_thr], [b_bbqs[k]])
            self.memset("dve", bbqs[k][:, s_, own:own + 1], 0.0, [b_bbqs[k]])

    def gate_b(h, c, hb, k):
        for s_ in range(4):
            self.tr(psT_b[0:32, 512 + s_ * 128:512 + (s_ + 1) * 128], bbqs[k][:, s_, :], self.ident_b, [b_bbqs[k], self.b_cb], [b_psT])
        self.cp("act", raug[k][0:32, :], psT_b[0:32, 512:1024], [b_psT], [b_raug[k]])
        self.tsc("dve", raug[k][32:33, :], irow[32:33, :], -slopes[h] * c * 512.0, None, ALU.add, None,
                 [b_irow], [b_raug[k]])

    for h in range(8):
        hb = h % 2
        self.dma(KTh[hb], KTd[:, h, :], allK, [b_kt[hb]])
        self.dma(QTh[hb], QTd[:, h, :], allQ, [b_qt[hb]])
        self.dma(Vh[hb], Vd[:, :, h * 128:(h + 1) * 128], allV, [b_vh[hb]])
        self.dma(arow_f[32:35, :], self.arow[h], [], [b_arow])
        self.cp("dve", irow[32:33, :], arow_f[32:33, :], [b_arow], [b_irow])
        for k in range(2):
            self.cp("dve", raug[k][32:35, :], arow_f[32:35, :], [b_arow], [b_raug[k]])
        gate_a(h, 0, hb, 0)
        gate_b(h, 0, hb, 0)
        SKEW = 2
        scb = [0, 1, 7]
        for c in range(NQC):
            k = c % 2
            if c + 1 < NQC:
                gate_a(h, c + 1, hb, (c + 1) % 2)
            pO, pS = 2 + k, 4 + k
            kts = [kt for kt in range(4 * c + 4) if slopes[h] * (c * 512 - (kt * 128 + 127)) <= 80.0]
            nkt = len(kts)
            pis = {}
            for step in range(nkt + SKEW):
                if step < nkt:
                    kt = kts[step]
                    n, a = kt // 2, kt % 2
                    pc = scb[cnt["sc"] % 3]
                    cnt["sc"] += 1
                    diag = n >= 2 * c
                    self.mm(self.ps[pc], KTh[hb][:, kt * 128:(kt + 1) * 128], QTh[hb][:, c * 512:(c + 1) * 512], True, False,
                            [b_kt[hb], b_qt[hb]], [self.psb[pc]])
                    self.mm(self.ps[pc], selb[0:35, kt * 128:(kt + 1) * 128], raug[k][0:35, :], False, not diag,
                            [b_selb, b_raug[k]], [self.psb[pc]])
                    if diag:
                        self.mm(self.ps[pc], self.ident_b, CM[n - 2 * c][a].rearrange("p a b -> p (a b)"), False, True,
                                [self.b_cb, b_cm], [self.psb[pc]])
                    pi = cnt["pt"] % 4
                    cnt["pt"] += 1
                    pis[kt] = pi
                    self.act(PT[pi], self.ps[pc], AF.Exp, [self.psb[pc]], [b_pt[pi]])
                if step >= SKEW:
                    j = step - SKEW
                    kt = kts[j]
                    pi = pis[kt]
                    self.mm(self.ps[pO], Vh[hb][:, kt, :], PT[pi], j == 0, j == nkt - 1, [b_vh[hb], b_pt[pi]], [self.psb[pO]])
                    self.mm(self.ps[pS], self.ones_b, PT[pi], j == 0, j == nkt - 1, [self.b_ones, b_pt[pi]], [self.psb[pS]])
            if c + 1 < NQC:
                gate_b(h, c + 1, hb, (c + 1) % 2)
            self.cp("act", rs[k], self.ps[pS], [self.psb[pS]], [b_rs[k]])
            self.recip(rs[k], rs[k], [b_rs[k]], [b_rs[k]])
            self.tt("dve", ya[k], self.ps[pO], rs[k], ALU.mult, [self.psb[pO], b_rs[k]], [b_ya[k]])
            self.dma(YAd[:, h, c * 512:(c + 1) * 512], ya[k], [b_ya[k]], [self.db(("YA", c))], eng="pool")


Prog.stage_attn = stage_attn


def stage_ssd(self, l):
    self.stage_begin()
    ar = self.ar
    S, NT = self.S, self.NT
    self.alloc_norm_tmp()
    selh = ar.f32(2048)
    b_selh = Buf("selh")
    self.dma(selh[0:16, :], self.selh, [], [b_selh])
    xc = [r3(ar.bf16(12 * TS), 12) for _ in range(2)]
    b_xc = bufs(2, "sxc")
    zs = [r3(ar.bf16(8 * TS), 8) for _ in range(2)]
    b_zs = bufs(2, "szs")
    dtt = [r3(ar.f32(4 * 32), 4) for _ in range(2)]
    b_dtt = bufs(2, "sdt")
    yg = r3(ar.f32(8 * TS), 8)
    b_yg = bufs(8, "yg")
    yo = r3(ar.bf16(8 * TS), 8)
    b_yo = Buf("yo")
    Sf = ar.f32(1024)
    Sb = ar.bf16(1024)
    b_Sf, b_Sb = Buf("Sf"), Buf("Sb")
    self.memset("pool", Sf, 0.0, [b_Sf])
    self.memset("pool", Sb, 0.0, [b_Sb])
    xdt = [r3(ar.bf16(1024), 16) for _ in range(2)]
    xw = [r3(ar.bf16(1024), 16) for _ in range(2)]
    b_xdt, b_xw = bufs(2, "xdt"), bufs(2, "xw")
    Btok = [ar.bf16(256) for _ in range(2)]
    b_btok = bufs(2, "btok")
    Asb = [ar.f32(32) for _ in range(2)]
    b_asb = bufs(2, "asb")
    acsT = [ar.f32(128) for _ in range(2)]
    b_acsT = bufs(2, "acsT")
    sm = [ar.f32(64) for _ in range(2)]
    b_sm = bufs(2, "sm")
    GTs = [r3(ar.f32(256), 2) for _ in range(2)]
    b_gts = bufs(2, "gts")
    Et = [r3(ar.f32(512), 4) for _ in range(2)]
    b_et = bufs(2, "et")
    WT = [r3(ar.bf16(512), 4) for _ in range(2)]
    b_wt = bufs(2, "wt")
    EA = [r3(ar.f32(512), 4) for _ in range(2)]
    b_ea = bufs(2, "ea")
    CwT = [r3(ar.bf16(512), 4) for _ in range(2)]
    b_cwt = bufs(2, "cwt")
    XCd = self.XC.rearrange("(c p) s -> p c s", p=128)
    ZSd = self.ZS.rearrange("(c p) s -> p c s", p=128)
    DTd = self.DT.rearrange("(s p) c -> p s c", p=128)
    YSd = self.YS.rearrange("(c p) s -> p c s", p=128)
    psX_b = self.ps[0].bitcast(BF16)
    ps1_b = self.ps[1].bitcast(BF16)
    Sbs = [Sb, ar.bf16(1024), ar.bf16(1024)]
    b_Sbs = [b_Sb, Buf("Sb1"), Buf("Sb2")]
    for i in (1, 2):
        self.memset("pool", Sbs[i], 0.0, [b_Sbs[i]])
    b_c5 = {5: self.psb[5], 7: self.psb[7]}
    b_y6 = bufs(2, "psY")
    cnt = {"hg": 0}

    def loads(t):
        tb = t % 2
        t0 = t * TS
        self.dma(xc[tb], XCd[:, :, t0:t0 + TS], [self.db(("XC", t))], [b_xc[tb]])
        self.dma(zs[tb], ZSd[:, :, t0:t0 + TS], [self.db(("ZS", t))], [b_zs[tb]])
        self.dma(dtt[tb], DTd[:, 4 * t:4 * t + 4, :], [self.db(("DT", t))], [b_dtt[tb]])

    def p1(t, q, ci):
        tb, k = t % 2, ci % 2
        cs = slice(q * 128, (q + 1) * 128)
        dt_ = dtt[tb][:, q, 0:16]
        dta_ = dtt[tb][:, q, 16:32]
        for ch in range(8):
            self.tr(psX_b[:, ch * 128:(ch + 1) * 128], xc[tb][:, ch, cs], self.ident_b, [b_xc[tb], self.b_cb], [self.psb[0]])
        for g in range(2):
            self.tr(ps1_b[:, 384 + g * 128:384 + (g + 1) * 128], xc[tb][:, 8 + g, cs], self.ident_b, [b_xc[tb], self.b_cb],
                    [self.psb[1]])
        self.mm(self.ps[1][:, 0:16], self.U_f, dta_, True, True, [self.b_cf, b_dtt[tb]], [self.psb[1]])
        self.mm(self.ps[1][:, 16:32], self.ones_f, dta_, True, True, [self.b_ones, b_dtt[tb]], [self.psb[1]])
        self.mm(self.ps[1][0:16, 32:160], dta_, self.U_f, True, True, [self.b_cf, b_dtt[tb]], [self.psb[1]])
        self.cp("act", Btok[k], ps1_b[:, 384:640], [self.psb[1]], [b_btok[k]])
        self.cp("act", Asb[k], self.ps[1][:, 0:32], [self.psb[1]], [b_asb[k]])
        self.cp("act", acsT[k][0:16, :], self.ps[1][0:16, 32:160], [self.psb[1]], [b_acsT[k]])
        nacs, dte, cd, tmp16 = sm[k][:, 0:16], sm[k][:, 16:32], sm[k][:, 32:48], sm[k][:, 48:64]
        self.tsc("dve", nacs, Asb[k][:, 0:16], -1.0, None, ALU.mult, None, [b_asb[k]], [b_sm[k]])
        self.tt("dve", tmp16, Asb[k][:, 16:32], Asb[k][:, 0:16], ALU.subtract, [b_asb[k]], [b_sm[k]])
        self.act(tmp16, tmp16, AF.Exp, [b_sm[k]], [b_sm[k]])
        self.tt("dve", dte, tmp16, dt_, ALU.mult, [b_sm[k], b_dtt[tb]], [b_sm[k]])
        self.act(cd, Asb[k][:, 16:32], AF.Exp, [b_asb[k]], [b_sm[k]])
        self.tt("dve", xdt[k], r3(psX_b, 16), dt_[:, :, None].to_broadcast([128, 16, 64]), ALU.mult,
                [self.psb[0], b_dtt[tb]], [b_xdt[k]])
        self.tt("dve", xw[k], r3(psX_b, 16), dte[:, :, None].to_broadcast([128, 16, 64]), ALU.mult,
                [self.psb[0], b_sm[k]], [b_xw[k]])
        for g in range(2):
            self.mm(self.ps[3 + g], Btok[k][:, g * 128:(g + 1) * 128], xw[k][:, g * 8:(g + 1) * 8, :].rearrange("p a b -> p (a b)"),
                    True, True, [b_btok[k], b_xw[k]], [self.psb[3 + g]])
        for g in range(2):
            self.mm(self.ps[2][:, g * 128:(g + 1) * 128], xc[tb][:, 8 + g, cs], xc[tb][:, 10 + g, cs], True, True,
                    [b_xc[tb]], [self.psb[2]])
        self.cp("act", GTs[k].rearrange("p a b -> p (a b)"), self.ps[2][:, 0:256], [self.psb[2]], [b_gts[k]])
        self.tt("dve", r3(Sf, 16), r3(Sf, 16), cd[:, :, None].to_broadcast([128, 16, 64]), ALU.mult, [b_Sf, b_sm[k]], [b_Sf])
        for g in range(2):
            self.tt("dve", Sf[:, g * 512:(g + 1) * 512], Sf[:, g * 512:(g + 1) * 512], self.ps[3 + g], ALU.add,
                    [b_Sf, self.psb[3 + g]], [b_Sf])
        self.cp("act", Sbs[ci % 3], Sf, [b_Sf], [b_Sbs[ci % 3]])

    def p2(t, q, ci):
        tb, k = t % 2, ci % 2
        cs = slice(q * 128, (q + 1) * 128)
        nacs = sm[k][:, 0:16]
        Sprev, b_Sprev = Sbs[(ci - 1) % 3], b_Sbs[(ci - 1) % 3]
        for hg in range(4):
            e = cnt["hg"] % 2
            cnt["hg"] += 1
            g = hg // 2
            pC = 5 if e == 0 else 7
            bC = b_c5[pC]
            for hh in range(4):
                h = hg * 4 + hh
                self.mm(self.ps[pC][:, hh * 128:(hh + 1) * 128], selh[0:16, h * 128:(h + 1) * 128], acsT[k][0:16, :], True, True,
                        [b_selh, b_acsT[k]], [bC])
            self.tt("dve", Et[e], r3(self.ps[pC], 4), self.negm_f[:, None, :].to_broadcast([128, 4, 128]), ALU.add,
                    [bC, self.b_cf], [b_et[e]])
            for hh in range(4):
                h = hg * 4 + hh
                self.act(Et[e][:, hh, :], Et[e][:, hh, :], AF.Exp, [b_et[e], b_sm[k]], [b_et[e]], bias=nacs[:, h:h + 1], scale=1.0)
            self.tt("dve", WT[e], Et[e], GTs[k][:, g:g + 1, :].to_broadcast([128, 4, 128]), ALU.mult,
                    [b_et[e], b_gts[k]], [b_wt[e]])
            self.act(EA[e].rearrange("p a b -> p (a b)"), self.ps[pC], AF.Exp, [bC], [b_ea[e]])
            self.tt("pool", CwT[e], EA[e], xc[tb][:, 10 + g:11 + g, cs].to_broadcast([128, 4, 128]), ALU.mult,
                    [b_ea[e], b_xc[tb]], [b_cwt[e]])
            pY = 6
            yoff = e * 256
            bY = b_y6[e]
            for hh in range(4):
                h = hg * 4 + hh
                po = (h % 2) * 64
                col = yoff + (hh // 2) * 128
                kw = {"tile_position": (0, 64)} if po else {}
                self.mm(self.ps[pY][po:po + 64, col:col + 128], xdt[k][:, h, :], WT[e][:, hh, :], True, False,
                        [b_xdt[k], b_wt[e]], [bY], **kw)
                self.mm(self.ps[pY][po:po + 64, col:col + 128], Sprev[:, h * 64:(h + 1) * 64], CwT[e][:, hh, :], False, True,
                        [b_Sprev, b_cwt[e]], [bY], **kw)
            for cc in range(2):
                cp_ = hg * 2 + cc
                self.stt("dve", yg[:, cp_, cs], xc[tb][:, cp_, cs], self.vcol(l, "dskip", cp_),
                         self.ps[pY][:, yoff + cc * 128:yoff + (cc + 1) * 128], ALU.mult, ALU.add,
                         [b_xc[tb], self.b_vec[l], bY], [b_yg[cp_]])
                self.tt("pool", yg[:, cp_, cs], yg[:, cp_, cs], zs[tb][:, cp_, cs], ALU.mult, [b_yg[cp_], b_zs[tb]], [b_yg[cp_]])
        if q == 3:
            t0 = t * TS
            self.rms_stats(yg, b_yg, self.rstd[tb], self.b_rstd[tb], self.sq, self.b_sq, 2)
            for c in range(8):
                self.stt("dve", yo[:, c, :], yg[:, c, :], self.vcol(l, "g_ssm", c), self.rstd[tb], ALU.mult, ALU.mult,
                         [b_yg[c], self.b_rstd[tb], self.b_vec[l]], [b_yo])
            self.dma(YSd[:, :, t0:t0 + TS], yo, [b_yo], [self.db(("YS", t))], eng="pool")

    chunks = [(t, q) for t in range(NT) for q in range(4)]
    for i, (t, q) in enumerate(chunks):
        if q == 0:
            loads(t)
        p1(t, q, i)
        if i >= 1:
            p2(chunks[i - 1][0], chunks[i - 1][1], i - 1)
    p2(chunks[-1][0], chunks[-1][1], len(chunks) - 1)


Prog.stage_ssd = stage_ssd


def stage_merge(self, l):
    self.stage_begin()
    ar = self.ar
    NT = self.NT
    self.alloc_norm_tmp()
    Wn = ["pa", "pss", "pc", "wo"]
    Wt = []
    b_W = []
    for nm in Wn:
        w = r3(ar.bf16(8 * 1024), 8)
        b = Buf("mw" + nm)
        self.dma(w, self.WB[l][nm].rearrange("(kc p) n -> p kc n", p=128), [self.db(("wb", l, nm))], [b])
        Wt.append(w)
        b_W.append(b)
    yt = [r3(ar.bf16(8 * TS), 8) for _ in range(3)]
    b_yt = bufs(3, "myt")
    gt = r3(ar.bf16(24 * TS), 24)
    b_gt = Buf("mgt")
    hres = r3(ar.f32(8 * TS), 8)
    b_h = bufs(8, "mh")
    f = r3(ar.f32(8 * TS), 8)
    b_f = bufs(8, "mf")
    mg = r3(ar.bf16(8 * TS), 8)
    b_mg = bufs(8, "mmg")
    tA = [ar.f32(TS) for _ in range(2)]
    tB = [ar.f32(TS) for _ in range(2)]
    tC = [ar.f32(TS) for _ in range(2)]
    b_tA, b_tB, b_tC = bufs(2, "tA"), bufs(2, "tB"), bufs(2, "tC")
    srcs = [(self.YA, "YA"), (self.YS, "YS"), (self.YC, "YC")]
    hv = self.hT.rearrange("(c p) s -> p c s", p=128)
    GTv = self.GT.rearrange("(c p) s -> p c s", p=128)
    for t in range(NT):
        t0 = t * TS
        for b in range(3):
            key = (srcs[b][1], t)
            deps = [self.db(key)] if srcs[b][1] != "YA" else [self.db(("YA", t))]
            self.dma(yt[b], srcs[b][0].rearrange("(c p) s -> p c s", p=128)[:, :, t0:t0 + TS], deps, [b_yt[b]])
        self.dma(gt, GTv[:, :, t0:t0 + TS], [self.db(("GT", t))], [b_gt])
        self.dma(hres, hv[:, :, t0:t0 + TS], [self.db(("h", t))], b_h)
        for c in range(8):
            k = c % 2
            for b in range(3):
                p = k * 3 + b
                for kc in range(8):
                    self.mm(self.ps[p], Wt[b][:, kc, c * 128:(c + 1) * 128], yt[b][:, kc, :], kc == 0, kc == 7,
                            [b_W[b], b_yt[b]], [self.psb[p]])
            self.tt("dve", tA[k], self.ps[k * 3 + 0], gt[:, c, :], ALU.mult, [self.psb[k * 3 + 0], b_gt], [b_tA[k]])
            self.tt("dve", tB[k], self.ps[k * 3 + 1], gt[:, 8 + c, :], ALU.mult, [self.psb[k * 3 + 1], b_gt], [b_tB[k]])
            self.tt("dve", tC[k], self.ps[k * 3 + 2], gt[:, 16 + c, :], ALU.mult, [self.psb[k * 3 + 2], b_gt], [b_tC[k]])
            self.tt("pool", tA[k], tA[k], tB[k], ALU.add, [b_tA[k], b_tB[k]], [b_tA[k]])
            self.tt("pool", mg[:, c, :], tA[k], tC[k], ALU.add, [b_tA[k], b_tC[k]], [b_mg[c]])
        for c in range(8):
            p = 6 + c % 2
            for kc in range(8):
                self.mm(self.ps[p], Wt[3][:, kc, c * 128:(c + 1) * 128], mg[:, kc, :], kc == 0, kc == 7,
                        [b_W[3], b_mg[kc]], [self.psb[p]])
            self.cp("act", f[:, c, :], self.ps[p], [self.psb[p]], [b_f[c]])
        self.postnorm_res(l, "g_mpost", f, b_f, hres, b_h, None, t % 2)
        self.dma(hv[:, :, t0:t0 + TS], hres, b_h, [self.db(("h", t))], eng="pool")


Prog.stage_merge = stage_merge
```

```python
import numpy as np
from contextlib import ExitStack
import concourse.bass as bass
import concourse.mybir as mybir
from concourse.bass_utils import run_bass_kernel_spmd

F32 = mybir.dt.float32
BF16 = mybir.dt.bfloat16
AF = mybir.ActivationFunctionType
ALU = mybir.AluOpType
AX = mybir.AxisListType

D = 1024
NL = 2
FH = 2816
NJ = FH // 128
INC = 10768
TS = 512
EPS = 1e-6
NEG = -30000.0
OQ, OK_, OV, OZ, OX, ODT, OU, OVG, OG = 0, 1024, 2048, 3072, 4096, 5632, 5648, 6672, 7696

EPOCH = 30000
DEPOCH = 1800
KSLOT = 8


class Buf:
    __slots__ = ("w", "r", "name")

    def __init__(self, name=""):
        self.w = None
        self.r = {}
        self.name = name


def bufs(n, name=""):
    return [Buf(f"{name}{i}") for i in range(n)]


class _Op:
    __slots__ = ("fn", "kind", "waits", "signal", "sigval", "slot", "use")

    def __init__(self, fn, kind):
        self.fn = fn
        self.kind = kind
        self.waits = []
        self.signal = False
        self.sigval = 0
        self.slot = -1
        self.use = 0


class _Eng:
    def __init__(self, name):
        self.name = name
        self.ops = []
        self.seen = {}
        self.ndma = 0


class Sched:
    ENGS = ("pe", "act", "dve", "pool", "sp")

    def __init__(self, nc):
        self.nc = nc
        self.e = {n: _Eng(n) for n in self.ENGS}

    def op(self, eng, fn, reads=(), writes=(), kind="c"):
        e = self.e[eng]
        o = _Op(fn, kind)
        idx = len(e.ops)
        deps = {}

        def add(ev, hard):
            if ev is None:
                return
            k, v = ev
            if (not hard) and kind == "c" and k == ("c", eng):
                return
            if deps.get(k, -1) < v:
                deps[k] = v

        for b in reads:
            add(b.w, True)
        for b in writes:
            add(b.w, eng != "pe")
            for k, v in b.r.items():
                add((k, v), False)
        if kind == "d":
            d = e.ndma
            e.ndma += 1
            o.slot = d % KSLOT
            o.use = d // KSLOT + 1
            if o.use > 1:
                add((("d", eng, o.slot), o.use - 1), True)
            ev = (("d", eng, o.slot), o.use)
        else:
            ev = (("c", eng), idx)
        for k, v in deps.items():
            if e.seen.get(k, -1) >= v:
                continue
            e.seen[k] = v
            o.waits.append((k, v))
            if k[0] == "c":
                self.e[k[1]].ops[v].signal = True
        for b in reads:
            k, v = ev
            if b.r.get(k, -1) < v:
                b.r[k] = v
        for b in writes:
            b.w = ev
            b.r = {}
        e.ops.append(o)
        return ev

    def barrier(self):
        evs = []
        for n, e in self.e.items():
            for i in range(len(e.ops) - 1, -1, -1):
                if e.ops[i].kind == "c":
                    evs.append((("c", n), i))
                    break
            d = e.ndma
            for s in range(min(KSLOT, d)):
                evs.append((("d", n, s), (d - 1 - s) // KSLOT + 1))
        for n in self.ENGS:
            e = self.e[n]
            o = _Op(None, "n")
            for k, v in evs:
                if k == ("c", n):
                    continue
                if e.seen.get(k, -1) >= v:
                    continue
                e.seen[k] = v
                o.waits.append((k, v))
                if k[0] == "c":
                    self.e[k[1]].ops[v].signal = True
            if o.waits:
                e.ops.append(o)

    def emit(self):
        nc = self.nc
        for e in self.e.values():
            c = 0
            for o in e.ops:
                if o.kind == "c" and o.signal:
                    c += 1
                    o.sigval = c
            e.nsig = c
        with ExitStack() as st:
            sems = {}

            def sem(key):
                if key not in sems:
                    sems[key] = st.enter_context(nc.semaphore("s_" + "_".join(str(x) for x in key)))
                return sems[key]

            for n, e in self.e.items():
                for ep in range((e.nsig + EPOCH - 1) // EPOCH):
                    sem(("c", n, ep))
                if e.ndma:
                    maxuse = (e.ndma - 1) // KSLOT + 1
                    for s in range(min(KSLOT, e.ndma)):
                        for ep in range((maxuse + DEPOCH - 1) // DEPOCH):
                            sem(("d", n, s, ep))

            def resolve(k, v):
                if k[0] == "c":
                    sv = self.e[k[1]].ops[v].sigval
                    assert sv > 0
                    return sem(("c", k[1], (sv - 1) // EPOCH)), (sv - 1) % EPOCH + 1
                return sem(("d", k[1], k[2], (v - 1) // DEPOCH)), 16 * ((v - 1) % DEPOCH + 1)

            block = st.enter_context(nc.Block())
            regs = {"pe": block.tensor, "act": block.scalar, "dve": block.vector,
                    "pool": block.gpsimd, "sp": block.sync}
            for n in self.ENGS:
                e = self.e[n]
                if not e.ops:
                    continue

                def body(eng, e=e, n=n):
                    for o in e.ops:
                        for k, v in o.waits:
                            s, val = resolve(k, v)
                            eng.wait_ge(s, val)
                        if o.fn is None:
                            continue
                        ins = o.fn(eng)
                        if o.kind == "d":
                            ins.then_inc(sem(("d", n, o.slot, (o.use - 1) // DEPOCH)), 16)
                        elif o.signal:
                            ins.then_inc(sem(("c", n, (o.sigval - 1) // EPOCH)), 1)

                regs[n](body)

    def stats(self):
        return {n: (len(e.ops), sum(len(o.waits) for o in e.ops), e.ndma) for n, e in self.e.items()}


class Arena:
    def __init__(self, nc, nfloats):
        self.t = nc.alloc_sbuf_tensor("arena", [128, nfloats], F32)
        self.n = nfloats
        self.p = 0
        self.base = 0

    def f32(self, n):
        n8 = (n + 7) // 8 * 8
        assert self.p + n8 <= self.n, f"arena overflow {self.p}+{n8}>{self.n}"
        a = self.t[:, self.p:self.p + n]
        self.p += n8
        return a

    def bf16(self, n):
        nf = (n + 1) // 2
        n8 = (nf + 7) // 8 * 8
        assert self.p + n8 <= self.n, f"arena overflow {self.p}+{n8}>{self.n}"
        a = self.t[:, self.p:self.p + nf].bitcast(BF16)
        self.p += n8
        return a

    def keep(self):
        self.base = self.p

    def reset(self):
        self.p = self.base


def r3(ap, a):
    return ap.rearrange("p (a b) -> p a b", a=a)


class Prog:
    def __init__(self, S, nl=NL, dbg=(), stop_after=None):
        self.S = S
        self.nl = nl
        self.NT = S // TS
        self.NB = S // 256
        self.dbg = set(dbg)
        self.stop_after = stop_after
        nc = self.nc = bass.Bass("TRN2", target_bir_lowering=False)
        self.sch = Sched(nc)
        self.ar = Arena(nc, 52736)
        self.dram = {}
        self.dbuf = {}
        self.ps = [nc.alloc_psum_tensor(f"ps{i}", [128, 512], F32)[:] for i in range(8)]
        self.psb = bufs(8, "ps")
        self.rr = 0

    def din(self, name, shape, dt=F32):
        t = self.nc.dram_tensor(name, list(shape), dt, kind="ExternalInput").ap()
        self.dram[name] = t
        return t

    def dscr(self, name, shape, dt, out=False):
        kind = "ExternalOutput" if (out or name in self.dbg) else "Internal"
        t = self.nc.dram_tensor(name, list(shape), dt, kind=kind).ap()
        self.dram[name] = t
        return t

    def db(self, key):
        if key not in self.dbuf:
            self.dbuf[key] = Buf(str(key))
        return self.dbuf[key]

    def dma(self, out, in_, reads, writes, eng="sp"):
        self.sch.op(eng, lambda e: e.dma_start(out=out, in_=in_), reads, writes, kind="d")

    def mm(self, out, lhsT, rhs, start, stop, reads, writes, **kw):
        self.sch.op("pe", lambda e: e.matmul(out, lhsT, rhs, start=start, stop=stop, **kw), reads, writes)

    def tr(self, out, in_, ident, reads, writes):
        self.sch.op("pe", lambda e: e.transpose(out, in_, ident), reads, writes)

    def act(self, out, in_, func, reads, writes, bias=None, scale=None, accum=None):
        kw = {}
        if bias is not None:
            kw["bias"] = bias
        if scale is not None:
            kw["scale"] = scale
        if accum is not None:
            kw["accum_out"] = accum
        self.sch.op("act", lambda e: e.activation(out=out, in_=in_, func=func, **kw), reads, writes)

    def tt(self, eng, out, in0, in1, op, reads, writes):
        self.sch.op(eng, lambda e: e.tensor_tensor(out=out, in0=in0, in1=in1, op=op), reads, writes)

    def tsc(self, eng, out, in0, s1, s2, op0, op1, reads, writes):
        if s2 is None:
            self.sch.op(eng, lambda e: e.tensor_scalar(out=out, in0=in0, scalar1=s1, scalar2=None, op0=op0), reads, writes)
        else:
            self.sch.op(eng, lambda e: e.tensor_scalar(out=out, in0=in0, scalar1=s1, scalar2=s2, op0=op0, op1=op1), reads, writes)

    def stt(self, eng, out, in0, scalar, in1, op0, op1, reads, writes):
        self.sch.op(eng, lambda e: e.scalar_tensor_tensor(out=out, in0=in0, scalar=scalar, in1=in1, op0=op0, op1=op1), reads, writes)

    def cp(self, eng, out, in_, reads, writes):
        if eng == "act":
            self.act(out, in_, AF.Copy, reads, writes)
        else:
            self.sch.op(eng, lambda e: e.tensor_copy(out=out, in_=in_), reads, writes)

    def memset(self, eng, ap, val, writes):
        self.sch.op(eng, lambda e: e.memset(ap, val), (), writes)

    def recip(self, out, in_, reads, writes):
        self.sch.op("dve", lambda e: e.reciprocal(out=out, in_=in_), reads, writes)

    def stage_begin(self):
        self.sch.barrier()
        self.ar.reset()

    def declare(self):
        S, nl = self.S, self.nl
        self.xT = self.din("xT", [D, S])
        self.cst = self.din("cst", [128, 512])
        self.selb = self.din("selb", [35, self.S])
        self.arow = self.din("arow", [8, 3, 512])
        self.selh = self.din("selh", [16, 16 * 128])
        self.W = []
        for l in range(nl):
            w = {}
            w["gu1"] = self.din(f"gu1_{l}", [D, 2 * FH])
            w["dn1"] = self.din(f"dn1_{l}", [FH, D])
            w["win"] = self.din(f"win_{l}", [D, INC])
            w["pa"] = self.din(f"pa_{l}", [D, D])
            w["pss"] = self.din(f"pss_{l}", [D, D])
            w["pc"] = self.din(f"pc_{l}", [D, D])
            w["wo"] = self.din(f"wo_{l}", [D, D])
            w["gu2"] = self.din(f"gu2_{l}", [D, 2 * FH])
            w["dn2"] = self.din(f"dn2_{l}", [FH, D])
            w["vec"] = self.din(f"vec_{l}", [128, 128])
            w["rows"] = self.din(f"rows_{l}", [1, 3 * 1024 + 32])
            w["sguwT"] = self.din(f"sguwT_{l}", [128, 8 * 128])
            self.W.append(w)
        self.WB = []
        for l in range(nl):
            wb = {}
            for k, shp in (("gu1", [D, 2 * FH]), ("dn1", [FH, D]), ("win", [D, INC]), ("pa", [D, D]),
                           ("pss", [D, D]), ("pc", [D, D]), ("wo", [D, D]), ("gu2", [D, 2 * FH]), ("dn2", [FH, D])):
                wb[k] = self.dscr(f"wb_{k}_{l}", shp, BF16)
            self.WB.append(wb)
        self.hT = self.dscr("hT", [D, S], F32)
        self.outT = self.dscr("outT", [D, S], F32, out=True)
        self.QT = self.dscr("QT", [D, S], BF16)
        self.KT = self.dscr("KT", [D, S], BF16)
        self.V = self.dscr("V", [S, D], BF16)
        self.ZS = self.dscr("ZS", [D, S], BF16)
        self.XC = self.dscr("XC", [1536, S], BF16)
        self.DT = self.dscr("DT", [S, 32], F32)
        self.GT = self.dscr("GT", [3 * D, S], BF16)
        self.YC = self.dscr("YC", [D, S], BF16)
        self.YA = self.dscr("YA", [D, S], BF16)
        self.YS = self.dscr("YS", [D, S], BF16)
        self.KM = self.dscr("KM", [128, 8 * self.NB], F32)

    def setup_consts(self):
        ar = self.ar
        self.c_f = ar.f32(512)
        self.b_cf = Buf("cst")
        self.dma(self.c_f, self.cst, [], [self.b_cf])
        self.ident_f = self.c_f[:, 0:128]
        self.U_f = self.c_f[:, 128:256]
        self.negm_f = self.c_f[:, 256:384]
        self.c_b = ar.bf16(384)
        self.b_cb = Buf("cstb")
        self.cp("dve", self.c_b, self.c_f[:, 0:384], [self.b_cf], [self.b_cb])
        self.ident_b = self.c_b[:, 0:128]
        self.U_b = self.c_b[:, 128:256]
        self.ones_b = ar.bf16(128)
        self.ones_f = ar.f32(128)
        self.b_ones = Buf("ones")
        self.memset("pool", self.ones_b, 1.0, [self.b_ones])
        self.memset("pool", self.ones_f, 1.0, [self.b_ones])
        self.vec = []
        self.b_vec = []
        for l in range(self.nl):
            v = ar.f32(128)
            b = Buf(f"vec{l}")
            self.dma(v, self.W[l]["vec"], [], [b])
            self.vec.append(v)
            self.b_vec.append(b)
        ar.keep()

    def vcol(self, l, name, c):
        off = {"g_f1pre": 0, "g_f1post": 8, "g_mpre": 16, "g_mpost": 24, "g_f2pre": 32, "g_f2post": 40,
               "g_ssm": 48, "conv_w": 56, "conv_b": 104, "dskip": 116}[name]
        return self.vec[l][:, off + c: off + c + 1]

    def stage_convert(self):
        self.stage_begin()
        ar = self.ar
        NBUF = 4
        CW = 2048
        src = [ar.f32(CW) for _ in range(NBUF)]
        dst = [ar.bf16(CW) for _ in range(NBUF)]
        bs = bufs(NBUF, "cvs")
        bd = bufs(NBUF, "cvd")
        engs = ["act", "dve", "pool"]
        i = 0
        for l in range(self.nl):
            for k, wsrc in self.W[l].items():
                if k in ("vec", "rows", "sguwT"):
                    continue
                wdst = self.WB[l][k]
                K, N = wsrc.shape
                for kc in range(K // 128):
                    for c0 in range(0, N, CW):
                        cw = min(CW, N - c0)
                        import os
                        if i >= int(os.environ.get("CVLIM", "1000000")):
                            continue
                        b = i % NBUF
                        self.dma(src[b][:, 0:cw], wsrc[kc * 128:(kc + 1) * 128, c0:c0 + cw], [], [bs[b]])
                        self.cp(engs[i % 3], dst[b][:, 0:cw], src[b][:, 0:cw], [bs[b]], [bd[b]])
                        self.dma(wdst[kc * 128:(kc + 1) * 128, c0:c0 + cw], dst[b][:, 0:cw], [bd[b]],
                                 [self.db(("wb", l, k))], eng=("pool" if i % 2 == 0 else "act"))
                        i += 1

    def rms_stats(self, src_f, b_src, rstd, b_rstd, sq, b_sq, psi):
        ps, pb = self.ps[psi], self.psb[psi]
        for c in range(8):
            j = c % 2
            self.act(sq[j], src_f[:, c, :], AF.Square, [b_src[c]], [b_sq[j]], scale=1.0 / 32.0)
            self.mm(ps, self.ones_b, sq[j], c == 0, c == 7, [self.b_ones, b_sq[j]], [pb])
        self.act(rstd, ps, AF.Sqrt, [pb, self.b_eps], [b_rstd], bias=self.eps_col, scale=1.0)
        self.recip(rstd, rstd, [b_rstd], [b_rstd])

    def alloc_norm_tmp(self):
        ar = self.ar
        self.sq = [ar.bf16(512) for _ in range(2)]
        self.b_sq = bufs(2, "sq")
        self.rstd = [ar.f32(512) for _ in range(2)]
        self.b_rstd = bufs(2, "rstd")
        self.eps_col = ar.f32(8)[:, 0:1]
        self.b_eps = Buf("eps")
        self.memset("pool", self.eps_col, EPS, [self.b_eps])

    def prenorm(self, l, gname, hres, b_h, xn, b_xn, k):
        self.rms_stats(hres, b_h, self.rstd[k], self.b_rstd[k], self.sq, self.b_sq, 7)
        for c in range(8):
            self.stt("dve", xn[:, c, :], hres[:, c, :], self.vcol(l, gname, c), self.rstd[k],
                     ALU.mult, ALU.mult, [b_h[c], self.b_rstd[k], self.b_vec[l]], [b_xn[c]])

    def postnorm_res(self, l, gname, f, b_f, hres, b_h, coef, k):
        self.rms_stats(f, b_f, self.rstd[k], self.b_rstd[k], self.sq, self.b_sq, 7)
        for c in range(8):
            eng = "pool"
            self.stt("dve", f[:, c, :], f[:, c, :], self.gc[l][gname][:, c:c + 1], self.rstd[k], ALU.mult, ALU.mult,
                     [b_f[c], self.b_rstd[k], self.b_gc], [b_f[c]])
            self.tt(eng, hres[:, c, :], hres[:, c, :], f[:, c, :], ALU.add, [b_f[c], b_h[c]], [b_h[c]])

    def setup_gc(self):
        ar = self.ar
        self.gc = []
        self.b_gc = Buf("gc")
        for l in range(self.nl):
            t = ar.f32(24)
            d = {}
            for i, (nm, coef) in enumerate((("g_f1post", 0.5), ("g_mpost", 1.0), ("g_f2post", 0.5))):
                off = {"g_f1post": 8, "g_mpost": 24, "g_f2post": 40}[nm]
                self.tsc("dve", t[:, i * 8:(i + 1) * 8], self.vec[l][:, off:off + 8], coef, None, ALU.mult, None,
                         [self.b_vec[l]], [self.b_gc])
                d[nm] = t[:, i * 8:(i + 1) * 8]
            self.gc.append(d)
        ar.keep()

    def stage_ffn(self, l, which, src, dst, src_key, dst_key):
        self.stage_begin()
        ar = self.ar
        NT = self.NT
        wgu = self.WB[l]["gu1" if which == 1 else "gu2"]
        wdn = self.WB[l]["dn1" if which == 1 else "dn2"]
        bw_gu = self.db(("wb", l, "gu1" if which == 1 else "gu2"))
        bw_dn = self.db(("wb", l, "dn1" if which == 1 else "dn2"))
        gpre = "g_f1pre" if which == 1 else "g_f2pre"
        gpost = "g_f1post" if which == 1 else "g_f2post"
        self.alloc_norm_tmp()
        hres = [r3(ar.f32(8 * TS), 8) for _ in range(2)]
        b_h = [bufs(8, f"hres{i}_") for i in range(2)]
        xn = r3(ar.bf16(8 * TS), 8)
        b_xn = bufs(8, "xn")
        HT = r3(ar.bf16(NJ * TS), NJ)
        b_H = bufs(NJ, "H")
        f = r3(ar.f32(8 * TS), 8)
        b_f = bufs(8, "f")
        sg = [ar.f32(TS) for _ in range(2)]
        b_sg = bufs(2, "sg")
        JG = 2
        NG = NJ // JG
        wg = [r3(ar.bf16(8 * 2 * JG * 128), 8) for _ in range(3)]
        b_wg = bufs(3, "wg")
        CG = 2
        wd = [r3(ar.bf16(NJ * CG * 128), NJ) for _ in range(2)]
        b_wd = bufs(2, "wd")
        srcv = src.rearrange("(c p) s -> p c s", p=128)
        dstv = dst.rearrange("(c p) s -> p c s", p=128)
        wguv = wgu.rearrange("(kc p) n -> p kc n", p=128)
        wdnv = wdn.rearrange("(j p) n -> p j n", p=128)
        gi = 0
        di = 0
        self.dma(hres[0], srcv[:, :, 0:TS], [self.db((src_key, 0))], b_h[0])
        self.prenorm(l, gpre, hres[0], b_h[0], xn, b_xn, 0)
        for t in range(NT):
            k = t % 2
            t0 = t * TS
            for g in range(NG):
                wb_ = gi % 3
                gi += 1
                j0 = g * JG
                self.dma(wg[wb_][:, :, 0:JG * 128], wguv[:, :, j0 * 128:(j0 + JG) * 128], [bw_gu], [b_wg[wb_]])
                self.dma(wg[wb_][:, :, JG * 128:2 * JG * 128], wguv[:, :, FH + j0 * 128:FH + (j0 + JG) * 128], [bw_gu], [b_wg[wb_]])
                for jj in range(JG):
                    j = j0 + jj
                    pg = (2 * (j % 2))
                    pu = pg + 1
                    for kc in range(8):
                        self.mm(self.ps[pg], wg[wb_][:, kc, jj * 128:(jj + 1) * 128], xn[:, kc, :], kc == 0, kc == 7,
                                [b_wg[wb_], b_xn[kc]], [self.psb[pg]])
                    for kc in range(8):
                        self.mm(self.ps[pu], wg[wb_][:, kc, JG * 128 + jj * 128:JG * 128 + (jj + 1) * 128], xn[:, kc, :],
                                kc == 0, kc == 7, [b_wg[wb_], b_xn[kc]], [self.psb[pu]])
                    s = j % 2
                    self.act(sg[s], self.ps[pg], AF.Silu, [self.psb[pg]], [b_sg[s]])
                    self.tt("dve", HT[:, j, :], sg[s], self.ps[pu], ALU.mult, [b_sg[s], self.psb[pu]], [b_H[j]])
            if t + 1 < NT:
                self.dma(hres[1 - k], srcv[:, :, t0 + TS:t0 + 2 * TS], [self.db((src_key, t + 1))], b_h[1 - k])
                self.prenorm(l, gpre, hres[1 - k], b_h[1 - k], xn, b_xn, 1 - k)
            for cg in range(8 // CG):
                wb_ = di % 2
                di += 1
                self.dma(wd[wb_], wdnv[:, :, cg * CG * 128:(cg + 1) * CG * 128], [bw_dn], [b_wd[wb_]])
                for cc in range(CG):
                    c = cg * CG + cc
                    pd = 4 + (c % 2)
                    for j in range(NJ):
                        self.mm(self.ps[pd], wd[wb_][:, j, cc * 128:(cc + 1) * 128], HT[:, j, :], j == 0, j == NJ - 1,
                                [b_wd[wb_], b_H[j]], [self.psb[pd]])
                    self.cp("act", f[:, c, :], self.ps[pd], [self.psb[pd]], [b_f[c]])
            self.postnorm_res(l, gpost, f, b_f, hres[k], b_h[k], None, k)
            self.dma(dstv[:, :, t0:t0 + TS], hres[k], b_h[k], [self.db((dst_key, t))], eng="pool")

    def build(self):
        self.declare()
        self.setup_consts()
        self.setup_gc()
        self.stage_convert()
        stages = []
        for l in range(self.nl):
            src = self.xT if l == 0 else self.hT
            stages.append(("ffn1", lambda l=l, src=src: self.stage_ffn(l, 1, src, self.hT, "x" if l == 0 else "h", "h")))
            stages.append(("proj", lambda l=l: self.stage_proj(l)))
            stages.append(("attn", lambda l=l: self.stage_attn(l)))
            stages.append(("ssd", lambda l=l: self.stage_ssd(l)))
            stages.append(("merge", lambda l=l: self.stage_merge(l)))
            last = (l == self.nl - 1)
            stages.append(("ffn2", lambda l=l, last=last: self.stage_ffn(l, 2, self.hT, self.outT if last else self.hT, "h",
                                                                          "o" if last else "h")))
        for i, (nm, fn) in enumerate(stages):
            if self.stop_after is not None and i > self.stop_after:
                break
            fn()
        self.sch.barrier()
        self.sch.emit()
        return self.nc


def host_consts(S):
    NB = S // 256
    cst = np.zeros((128, 512), np.float32)
    cst[:, 0:128] = np.eye(128, dtype=np.float32)
    t = np.arange(128)
    cst[:, 128:256] = (t[:, None] <= t[None, :]).astype(np.float32)
    cst[:, 256:384] = np.where(t[:, None] <= t[None, :], 0.0, NEG)
    cst[:, 384:512] = t[None, :].astype(np.float32)
    NKT = S // 128
    selb = np.zeros((35, NKT * 128), np.float32)
    for kt in range(NKT):
        selb[kt // 2, kt * 128:(kt + 1) * 128] = -NEG
        selb[34, kt * 128:(kt + 1) * 128] = kt
    selb[32, :] = 1.0
    selb[33, :] = np.tile(t, NKT).astype(np.float32)
    slopes = 2.0 ** (-8.0 * (np.arange(8) + 1.0) / 8)
    arow = np.zeros((8, 3, 512), np.float32)
    for h in range(8):
        arow[h, 0, :] = -slopes[h] * np.arange(512)
        arow[h, 1, :] = slopes[h]
        arow[h, 2, :] = slopes[h] * 128.0
    selh = np.zeros((16, 16 * 128), np.float32)
    for h in range(16):
        selh[h, h * 128:(h + 1) * 128] = 1.0
    return {"cst": cst, "selb": selb, "arow": arow, "selh": selh}


def host_layer_inputs(inp, l):
    def colv(v):
        return np.ascontiguousarray(v.reshape(-1, 128).T)
    vec = np.zeros((128, 128), np.float32)
    for i, nm in enumerate(("ffn1_pre_g", "ffn1_post_g", "mix_pre_g", "mix_post_g", "ffn2_pre_g", "ffn2_post_g")):
        vec[:, i * 8:(i + 1) * 8] = colv(inp[nm][l])
    vec[:, 48:56] = colv(inp["ssm_norm_g"][l])
    cw = inp["conv_w"][l]
    vec[:, 56:104] = cw.T.reshape(12, 128, 4).transpose(1, 0, 2).reshape(128, 48)
    vec[:, 104:116] = colv(inp["conv_b"][l])
    vec[:, 116:124] = colv(np.repeat(inp["d_skip"][l], 64))
    rows = np.concatenate([inp["sgu_ln_g"][l], inp["sgu_ln_b"][l], inp["sgu_b"][l].reshape(-1),
                           inp["dt_bias"][l], inp["a_log"][l]])[None, :].astype(np.float32)
    sguwT = np.ascontiguousarray(inp["sgu_w"][l].transpose(2, 0, 1).reshape(128, 1024))
    d = {
        f"gu1_{l}": inp["ffn1_w_gu"][l], f"dn1_{l}": inp["ffn1_w_down"][l], f"win_{l}": inp["w_in"][l],
        f"pa_{l}": inp["p_attn"][l], f"pss_{l}": inp["p_ssm"][l], f"pc_{l}": inp["p_sgu"][l], f"wo_{l}": inp["w_out"][l],
        f"gu2_{l}": inp["ffn2_w_gu"][l], f"dn2_{l}": inp["ffn2_w_down"][l],
        f"vec_{l}": vec, f"rows_{l}": rows, f"sguwT_{l}": sguwT,
    }
    return {k: np.ascontiguousarray(v, dtype=np.float32) for k, v in d.items()}


_CACHE = {}


def kernel(**inputs):
    inp = {k: np.asarray(v) for k, v in inputs.items()}
    x = inp["x"]
    B, S, _ = x.shape
    key = (S,)
    if key not in _CACHE:
        _CACHE[key] = Prog(S).build()
    nc = _CACHE[key]
    shared = host_consts(S)
    for l in range(NL):
        shared.update(host_layer_inputs(inp, l))
    workers = [0, 1, 4, 5]
    big = [k for k, v in shared.items() if v.size >= (1 << 20)]
    zeros = dict(shared)
    for k in big:
        zeros[k] = np.zeros_like(shared[k])
    xz = np.zeros((D, S), np.float32)
    in_maps = []
    for c in range(8):
        if c in workers:
            m = dict(shared)
            m["xT"] = np.ascontiguousarray(x[workers.index(c)].T)
        else:
            m = dict(zeros)
            m["xT"] = xz
        in_maps.append(m)
    res = run_bass_kernel_spmd(nc, in_maps, core_ids=list(range(8)))
    out = np.stack([np.ascontiguousarray(np.asarray(res.results[workers[b]]["outT"]).T) for b in range(B)], axis=0)
    return out.astype(np.float32)


def stage_proj(self, l):
    self.stage_begin()
    ar = self.ar
    NT, NB, S = self.NT, self.NB, self.S
    winv = self.WB[l]["win"].rearrange("(kc p) n -> p kc n", p=128)
    bw = self.db(("wb", l, "win"))
    self.alloc_norm_tmp()
    self.alloc_gelu()
    hres = r3(ar.f32(8 * TS), 8)
    b_h = bufs(8, "ph")
    xn = r3(ar.bf16(8 * TS), 8)
    b_xn = bufs(8, "pxn")
    NWB = 4
    wblk = [r3(ar.bf16(8 * 512), 8) for _ in range(NWB)]
    b_wblk = bufs(NWB, "wblk")
    ob = [r3(ar.bf16(8 * TS), 8) for _ in range(3)]
    b_ob = bufs(3, "ob")
    gu = r3(ar.bf16(8 * TS), 8)
    b_gu = Buf("gu")
    xbc = r3(ar.f32(12 * (TS + 8)), 12)
    b_xbc = bufs(12, "xbc")
    cacc = [ar.f32(TS) for _ in range(2)]
    b_cacc = bufs(2, "cacc")
    xco = r3(ar.bf16(12 * TS), 12)
    b_xco = Buf("xco")
    vt = r3(ar.bf16(4 * 1024), 4)
    b_vt = Buf("vt")
    vgf = [ar.f32(1024) for _ in range(2)]
    b_vgf = bufs(2, "vgf")
    vgt = [ar.f32(1024) for _ in range(2)]
    b_vgt = bufs(2, "vgt")
    vgn = [ar.bf16(1024) for _ in range(2)]
    b_vgn = bufs(2, "vgn")
    rows = ar.f32(3 * 1024 + 32)
    b_rows = Buf("rows")
    self.dma(rows, self.W[l]["rows"].to_broadcast([128, 3 * 1024 + 32]), [], [b_rows])
    lng, lnb, sgub = rows[:, 0:1024], rows[:, 1024:2048], rows[:, 2048:3072]
    dtb, alog = rows[:, 3072:3088], rows[:, 3088:3104]
    abc = ar.f32(16)
    b_abc = Buf("abc")
    self.act(abc, alog, AF.Exp, [b_rows], [b_abc])
    self.tsc("dve", abc, abc, -1.0, None, ALU.mult, None, [b_abc], [b_abc])
    sg_all = ar.f32(1024)
    sgt = [sg_all[:, 0:512], sg_all[:, 512:1024]]
    b_sgt = bufs(2, "sgt")
    self.dma(sg_all, self.W[l]["sguwT"], [], b_sgt)
    wsT = r3(ar.bf16(1024), 8)
    b_wsT = Buf("wsT")
    self.tt("dve", wsT, r3(sg_all, 8), self.U_f[:, None, :].to_broadcast([128, 8, 128]), ALU.mult, b_sgt + [self.b_cf], [b_wsT])
    st = ar.f32(16)
    b_st = bufs(2, "st")
    st2 = [ar.f32(8) for _ in range(2)]
    yc = r3(ar.bf16(8 * TS), 8)
    b_yc = Buf("yc")
    dtt = r3(ar.f32(4 * 32), 4)
    b_dtt = Buf("dtt")
    dtmp = [ar.f32(16) for _ in range(2)]
    b_dtmp = bufs(2, "dtmp")
    km = ar.f32(8 * NB)
    b_km = Buf("km")

    hv = self.hT.rearrange("(c p) s -> p c s", p=128)
    QTv = self.QT.rearrange("(c p) s -> p c s", p=128)
    KTv = self.KT.rearrange("(c p) s -> p c s", p=128)
    ZSv = self.ZS.rearrange("(c p) s -> p c s", p=128)
    XCv = self.XC.rearrange("(c p) s -> p c s", p=128)
    GTv = self.GT.rearrange("(c p) s -> p c s", p=128)
    YCv = self.YC.rearrange("(c p) s -> p c s", p=128)
    Vv = self.V.rearrange("(s p) f -> p s f", p=128)
    DTv = self.DT.rearrange("(s p) c -> p s c", p=128)

    cnt = {"w": 0, "ps": 0, "ob": 0}
    import os
    PL = int(os.environ.get("PROJ_LIM", "99"))

    def load_block(c0, ncols, avoid=()):
        i = cnt["w"] % NWB
        cnt["w"] += 1
        while i in avoid:
            i = cnt["w"] % NWB
            cnt["w"] += 1
        self.dma(wblk[i][:, :, 0:ncols], winv[:, :, c0:c0 + ncols], [bw], [b_wblk[i]])
        return i

    def next_ps():
        i = cnt["ps"] % 6
        cnt["ps"] += 1
        return i

    def fm_segment(c0, nchunks, evac):
        for b0 in range(0, nchunks, 4):
            nb_ = min(4, nchunks - b0)
            wi = load_block(c0 + b0 * 128, nb_ * 128)
            for cc in range(nb_):
                p = next_ps()
                for kc in range(8):
                    self.mm(self.ps[p], wblk[wi][:, kc, cc * 128:(cc + 1) * 128], xn[:, kc, :], kc == 0, kc == 7,
                            [b_wblk[wi], b_xn[kc]], [self.psb[p]])
                evac(b0 + cc, p)

    def tm_segment(c0, ncols, evac):
        for bi, b0 in enumerate(range(0, ncols, 512)):
            w_ = min(512, ncols - b0)
            wi = load_block(c0 + b0, w_)
            for sub in range(4):
                p = next_ps()
                for kc in range(8):
                    self.mm(self.ps[p][:, 0:w_], xn[:, kc, sub * 128:(sub + 1) * 128], wblk[wi][:, kc, 0:w_], kc == 0, kc == 7,
                            [b_wblk[wi], b_xn[kc]], [self.psb[p]])
                evac(bi, sub, p, w_)

    for t in range(NT):
        t0 = t * TS
        self.dma(hres, hv[:, :, t0:t0 + TS], [self.db(("h", t))], b_h)
        self.prenorm(l, "g_mpre", hres, b_h, xn, b_xn, t % 2)
        if t == 0:
            self.memset("pool", xbc[:, :, 0:3], 0.0, b_xbc)
        else:
            self.cp("pool", xbc[:, :, 0:3], xbc[:, :, TS:TS + 3], b_xbc, b_xbc)

        if PL <= 0:
            continue
        oq = cnt["ob"] % 3
        cnt["ob"] += 1

        def ev_q(c, p, oq=oq):
            self.act(ob[oq][:, c, :], self.ps[p], AF.Copy, [self.psb[p]], [b_ob[oq]], scale=128.0 ** -0.5)
        fm_segment(OQ, 8, ev_q)
        self.dma(QTv[:, :, t0:t0 + TS], ob[oq], [b_ob[oq]], [self.db(("QT", t))], eng="act")
        if PL <= 1:
            continue
        okk = cnt["ob"] % 3
        cnt["ob"] += 1

        def ev_k(c, p, okk=okk, t=t):
            for hh in range(2):
                col = c * NB + 2 * t + hh
                self.act(ob[okk][:, c, hh * 256:(hh + 1) * 256], self.ps[p][:, hh * 256:(hh + 1) * 256], AF.Copy,
                         [self.psb[p]], [b_ob[okk], b_km], accum=km[:, col:col + 1])
        fm_segment(OK_, 8, ev_k)
        self.dma(KTv[:, :, t0:t0 + TS], ob[okk], [b_ob[okk]], [self.db(("KT", t))], eng="act")
        if PL <= 2:
            continue

        def ev_v(bi, sub, p, w_):
            self.act(vt[:, sub, bi * 512:(bi + 1) * 512], self.ps[p], AF.Copy, [self.psb[p]], [b_vt])
        tm_segment(OV, 1024, ev_v)
        self.dma(Vv[:, 4 * t:4 * t + 4, :], vt, [b_vt], [self.db(("V", t))], eng="act")
        if PL <= 3:
            continue
        oz = cnt["ob"] % 3
        cnt["ob"] += 1

        def ev_z(c, p, oz=oz):
            self.act(ob[oz][:, c, :], self.ps[p], AF.Silu, [self.psb[p]], [b_ob[oz]])
        fm_segment(OZ, 8, ev_z)
        self.dma(ZSv[:, :, t0:t0 + TS], ob[oz], [b_ob[oz]], [self.db(("ZS", t))], eng="act")
        if PL <= 4:
            continue

        def ev_x(c, p):
            self.cp("dve", xbc[:, c, 3:3 + TS], self.ps[p], [self.psb[p]], [b_xbc[c]])
            a = c % 2
            self.tsc("dve", cacc[a], xbc[:, c, 3:3 + TS], self.vcol(l, "conv_w", c * 4 + 3), self.vcol(l, "conv_b", c),
                     ALU.mult, ALU.add, [b_xbc[c], self.b_vec[l]], [b_cacc[a]])
            for k in range(3):
                self.stt("dve", cacc[a], xbc[:, c, k:k + TS], self.vcol(l, "conv_w", c * 4 + k), cacc[a], ALU.mult, ALU.add,
                         [b_xbc[c], b_cacc[a], self.b_vec[l]], [b_cacc[a]])
            self.act(xco[:, c, :], cacc[a], AF.Silu, [b_cacc[a]], [b_xco])
        fm_segment(OX, 12, ev_x)
        self.dma(XCv[:, :, t0:t0 + TS], xco, [b_xco], [self.db(("XC", t))], eng="act")
        if PL <= 5:
            continue

        def ev_dt(bi, sub, p, w_):
            a = sub % 2
            self.tt("dve", dtmp[a], self.ps[p][:, 0:16], dtb, ALU.add, [self.psb[p], b_rows], [b_dtmp[a]])
            self.act(dtmp[a], dtmp[a], AF.Exp, [b_dtmp[a]], [b_dtmp[a]])
            self.act(dtt[:, sub, 0:16], dtmp[a], AF.Ln, [b_dtmp[a]], [b_dtt], bias=1.0, scale=1.0)
            self.tt("pool", dtt[:, sub, 16:32], dtt[:, sub, 0:16], abc, ALU.mult, [b_dtt, b_abc], [b_dtt])
        tm_segment(ODT, 16, ev_dt)
        self.dma(DTv[:, 4 * t:4 * t + 4, :], dtt, [b_dtt], [self.db(("DT", t))], eng="pool")
        if PL <= 6:
            continue

        def ev_u(c, p):
            self.gelu(gu[:, c, :], self.ps[p], [self.psb[p]], [b_gu], 512)
        fm_segment(OU, 8, ev_u)
        self.gelu_flush()
        if PL <= 7:
            continue

        def ev_vg(bi, sub, p, w_):
            a = sub % 2
            self.gelu(vgf[a][:, bi * 512:(bi + 1) * 512], self.ps[p], [self.psb[p]], [b_vgf[a]], 512)
        wi0 = load_block(OVG, 512)
        wi1 = load_block(OVG + 512, 512)
        gstate = {"i": 0, "wi": None, "og": None, "hold": (wi0, wi1)}

        def emit_gate_chunks(n, t0=t0, t=t):
            for _ in range(n):
                gi_ = gstate["i"]
                if gi_ >= 24:
                    return
                gstate["i"] += 1
                gb, c = gi_ // 8, gi_ % 8
                if gi_ % 4 == 0:
                    gstate["wi"] = load_block(OG + gi_ * 128, 512, avoid=gstate["hold"])
                if c == 0:
                    gstate["og"] = cnt["ob"] % 3
                    cnt["ob"] += 1
                wi, og = gstate["wi"], gstate["og"]
                p = next_ps()
                cc = gi_ % 4
                for kc in range(8):
                    self.mm(self.ps[p], wblk[wi][:, kc, cc * 128:(cc + 1) * 128], xn[:, kc, :], kc == 0, kc == 7,
                            [b_wblk[wi], b_xn[kc]], [self.psb[p]])
                self.act(ob[og][:, c, :], self.ps[p], AF.Sigmoid, [self.psb[p]], [b_ob[og]])
                if c == 7:
                    self.dma(GTv[:, gb * 8:(gb + 1) * 8, t0:t0 + TS], ob[og], [b_ob[og]], [self.db(("GT", t))], eng="act")

        def vg_mm(sub):
            for bi, wi in ((0, wi0), (1, wi1)):
                p = next_ps()
                for kc in range(8):
                    self.mm(self.ps[p], xn[:, kc, sub * 128:(sub + 1) * 128], wblk[wi][:, kc, :], kc == 0, kc == 7,
                            [b_wblk[wi], b_xn[kc]], [self.psb[p]])
                ev_vg(bi, sub, p, 512)

        def ln(sub):
            self.gelu_flush()
            a = sub % 2
            s_ = st2[a]
            self.sch.op("dve", lambda e, a=a, s_=s_: e.tensor_reduce(out=s_[:, 0:1], in_=vgf[a], axis=AX.X, op=ALU.add),
                        [b_vgf[a]], [b_st[a]])
            self.tsc("dve", s_[:, 1:2], s_[:, 0:1], -1.0 / 1024.0, None, ALU.mult, None, [b_st[a]], [b_st[a]])
            self.act(vgt[a], vgf[a], AF.Square, [b_vgf[a], b_st[a]], [b_vgt[a], b_st[a]], bias=s_[:, 1:2], scale=1.0, accum=s_[:, 2:3])
            self.act(s_[:, 3:4], s_[:, 2:3], AF.Sqrt, [b_st[a], self.b_eps], [b_st[a]], bias=self.eps_col, scale=1.0 / 1024.0)
            self.recip(s_[:, 3:4], s_[:, 3:4], [b_st[a]], [b_st[a]])
            self.tsc("dve", vgt[a], vgf[a], s_[:, 1:2], s_[:, 3:4], ALU.add, ALU.mult, [b_vgf[a], b_st[a]], [b_vgt[a]])
            self.tt("pool", vgt[a], vgt[a], lng, ALU.mult, [b_vgt[a], b_rows], [b_vgt[a]])
            self.tt("pool", vgn[a], vgt[a], lnb, ALU.add, [b_vgt[a], b_rows], [b_vgn[a]])

        def sgu(sub):
            a = sub % 2
            for half in range(2):
                pS = 6
                for gg in range(4):
                    g = half * 4 + gg
                    self.mm(self.ps[pS][:, gg * 128:(gg + 1) * 128], vgn[a][:, g * 128:(g + 1) * 128], wsT[:, g, :], True, True,
                            [b_vgn[a], b_wsT], [self.psb[pS]])
                self.tt("dve", sgt[half], self.ps[pS], sgub[:, half * 512:(half + 1) * 512], ALU.add,
                        [self.psb[pS], b_rows], [b_sgt[half]])
                self.tt("pool", yc[:, half * 4:(half + 1) * 4, sub * 128:(sub + 1) * 128], r3(sgt[half], 4),
                        gu[:, half * 4:(half + 1) * 4, sub * 128:(sub + 1) * 128], ALU.mult, [b_sgt[half], b_gu], [b_yc])

        if PL > 8:
            for sub in range(4):
                vg_mm(sub)
                emit_gate_chunks(3)
                ln(sub)
                emit_gate_chunks(3)
                if sub >= 1:
                    sgu(sub - 1)
            gstate["hold"] = ()
            sgu(3)
            emit_gate_chunks(24)
            self.dma(YCv[:, :, t0:t0 + TS], yc, [b_yc], [self.db(("YC", t))], eng="pool")
    self.dma(self.KM, km, [b_km], [self.db("KM")], eng="pool")


def gelu(self, out, in_, reads, writes, n):
    i = self._gl["i"] % 2
    self._gl["i"] += 1
    x, u, bx, bu = self._gl["x"][i][:, 0:n], self._gl["u"][i][:, 0:n], self._gl["bx"][i], self._gl["bu"][i]
    self.cp("act", x, in_, reads, [bx])
    self.tt("pool", u, x, x, ALU.mult, [bx], [bu])
    self.tsc("pool", u, u, 0.044715, 1.0, ALU.mult, ALU.add, [bu], [bu])
    self.tt("pool", u, u, x, ALU.mult, [bu, bx], [bu])
    self.gelu_flush()

    def fin():
        self.act(u, u, AF.Sigmoid, [bu], [bu], scale=2.0 * 0.7978845608028654)
        self.tt("dve", out, u, x, ALU.mult, [bu, bx], writes)
    self._gl["pend"] = fin


def gelu_flush(self):
    f = self._gl.get("pend")
    if f is not None:
        self._gl["pend"] = None
        f()


def alloc_gelu(self):
    ar = self.ar
    self._gl = {"i": 0, "x": [ar.f32(512) for _ in range(2)], "u": [ar.f32(512) for _ in range(2)],
                "bx": bufs(2, "glx"), "bu": bufs(2, "glu")}


Prog.stage_proj = stage_proj
Prog.gelu = gelu
Prog.gelu_flush = gelu_flush
Prog.alloc_gelu = alloc_gelu


def stage_attn(self, l):
    self.stage_begin()
    ar = self.ar
    S, NB = self.S, self.NB
    NQC = S // 512
    NKT = S // 128
    GW = max(NB, 8)
    kmf = ar.f32(8 * NB)
    b_kmf = Buf("kmf")
    self.dma(kmf, self.KM, [self.db("KM")], [b_kmf])
    kmb = ar.bf16(8 * NB)
    b_kmb = Buf("kmb")
    self.cp("dve", kmb, kmf, [b_kmf], [b_kmb])
    self_f = ar.f32(512)
    selb = ar.bf16(S)
    b_self = Buf("self")
    b_selb = Buf("selb")
    for c0 in range(0, S, 512):
        self.dma(self_f[0:35, :], self.selb[0:35, c0:c0 + 512], [], [b_self])
        self.cp("dve", selb[0:35, c0:c0 + 512], self_f[0:35, :], [b_self], [b_selb])
    negtab = ar.f32(2 * GW)
    b_negtab = Buf("negtab")
    self.memset("pool", negtab[:, 0:GW], 0.0, [b_negtab])
    self.memset("pool", negtab[:, GW:2 * GW], -1e30, [b_negtab])
    CM = [[r3(ar.bf16(512), 4) for _ in range(2)] for _ in range(2)]
    b_cm = Buf("cm")
    for blk in range(2):
        for a in range(2):
            m = CM[blk][a]
            self.memset("pool", m, 0.0, [b_cm])
            for rel in range(2):
                s_ = blk * 2 + rel
                if rel < a:
                    self.memset("pool", m[:, s_, :], NEG, [b_cm])
                elif rel == a:
                    self.cp("pool", m[:, s_, :], self.negm_f, [self.b_cf], [b_cm])
    arow_f = ar.f32(512)
    b_arow = Buf("arowf")
    raug = [ar.bf16(512) for _ in range(2)]
    b_raug = bufs(2, "raug")
    for k in range(2):
        self.memset("pool", raug[k][0:35, :], 0.0, [b_raug[k]])
    irow = ar.bf16(512)
    b_irow = Buf("irow")
    KTh = [ar.bf16(S) for _ in range(2)]
    QTh = [ar.bf16(S) for _ in range(2)]
    Vh = [r3(ar.bf16(S), NKT) for _ in range(2)]
    b_kt = bufs(2, "kth")
    b_qt = bufs(2, "qth")
    b_vh = bufs(2, "vh")
    gm = r3(ar.f32(4 * GW), 4)
    b_gm = Buf("gm")
    top8 = r3(ar.f32(32), 4)
    b_top8 = Buf("top8")
    thr = ar.f32(8)
    b_thr = Buf("thr")
    bbqs = [r3(ar.bf16(4 * 32), 4) for _ in range(2)]
    b_bbqs = bufs(2, "bbq")
    b_psT = Buf("psT")
    b_psG = Buf("psG")
    PT = [ar.bf16(512) for _ in range(4)]
    b_pt = bufs(4, "pt")
    rs = [ar.f32(512) for _ in range(2)]
    b_rs = bufs(2, "rs")
    ya = [ar.bf16(512) for _ in range(2)]
    b_ya = bufs(2, "ya")
    for k in range(2):
        self.memset("pool", bbqs[k], 0.0, [b_bbqs[k]])
    psT_b = self.ps[6].bitcast(BF16)
    slopes = [2.0 ** (-8.0 * (h + 1.0) / 8) for h in range(8)]
    QTd = self.QT.rearrange("(c p) s -> p c s", p=128)
    KTd = self.KT.rearrange("(c p) s -> p c s", p=128)
    Vd = self.V.rearrange("(t p) f -> p t f", p=128)
    YAd = self.YA.rearrange("(c p) s -> p c s", p=128)
    allQ = [self.db(("QT", t)) for t in range(self.NT)]
    allK = [self.db(("KT", t)) for t in range(self.NT)]
    allV = [self.db(("V", t)) for t in range(self.NT)]
    cnt = {"pt": 0, "sc": 0}

    def gate_a(h, c, hb, k):
        pG = 6
        for s_ in range(4):
            qs = c * 512 + s_ * 128
            self.mm(self.ps[pG][:, s_ * GW:s_ * GW + NB], QTh[hb][:, qs:qs + 128], kmb[:, h * NB:(h + 1) * NB], True, True,
                    [b_qt[hb], b_kmb], [b_psG])
        for half in range(2):
            own = 2 * c + half
            self.memset("dve", gm[:, 2 * half:2 * half + 2, :], -1e30, [b_gm]) if GW > NB else None
            self.tt("dve", gm[:, 2 * half:2 * half + 2, 0:NB], r3(self.ps[pG][:, 0:4 * GW], 4)[:, 2 * half:2 * half + 2, 0:NB],
                    negtab[:, None, GW - own:GW - own + NB].to_broadcast([128, 2, NB]), ALU.add,
                    [b_psG, b_negtab], [b_gm])
        for s_ in range(4):
            self.sch.op("dve", lambda e, s_=s_: e.max(out=top8[:, s_, :], in_=gm[:, s_, :]), [b_gm], [b_top8])
        self.tsc("dve", thr[:, 0:4], top8[:, :, 2], -1e29, None, ALU.max, None, [b_top8], [b_thr])
        for s_ in range(4):
            own = 2 * c + s_ // 2
            self.tsc("dve", bbqs[k][:, s_, 0:NB], gm[:, s_, 0:NB], thr[:, s_:s_ + 1], 1.0, ALU.is_ge, ALU.subtract,
                     [b_gm, b_thr], [b_bbqs[k]])
            self.memset("dve", bbqs[k][:, s_, own:own + 1], 0.0, [b_bbqs[k]])

    def gate_b(h, c, hb, k):
        for s_ in range(4):
            self.tr(psT_b[0:32, 512 + s_ * 128:512 + (s_ + 1) * 128], bbqs[k][:, s_, :], self.ident_b, [b_bbqs[k], self.b_cb], [b_psT])
        self.cp("act", raug[k][0:32, :], psT_b[0:32, 512:1024], [b_psT], [b_raug[k]])
        self.tsc("dve", raug[k][32:33, :], irow[32:33, :], -slopes[h] * c * 512.0, None, ALU.add, None,
                 [b_irow], [b_raug[k]])

    for h in range(8):
        hb = h % 2
        self.dma(KTh[hb], KTd[:, h, :], allK, [b_kt[hb]])
        self.dma(QTh[hb], QTd[:, h, :], allQ, [b_qt[hb]])
        self.dma(Vh[hb], Vd[:, :, h * 128:(h + 1) * 128], allV, [b_vh[hb]])
        self.dma(arow_f[32:35, :], self.arow[h], [], [b_arow])
        self.cp("dve", irow[32:33, :], arow_f[32:33, :], [b_arow], [b_irow])
        for k in range(2):
            self.cp("dve", raug[k][32:35, :], arow_f[32:35, :], [b_arow], [b_raug[k]])
        gate_a(h, 0, hb, 0)
        gate_b(h, 0, hb, 0)
        SKEW = 2
        scb = [0, 1, 7]
        for c in range(NQC):
            k = c % 2
            if c + 1 < NQC:
                gate_a(h, c + 1, hb, (c + 1) % 2)
            pO, pS = 2 + k, 4 + k
            kts = [kt for kt in range(4 * c + 4) if slopes[h] * (c * 512 - (kt * 128 + 127)) <= 80.0]
            nkt = len(kts)
            pis = {}
            for step in range(nkt + SKEW):
                if step < nkt:
                    kt = kts[step]
                    n, a = kt // 2, kt % 2
                    pc = scb[cnt["sc"] % 3]
                    cnt["sc"] += 1
                    diag = n >= 2 * c
                    self.mm(self.ps[pc], KTh[hb][:, kt * 128:(kt + 1) * 128], QTh[hb][:, c * 512:(c + 1) * 512], True, False,
                            [b_kt[hb], b_qt[hb]], [self.psb[pc]])
                    self.mm(self.ps[pc], selb[0:35, kt * 128:(kt + 1) * 128], raug[k][0:35, :], False, not diag,
                            [b_selb, b_raug[k]], [self.psb[pc]])
                    if diag:
                        self.mm(self.ps[pc], self.ident_b, CM[n - 2 * c][a].rearrange("p a b -> p (a b)"), False, True,
                                [self.b_cb, b_cm], [self.psb[pc]])
                    pi = cnt["pt"] % 4
                    cnt["pt"] += 1
                    pis[kt] = pi
                    self.act(PT[pi], self.ps[pc], AF.Exp, [self.psb[pc]], [b_pt[pi]])
                if step >= SKEW:
                    j = step - SKEW
                    kt = kts[j]
                    pi = pis[kt]
                    self.mm(self.ps[pO], Vh[hb][:, kt, :], PT[pi], j == 0, j == nkt - 1, [b_vh[hb], b_pt[pi]], [self.psb[pO]])
                    self.mm(self.ps[pS], self.ones_b, PT[pi], j == 0, j == nkt - 1, [self.b_ones, b_pt[pi]], [self.psb[pS]])
            if c + 1 < NQC:
                gate_b(h, c + 1, hb, (c + 1) % 2)
            self.cp("act", rs[k], self.ps[pS], [self.psb[pS]], [b_rs[k]])
            self.recip(rs[k], rs[k], [b_rs[k]], [b_rs[k]])
            self.tt("dve", ya[k], self.ps[pO], rs[k], ALU.mult, [self.psb[pO], b_rs[k]], [b_ya[k]])
            self.dma(YAd[:, h, c * 512:(c + 1) * 512], ya[k], [b_ya[k]], [self.db(("YA", c))], eng="pool")


Prog.stage_attn = stage_attn


def stage_ssd(self, l):
    self.stage_begin()
    ar = self.ar
    S, NT = self.S, self.NT
    self.alloc_norm_tmp()
    selh = ar.f32(2048)
    b_selh = Buf("selh")
    self.dma(selh[0:16, :], self.selh, [], [b_selh])
    xc = [r3(ar.bf16(12 * TS), 12) for _ in range(2)]
    b_xc = bufs(2, "sxc")
    zs = [r3(ar.bf16(8 * TS), 8) for _ in range(2)]
    b_zs = bufs(2, "szs")
    dtt = [r3(ar.f32(4 * 32), 4) for _ in range(2)]
    b_dtt = bufs(2, "sdt")
    yg = r3(ar.f32(8 * TS), 8)
    b_yg = bufs(8, "yg")
    yo = r3(ar.bf16(8 * TS), 8)
    b_yo = Buf("yo")
    Sf = ar.f32(1024)
    Sb = ar.bf16(1024)
    b_Sf, b_Sb = Buf("Sf"), Buf("Sb")
    self.memset("pool", Sf, 0.0, [b_Sf])
    self.memset("pool", Sb, 0.0, [b_Sb])
    xdt = [r3(ar.bf16(1024), 16) for _ in range(2)]
    xw = [r3(ar.bf16(1024), 16) for _ in range(2)]
    b_xdt, b_xw = bufs(2, "xdt"), bufs(2, "xw")
    Btok = [ar.bf16(256) for _ in range(2)]
    b_btok = bufs(2, "btok")
    Asb = [ar.f32(32) for _ in range(2)]
    b_asb = bufs(2, "asb")
    acsT = [ar.f32(128) for _ in range(2)]
    b_acsT = bufs(2, "acsT")
    sm = [ar.f32(64) for _ in range(2)]
    b_sm = bufs(2, "sm")
    GTs = [r3(ar.f32(256), 2) for _ in range(2)]
    b_gts = bufs(2, "gts")
    Et = [r3(ar.f32(512), 4) for _ in range(2)]
    b_et = bufs(2, "et")
    WT = [r3(ar.bf16(512), 4) for _ in range(2)]
    b_wt = bufs(2, "wt")
    EA = [r3(ar.f32(512), 4) for _ in range(2)]
    b_ea = bufs(2, "ea")
    CwT = [r3(ar.bf16(512), 4) for _ in range(2)]
    b_cwt = bufs(2, "cwt")
    XCd = self.XC.rearrange("(c p) s -> p c s", p=128)
    ZSd = self.ZS.rearrange("(c p) s -> p c s", p=128)
    DTd = self.DT.rearrange("(s p) c -> p s c", p=128)
    YSd = self.YS.rearrange("(c p) s -> p c s", p=128)
    psX_b = self.ps[0].bitcast(BF16)
    ps1_b = self.ps[1].bitcast(BF16)
    Sbs = [Sb, ar.bf16(1024), ar.bf16(1024)]
    b_Sbs = [b_Sb, Buf("Sb1"), Buf("Sb2")]
    for i in (1, 2):
        self.memset("pool", Sbs[i], 0.0, [b_Sbs[i]])
    b_c5 = {5: self.psb[5], 7: self.psb[7]}
    b_y6 = bufs(2, "psY")
    cnt = {"hg": 0}

    def loads(t):
        tb = t % 2
        t0 = t * TS
        self.dma(xc[tb], XCd[:, :, t0:t0 + TS], [self.db(("XC", t))], [b_xc[tb]])
        self.dma(zs[tb], ZSd[:, :, t0:t0 + TS], [self.db(("ZS", t))], [b_zs[tb]])
        self.dma(dtt[tb], DTd[:, 4 * t:4 * t + 4, :], [self.db(("DT", t))], [b_dtt[tb]])

    def p1(t, q, ci):
        tb, k = t % 2, ci % 2
        cs = slice(q * 128, (q + 1) * 128)
        dt_ = dtt[tb][:, q, 0:16]
        dta_ = dtt[tb][:, q, 16:32]
        for ch in range(8):
            self.tr(psX_b[:, ch * 128:(ch + 1) * 128], xc[tb][:, ch, cs], self.ident_b, [b_xc[tb], self.b_cb], [self.psb[0]])
        for g in range(2):
            self.tr(ps1_b[:, 384 + g * 128:384 + (g + 1) * 128], xc[tb][:, 8 + g, cs], self.ident_b, [b_xc[tb], self.b_cb],
                    [self.psb[1]])
        self.mm(self.ps[1][:, 0:16], self.U_f, dta_, True, True, [self.b_cf, b_dtt[tb]], [self.psb[1]])
        self.mm(self.ps[1][:, 16:32], self.ones_f, dta_, True, True, [self.b_ones, b_dtt[tb]], [self.psb[1]])
        self.mm(self.ps[1][0:16, 32:160], dta_, self.U_f, True, True, [self.b_cf, b_dtt[tb]], [self.psb[1]])
        self.cp("act", Btok[k], ps1_b[:, 384:640], [self.psb[1]], [b_btok[k]])
        self.cp("act", Asb[k], self.ps[1][:, 0:32], [self.psb[1]], [b_asb[k]])
        self.cp("act", acsT[k][0:16, :], self.ps[1][0:16, 32:160], [self.psb[1]], [b_acsT[k]])
        nacs, dte, cd, tmp16 = sm[k][:, 0:16], sm[k][:, 16:32], sm[k][:, 32:48], sm[k][:, 48:64]
        self.tsc("dve", nacs, Asb[k][:, 0:16], -1.0, None, ALU.mult, None, [b_asb[k]], [b_sm[k]])
        self.tt("dve", tmp16, Asb[k][:, 16:32], Asb[k][:, 0:16], ALU.subtract, [b_asb[k]], [b_sm[k]])
        self.act(tmp16, tmp16, AF.Exp, [b_sm[k]], [b_sm[k]])
        self.tt("dve", dte, tmp16, dt_, ALU.mult, [b_sm[k], b_dtt[tb]], [b_sm[k]])
        self.act(cd, Asb[k][:, 16:32], AF.Exp, [b_asb[k]], [b_sm[k]])
        self.tt("dve", xdt[k], r3(psX_b, 16), dt_[:, :, None].to_broadcast([128, 16, 64]), ALU.mult,
                [self.psb[0], b_dtt[tb]], [b_xdt[k]])
        self.tt("dve", xw[k], r3(psX_b, 16), dte[:, :, None].to_broadcast([128, 16, 64]), ALU.mult,
                [self.psb[0], b_sm[k]], [b_xw[k]])
        for g in range(2):
            self.mm(self.ps[3 + g], Btok[k][:, g * 128:(g + 1) * 128], xw[k][:, g * 8:(g + 1) * 8, :].rearrange("p a b -> p (a b)"),
                    True, True, [b_btok[k], b_xw[k]], [self.psb[3 + g]])
        for g in range(2):
            self.mm(self.ps[2][:, g * 128:(g + 1) * 128], xc[tb][:, 8 + g, cs], xc[tb][:, 10 + g, cs], True, True,
                    [b_xc[tb]], [self.psb[2]])
        self.cp("act", GTs[k].rearrange("p a b -> p (a b)"), self.ps[2][:, 0:256], [self.psb[2]], [b_gts[k]])
        self.tt("dve", r3(Sf, 16), r3(Sf, 16), cd[:, :, None].to_broadcast([128, 16, 64]), ALU.mult, [b_Sf, b_sm[k]], [b_Sf])
        for g in range(2):
            self.tt("dve", Sf[:, g * 512:(g + 1) * 512], Sf[:, g * 512:(g + 1) * 512], self.ps[3 + g], ALU.add,
                    [b_Sf, self.psb[3 + g]], [b_Sf])
        self.cp("act", Sbs[ci % 3], Sf, [b_Sf], [b_Sbs[ci % 3]])

    def p2(t, q, ci):
        tb, k = t % 2, ci % 2
        cs = slice(q * 128, (q + 1) * 128)
        nacs = sm[k][:, 0:16]
        Sprev, b_Sprev = Sbs[(ci - 1) % 3], b_Sbs[(ci - 1) % 3]
        for hg in range(4):
            e = cnt["hg"] % 2
            cnt["hg"] += 1
            g = hg // 2
            pC = 5 if e == 0 else 7
            bC = b_c5[pC]
            for hh in range(4):
                h = hg * 4 + hh
                self.mm(self.ps[pC][:, hh * 128:(hh + 1) * 128], selh[0:16, h * 128:(h + 1) * 128], acsT[k][0:16, :], True, True,
                        [b_selh, b_acsT[k]], [bC])
            self.tt("dve", Et[e], r3(self.ps[pC], 4), self.negm_f[:, None, :].to_broadcast([128, 4, 128]), ALU.add,
                    [bC, self.b_cf], [b_et[e]])
            for hh in range(4):
                h = hg * 4 + hh
                self.act(Et[e][:, hh, :], Et[e][:, hh, :], AF.Exp, [b_et[e], b_sm[k]], [b_et[e]], bias=nacs[:, h:h + 1], scale=1.0)
            self.tt("dve", WT[e], Et[e], GTs[k][:, g:g + 1, :].to_broadcast([128, 4, 128]), ALU.mult,
                    [b_et[e], b_gts[k]], [b_wt[e]])
            self.act(EA[e].rearrange("p a b -> p (a b)"), self.ps[pC], AF.Exp, [bC], [b_ea[e]])
            self.tt("pool", CwT[e], EA[e], xc[tb][:, 10 + g:11 + g, cs].to_broadcast([128, 4, 128]), ALU.mult,
                    [b_ea[e], b_xc[tb]], [b_cwt[e]])
            pY = 6
            yoff = e * 256
            bY = b_y6[e]
            for hh in range(4):
                h = hg * 4 + hh
                po = (h % 2) * 64
                col = yoff + (hh // 2) * 128
                kw = {"tile_position": (0, 64)} if po else {}
                self.mm(self.ps[pY][po:po + 64, col:col + 128], xdt[k][:, h, :], WT[e][:, hh, :], True, False,
                        [b_xdt[k], b_wt[e]], [bY], **kw)
                self.mm(self.ps[pY][po:po + 64, col:col + 128], Sprev[:, h * 64:(h + 1) * 64], CwT[e][:, hh, :], False, True,
                        [b_Sprev, b_cwt[e]], [bY], **kw)
            for cc in range(2):
                cp_ = hg * 2 + cc
                self.stt("dve", yg[:, cp_, cs], xc[tb][:, cp_, cs], self.vcol(l, "dskip", cp_),
                         self.ps[pY][:, yoff + cc * 128:yoff + (cc + 1) * 128], ALU.mult, ALU.add,
                         [b_xc[tb], self.b_vec[l], bY], [b_yg[cp_]])
                self.tt("pool", yg[:, cp_, cs], yg[:, cp_, cs], zs[tb][:, cp_, cs], ALU.mult, [b_yg[cp_], b_zs[tb]], [b_yg[cp_]])
        if q == 3:
            t0 = t * TS
            self.rms_stats(yg, b_yg, self.rstd[tb], self.b_rstd[tb], self.sq, self.b_sq, 2)
            for c in range(8):
                self.stt("dve", yo[:, c, :], yg[:, c, :], self.vcol(l, "g_ssm", c), self.rstd[tb], ALU.mult, ALU.mult,
                         [b_yg[c], self.b_rstd[tb], self.b_vec[l]], [b_yo])
            self.dma(YSd[:, :, t0:t0 + TS], yo, [b_yo], [self.db(("YS", t))], eng="pool")

    chunks = [(t, q) for t in range(NT) for q in range(4)]
    for i, (t, q) in enumerate(chunks):
        if q == 0:
            loads(t)
        p1(t, q, i)
        if i >= 1:
            p2(chunks[i - 1][0], chunks[i - 1][1], i - 1)
    p2(chunks[-1][0], chunks[-1][1], len(chunks) - 1)


Prog.stage_ssd = stage_ssd


def stage_merge(self, l):
    self.stage_begin()
    ar = self.ar
    NT = self.NT
    self.alloc_norm_tmp()
    Wn = ["pa", "pss", "pc", "wo"]
    Wt = []
    b_W = []
    for nm in Wn:
        w = r3(ar.bf16(8 * 1024), 8)
        b = Buf("mw" + nm)
        self.dma(w, self.WB[l][nm].rearrange("(kc p) n -> p kc n", p=128), [self.db(("wb", l, nm))], [b])
        Wt.append(w)
        b_W.append(b)
    yt = [r3(ar.bf16(8 * TS), 8) for _ in range(3)]
    b_yt = bufs(3, "myt")
    gt = r3(ar.bf16(24 * TS), 24)
    b_gt = Buf("mgt")
    hres = r3(ar.f32(8 * TS), 8)
    b_h = bufs(8, "mh")
    f = r3(ar.f32(8 * TS), 8)
    b_f = bufs(8, "mf")
    mg = r3(ar.bf16(8 * TS), 8)
    b_mg = bufs(8, "mmg")
    tA = [ar.f32(TS) for _ in range(2)]
    tB = [ar.f32(TS) for _ in range(2)]
    tC = [ar.f32(TS) for _ in range(2)]
    b_tA, b_tB, b_tC = bufs(2, "tA"), bufs(2, "tB"), bufs(2, "tC")
    srcs = [(self.YA, "YA"), (self.YS, "YS"), (self.YC, "YC")]
    hv = self.hT.rearrange("(c p) s -> p c s", p=128)
    GTv = self.GT.rearrange("(c p) s -> p c s", p=128)
    for t in range(NT):
        t0 = t * TS
        for b in range(3):
            key = (srcs[b][1], t)
            deps = [self.db(key)] if srcs[b][1] != "YA" else [self.db(("YA", t))]
            self.dma(yt[b], srcs[b][0].rearrange("(c p) s -> p c s", p=128)[:, :, t0:t0 + TS], deps, [b_yt[b]])
        self.dma(gt, GTv[:, :, t0:t0 + TS], [self.db(("GT", t))], [b_gt])
        self.dma(hres, hv[:, :, t0:t0 + TS], [self.db(("h", t))], b_h)
        for c in range(8):
            k = c % 2
            for b in range(3):
                p = k * 3 + b
                for kc in range(8):
                    self.mm(self.ps[p], Wt[b][:, kc, c * 128:(c + 1) * 128], yt[b][:, kc, :], kc == 0, kc == 7,
                            [b_W[b], b_yt[b]], [self.psb[p]])
            self.tt("dve", tA[k], self.ps[k * 3 + 0], gt[:, c, :], ALU.mult, [self.psb[k * 3 + 0], b_gt], [b_tA[k]])
            self.tt("dve", tB[k], self.ps[k * 3 + 1], gt[:, 8 + c, :], ALU.mult, [self.psb[k * 3 + 1], b_gt], [b_tB[k]])
            self.tt("dve", tC[k], self.ps[k * 3 + 2], gt[:, 16 + c, :], ALU.mult, [self.psb[k * 3 + 2], b_gt], [b_tC[k]])
            self.tt("pool", tA[k], tA[k], tB[k], ALU.add, [b_tA[k], b_tB[k]], [b_tA[k]])
            self.tt("pool", mg[:, c, :], tA[k], tC[k], ALU.add, [b_tA[k], b_tC[k]], [b_mg[c]])
        for c in range(8):
            p = 6 + c % 2
            for kc in range(8):
                self.mm(self.ps[p], Wt[3][:, kc, c * 128:(c + 1) * 128], mg[:, kc, :], kc == 0, kc == 7,
                        [b_W[3], b_mg[kc]], [self.psb[p]])
            self.cp("act", f[:, c, :], self.ps[p], [self.psb[p]], [b_f[c]])
        self.postnorm_res(l, "g_mpost", f, b_f, hres, b_h, None, t % 2)
        self.dma(hv[:, :, t0:t0 + TS], hres, b_h, [self.db(("h", t))], eng="pool")


Prog.stage_merge = stage_merge
```

```python
import numpy as np
from contextlib import ExitStack
import concourse.bass as bass
import concourse.mybir as mybir
from concourse.bass_utils import run_bass_kernel_spmd

F32 = mybir.dt.float32
BF16 = mybir.dt.bfloat16
AF = mybir.ActivationFunctionType
ALU = mybir.AluOpType
AX = mybir.AxisListType

D = 1024
NL = 2
FH = 2816
NJ = FH // 128
INC = 10768
TS = 512
EPS = 1e-6
NEG = -30000.0
OQ, OK_, OV, OZ, OX, ODT, OU, OVG, OG = 0, 1024, 2048, 3072, 4096, 5632, 5648, 6672, 7696

EPOCH = 30000
DEPOCH = 1800
KSLOT = 8


class Buf:
    __slots__ = ("w", "r", "name")

    def __init__(self, name=""):
        self.w = None
        self.r = {}
        self.name = name


def bufs(n, name=""):
    return [Buf(f"{name}{i}") for i in range(n)]


class _Op:
    __slots__ = ("fn", "kind", "waits", "signal", "sigval", "slot", "use")

    def __init__(self, fn, kind):
        self.fn = fn
        self.kind = kind
        self.waits = []
        self.signal = False
        self.sigval = 0
        self.slot = -1
        self.use = 0


class _Eng:
    def __init__(self, name):
        self.name = name
        self.ops = []
        self.seen = {}
        self.ndma = 0


class Sched:
    ENGS = ("pe", "act", "dve", "pool", "sp")

    def __init__(self, nc):
        self.nc = nc
        self.e = {n: _Eng(n) for n in self.ENGS}

    def op(self, eng, fn, reads=(), writes=(), kind="c"):
        e = self.e[eng]
        o = _Op(fn, kind)
        idx = len(e.ops)
        deps = {}

        def add(ev, hard):
            if ev is None:
                return
            k, v = ev
            if (not hard) and kind == "c" and k == ("c", eng):
                return
            if deps.get(k, -1) < v:
                deps[k] = v

        for b in reads:
            add(b.w, True)
        for b in writes:
            add(b.w, eng != "pe")
            for k, v in b.r.items():
                add((k, v), False)
        if kind == "d":
            d = e.ndma
            e.ndma += 1
            o.slot = d % KSLOT
            o.use = d // KSLOT + 1
            if o.use > 1:
                add((("d", eng, o.slot), o.use - 1), True)
            ev = (("d", eng, o.slot), o.use)
        else:
            ev = (("c", eng), idx)
        for k, v in deps.items():
            if e.seen.get(k, -1) >= v:
                continue
            e.seen[k] = v
            o.waits.append((k, v))
            if k[0] == "c":
                self.e[k[1]].ops[v].signal = True
        for b in reads:
            k, v = ev
            if b.r.get(k, -1) < v:
                b.r[k] = v
        for b in writes:
            b.w = ev
            b.r = {}
        e.ops.append(o)
        return ev

    def barrier(self):
        evs = []
        for n, e in self.e.items():
            for i in range(len(e.ops) - 1, -1, -1):
                if e.ops[i].kind == "c":
                    evs.append((("c", n), i))
                    break
            d = e.ndma
            for s in range(min(KSLOT, d)):
                evs.append((("d", n, s), (d - 1 - s) // KSLOT + 1))
        for n in self.ENGS:
            e = self.e[n]
            o = _Op(None, "n")
            for k, v in evs:
                if k == ("c", n):
                    continue
                if e.seen.get(k, -1) >= v:
                    continue
                e.seen[k] = v
                o.waits.append((k, v))
                if k[0] == "c":
                    self.e[k[1]].ops[v].signal = True
            if o.waits:
                e.ops.append(o)

    def emit(self):
        nc = self.nc
        for e in self.e.values():
            c = 0
            for o in e.ops:
                if o.kind == "c" and o.signal:
                    c += 1
                    o.sigval = c
            e.nsig = c
        with ExitStack() as st:
            sems = {}

            def sem(key):
                if key not in sems:
                    sems[key] = st.enter_context(nc.semaphore("s_" + "_".join(str(x) for x in key)))
                return sems[key]

            for n, e in self.e.items():
                for ep in range((e.nsig + EPOCH - 1) // EPOCH):
                    sem(("c", n, ep))
                if e.ndma:
                    maxuse = (e.ndma - 1) // KSLOT + 1
                    for s in range(min(KSLOT, e.ndma)):
                        for ep in range((maxuse + DEPOCH - 1) // DEPOCH):
                            sem(("d", n, s, ep))

            def resolve(k, v):
                if k[0] == "c":
                    sv = self.e[k[1]].ops[v].sigval
                    assert sv > 0
                    return sem(("c", k[1], (sv - 1) // EPOCH)), (sv - 1) % EPOCH + 1
                return sem(("d", k[1], k[2], (v - 1) // DEPOCH)), 16 * ((v - 1) % DEPOCH + 1)

            block = st.enter_context(nc.Block())
            regs = {"pe": block.tensor, "act": block.scalar, "dve": block.vector,
                    "pool": block.gpsimd, "sp": block.sync}
            for n in self.ENGS:
                e = self.e[n]
                if not e.ops:
                    continue

                def body(eng, e=e, n=n):
                    for o in e.ops:
                        for k, v in o.waits:
                            s, val = resolve(k, v)
                            eng.wait_ge(s, val)
                        if o.fn is None:
                            continue
                        ins = o.fn(eng)
                        if o.kind == "d":
                            ins.then_inc(sem(("d", n, o.slot, (o.use - 1) // DEPOCH)), 16)
                        elif o.signal:
                            ins.then_inc(sem(("c", n, (o.sigval - 1) // EPOCH)), 1)

                regs[n](body)

    def stats(self):
        return {n: (len(e.ops), sum(len(o.waits) for o in e.ops), e.ndma) for n, e in self.e.items()}


class Arena:
    def __init__(self, nc, nfloats):
        self.t = nc.alloc_sbuf_tensor("arena", [128, nfloats], F32)
        self.n = nfloats
        self.p = 0
        self.base = 0

    def f32(self, n):
        n8 = (n + 7) // 8 * 8
        assert self.p + n8 <= self.n, f"arena overflow {self.p}+{n8}>{self.n}"
        a = self.t[:, self.p:self.p + n]
        self.p += n8
        return a

    def bf16(self, n):
        nf = (n + 1) // 2
        n8 = (nf + 7) // 8 * 8
        assert self.p + n8 <= self.n, f"arena overflow {self.p}+{n8}>{self.n}"
        a = self.t[:, self.p:self.p + nf].bitcast(BF16)
        self.p += n8
        return a

    def keep(self):
        self.base = self.p

    def reset(self):
        self.p = self.base


def r3(ap, a):
    return ap.rearrange("p (a b) -> p a b", a=a)


class Prog:
    def __init__(self, S, nl=NL, dbg=(), stop_after=None):
        self.S = S
        self.nl = nl
        self.NT = S // TS
        self.NB = S // 256
        self.dbg = set(dbg)
        self.stop_after = stop_after
        nc = self.nc = bass.Bass("TRN2", target_bir_lowering=False)
        self.sch = Sched(nc)
        self.ar = Arena(nc, 52736)
        self.dram = {}
        self.dbuf = {}
        self.ps = [nc.alloc_psum_tensor(f"ps{i}", [128, 512], F32)[:] for i in range(8)]
        self.psb = bufs(8, "ps")
        self.rr = 0

    def din(self, name, shape, dt=F32):
        t = self.nc.dram_tensor(name, list(shape), dt, kind="ExternalInput").ap()
        self.dram[name] = t
        return t

    def dscr(self, name, shape, dt, out=False):
        kind = "ExternalOutput" if (out or name in self.dbg) else "Internal"
        t = self.nc.dram_tensor(name, list(shape), dt, kind=kind).ap()
        self.dram[name] = t
        return t

    def db(self, key):
        if key not in self.dbuf:
            self.dbuf[key] = Buf(str(key))
        return self.dbuf[key]

    def dma(self, out, in_, reads, writes, eng="sp"):
        self.sch.op(eng, lambda e: e.dma_start(out=out, in_=in_), reads, writes, kind="d")

    def mm(self, out, lhsT, rhs, start, stop, reads, writes, **kw):
        self.sch.op("pe", lambda e: e.matmul(out, lhsT, rhs, start=start, stop=stop, **kw), reads, writes)

    def tr(self, out, in_, ident, reads, writes):
        self.sch.op("pe", lambda e: e.transpose(out, in_, ident), reads, writes)

    def act(self, out, in_, func, reads, writes, bias=None, scale=None, accum=None):
        kw = {}
        if bias is not None:
            kw["bias"] = bias
        if scale is not None:
            kw["scale"] = scale
        if accum is not None:
            kw["accum_out"] = accum
        self.sch.op("act", lambda e: e.activation(out=out, in_=in_, func=func, **kw), reads, writes)

    def tt(self, eng, out, in0, in1, op, reads, writes):
        self.sch.op(eng, lambda e: e.tensor_tensor(out=out, in0=in0, in1=in1, op=op), reads, writes)

    def tsc(self, eng, out, in0, s1, s2, op0, op1, reads, writes):
        if s2 is None:
            self.sch.op(eng, lambda e: e.tensor_scalar(out=out, in0=in0, scalar1=s1, scalar2=None, op0=op0), reads, writes)
        else:
            self.sch.op(eng, lambda e: e.tensor_scalar(out=out, in0=in0, scalar1=s1, scalar2=s2, op0=op0, op1=op1), reads, writes)

    def stt(self, eng, out, in0, scalar, in1, op0, op1, reads, writes):
        self.sch.op(eng, lambda e: e.scalar_tensor_tensor(out=out, in0=in0, scalar=scalar, in1=in1, op0=op0, op1=op1), reads, writes)

    def cp(self, eng, out, in_, reads, writes):
        if eng == "act":
            self.act(out, in_, AF.Copy, reads, writes)
        else:
            self.sch.op(eng, lambda e: e.tensor_copy(out=out, in_=in_), reads, writes)

    def memset(self, eng, ap, val, writes):
        self.sch.op(eng, lambda e: e.memset(ap, val), (), writes)

    def recip(self, out, in_, reads, writes):
        self.sch.op("dve", lambda e: e.reciprocal(out=out, in_=in_), reads, writes)

    def stage_begin(self):
        self.sch.barrier()
        self.ar.reset()

    def declare(self):
        S, nl = self.S, self.nl
        self.xT = self.din("xT", [D, S])
        self.cst = self.din("cst", [128, 512])
        self.selb = self.din("selb", [35, self.S])
        self.arow = self.din("arow", [8, 3, 512])
        self.selh = self.din("selh", [16, 16 * 128])
        self.W = []
        for l in range(nl):
            w = {}
            w["gu1"] = self.din(f"gu1_{l}", [D, 2 * FH])
            w["dn1"] = self.din(f"dn1_{l}", [FH, D])
            w["win"] = self.din(f"win_{l}", [D, INC])
            w["pa"] = self.din(f"pa_{l}", [D, D])
            w["pss"] = self.din(f"pss_{l}", [D, D])
            w["pc"] = self.din(f"pc_{l}", [D, D])
            w["wo"] = self.din(f"wo_{l}", [D, D])
            w["gu2"] = self.din(f"gu2_{l}", [D, 2 * FH])
            w["dn2"] = self.din(f"dn2_{l}", [FH, D])
            w["vec"] = self.din(f"vec_{l}", [128, 128])
            w["rows"] = self.din(f"rows_{l}", [1, 3 * 1024 + 32])
            w["sguwT"] = self.din(f"sguwT_{l}", [128, 8 * 128])
            self.W.append(w)
        self.WB = []
        for l in range(nl):
            wb = {}
            for k, shp in (("gu1", [D, 2 * FH]), ("dn1", [FH, D]), ("win", [D, INC]), ("pa", [D, D]),
                           ("pss", [D, D]), ("pc", [D, D]), ("wo", [D, D]), ("gu2", [D, 2 * FH]), ("dn2", [FH, D])):
                wb[k] = self.dscr(f"wb_{k}_{l}", shp, BF16)
            self.WB.append(wb)
        self.hT = self.dscr("hT", [D, S], F32)
        self.outT = self.dscr("outT", [D, S], F32, out=True)
        self.QT = self.dscr("QT", [D, S], BF16)
        self.KT = self.dscr("KT", [D, S], BF16)
        self.V = self.dscr("V", [S, D], BF16)
        self.ZS = self.dscr("ZS", [D, S], BF16)
        self.XC = self.dscr("XC", [1536, S], BF16)
        self.DT = self.dscr("DT", [S, 32], F32)
        self.GT = self.dscr("GT", [3 * D, S], BF16)
        self.YC = self.dscr("YC", [D, S], BF16)
        self.YA = self.dscr("YA", [D, S], BF16)
        self.YS = self.dscr("YS", [D, S], BF16)
        self.KM = self.dscr("KM", [128, 8 * self.NB], F32)

    def setup_consts(self):
        ar = self.ar
        self.c_f = ar.f32(512)
        self.b_cf = Buf("cst")
        self.dma(self.c_f, self.cst, [], [self.b_cf])
        self.ident_f = self.c_f[:, 0:128]
        self.U_f = self.c_f[:, 128:256]
        self.negm_f = self.c_f[:, 256:384]
        self.c_b = ar.bf16(384)
        self.b_cb = Buf("cstb")
        self.cp("dve", self.c_b, self.c_f[:, 0:384], [self.b_cf], [self.b_cb])
        self.ident_b = self.c_b[:, 0:128]
        self.U_b = self.c_b[:, 128:256]
        self.ones_b = ar.bf16(128)
        self.ones_f = ar.f32(128)
        self.b_ones = Buf("ones")
        self.memset("pool", self.ones_b, 1.0, [self.b_ones])
        self.memset("pool", self.ones_f, 1.0, [self.b_ones])
        self.vec = []
        self.b_vec = []
        for l in range(self.nl):
            v = ar.f32(128)
            b = Buf(f"vec{l}")
            self.dma(v, self.W[l]["vec"], [], [b])
            self.vec.append(v)
            self.b_vec.append(b)
        ar.keep()

    def vcol(self, l, name, c):
        off = {"g_f1pre": 0, "g_f1post": 8, "g_mpre": 16, "g_mpost": 24, "g_f2pre": 32, "g_f2post": 40,
               "g_ssm": 48, "conv_w": 56, "conv_b": 104, "dskip": 116}[name]
        return self.vec[l][:, off + c: off + c + 1]

    def stage_convert(self):
        self.stage_begin()
        ar = self.ar
        NBUF = 4
        CW = 2048
        src = [ar.f32(CW) for _ in range(NBUF)]
        dst = [ar.bf16(CW) for _ in range(NBUF)]
        bs = bufs(NBUF, "cvs")
        bd = bufs(NBUF, "cvd")
        engs = ["act", "dve", "pool"]
        i = 0
        for l in range(self.nl):
            for k, wsrc in self.W[l].items():
                if k in ("vec", "rows", "sguwT"):
                    continue
                wdst = self.WB[l][k]
                K, N = wsrc.shape
                for kc in range(K // 128):
                    for c0 in range(0, N, CW):
                        cw = min(CW, N - c0)
                        import os
                        if i >= int(os.environ.get("CVLIM", "1000000")):
                            continue
                        b = i % NBUF
                        self.dma(src[b][:, 0:cw], wsrc[kc * 128:(kc + 1) * 128, c0:c0 + cw], [], [bs[b]])
                        self.cp(engs[i % 3], dst[b][:, 0:cw], src[b][:, 0:cw], [bs[b]], [bd[b]])
                        self.dma(wdst[kc * 128:(kc + 1) * 128, c0:c0 + cw], dst[b][:, 0:cw], [bd[b]],
                                 [self.db(("wb", l, k))], eng=("pool" if i % 2 == 0 else "act"))
                        i += 1

    def rms_stats(self, src_f, b_src, rstd, b_rstd, sq, b_sq, psi):
        ps, pb = self.ps[psi], self.psb[psi]
        for c in range(8):
            j = c % 2
            self.act(sq[j], src_f[:, c, :], AF.Square, [b_src[c]], [b_sq[j]], scale=1.0 / 32.0)
            self.mm(ps, self.ones_b, sq[j], c == 0, c == 7, [self.b_ones, b_sq[j]], [pb])
        self.act(rstd, ps, AF.Sqrt, [pb, self.b_eps], [b_rstd], bias=self.eps_col, scale=1.0)
        self.recip(rstd, rstd, [b_rstd], [b_rstd])

    def alloc_norm_tmp(self):
        ar = self.ar
        self.sq = [ar.bf16(512) for _ in range(2)]
        self.b_sq = bufs(2, "sq")
        self.rstd = [ar.f32(512) for _ in range(2)]
        self.b_rstd = bufs(2, "rstd")
        self.eps_col = ar.f32(8)[:, 0:1]
        self.b_eps = Buf("eps")
        self.memset("pool", self.eps_col, EPS, [self.b_eps])

    def prenorm(self, l, gname, hres, b_h, xn, b_xn, k):
        self.rms_stats(hres, b_h, self.rstd[k], self.b_rstd[k], self.sq, self.b_sq, 7)
        for c in range(8):
            self.stt("dve", xn[:, c, :], hres[:, c, :], self.vcol(l, gname, c), self.rstd[k],
                     ALU.mult, ALU.mult, [b_h[c], self.b_rstd[k], self.b_vec[l]], [b_xn[c]])

    def postnorm_res(self, l, gname, f, b_f, hres, b_h, coef, k):
        self.rms_stats(f, b_f, self.rstd[k], self.b_rstd[k], self.sq, self.b_sq, 7)
        for c in range(8):
            eng = "pool"
            self.stt("dve", f[:, c, :], f[:, c, :], self.gc[l][gname][:, c:c + 1], self.rstd[k], ALU.mult, ALU.mult,
                     [b_f[c], self.b_rstd[k], self.b_gc], [b_f[c]])
            self.tt(eng, hres[:, c, :], hres[:, c, :], f[:, c, :], ALU.add, [b_f[c], b_h[c]], [b_h[c]])

    def setup_gc(self):
        ar = self.ar
        self.gc = []
        self.b_gc = Buf("gc")
        for l in range(self.nl):
            t = ar.f32(24)
            d = {}
            for i, (nm, coef) in enumerate((("g_f1post", 0.5), ("g_mpost", 1.0), ("g_f2post", 0.5))):
                off = {"g_f1post": 8, "g_mpost": 24, "g_f2post": 40}[nm]
                self.tsc("dve", t[:, i * 8:(i + 1) * 8], self.vec[l][:, off:off + 8], coef, None, ALU.mult, None,
                         [self.b_vec[l]], [self.b_gc])
                d[nm] = t[:, i * 8:(i + 1) * 8]
            self.gc.append(d)
        ar.keep()

    def stage_ffn(self, l, which, src, dst, src_key, dst_key):
        self.stage_begin()
        ar = self.ar
        NT = self.NT
        wgu = self.WB[l]["gu1" if which == 1 else "gu2"]
        wdn = self.WB[l]["dn1" if which == 1 else "dn2"]
        bw_gu = self.db(("wb", l, "gu1" if which == 1 else "gu2"))
        bw_dn = self.db(("wb", l, "dn1" if which == 1 else "dn2"))
        gpre = "g_f1pre" if which == 1 else "g_f2pre"
        gpost = "g_f1post" if which == 1 else "g_f2post"
        self.alloc_norm_tmp()
        hres = [r3(ar.f32(8 * TS), 8) for _ in range(2)]
        b_h = [bufs(8, f"hres{i}_") for i in range(2)]
        xn = r3(ar.bf16(8 * TS), 8)
        b_xn = bufs(8, "xn")
        HT = r3(ar.bf16(NJ * TS), NJ)
        b_H = bufs(NJ, "H")
        f = r3(ar.f32(8 * TS), 8)
        b_f = bufs(8, "f")
        sg = [ar.f32(TS) for _ in range(2)]
        b_sg = bufs(2, "sg")
        JG = 2
        NG = NJ // JG
        wg = [r3(ar.bf16(8 * 2 * JG * 128), 8) for _ in range(3)]
        b_wg = bufs(3, "wg")
        CG = 2
        wd = [r3(ar.bf16(NJ * CG * 128), NJ) for _ in range(2)]
        b_wd = bufs(2, "wd")
        srcv = src.rearrange("(c p) s -> p c s", p=128)
        dstv = dst.rearrange("(c p) s -> p c s", p=128)
        wguv = wgu.rearrange("(kc p) n -> p kc n", p=128)
        wdnv = wdn.rearrange("(j p) n -> p j n", p=128)
        gi = 0
        di = 0
        self.dma(hres[0], srcv[:, :, 0:TS], [self.db((src_key, 0))], b_h[0])
        self.prenorm(l, gpre, hres[0], b_h[0], xn, b_xn, 0)
        for t in range(NT):
            k = t % 2
            t0 = t * TS
            for g in range(NG):
                wb_ = gi % 3
                gi += 1
                j0 = g * JG
                self.dma(wg[wb_][:, :, 0:JG * 128], wguv[:, :, j0 * 128:(j0 + JG) * 128], [bw_gu], [b_wg[wb_]])
                self.dma(wg[wb_][:, :, JG * 128:2 * JG * 128], wguv[:, :, FH + j0 * 128:FH + (j0 + JG) * 128], [bw_gu], [b_wg[wb_]])
                for jj in range(JG):
                    j = j0 + jj
                    pg = (2 * (j % 2))
                    pu = pg + 1
                    for kc in range(8):
                        self.mm(self.ps[pg], wg[wb_][:, kc, jj * 128:(jj + 1) * 128], xn[:, kc, :], kc == 0, kc == 7,
                                [b_wg[wb_], b_xn[kc]], [self.psb[pg]])
                    for kc in range(8):
                        self.mm(self.ps[pu], wg[wb_][:, kc, JG * 128 + jj * 128:JG * 128 + (jj + 1) * 128], xn[:, kc, :],
                                kc == 0, kc == 7, [b_wg[wb_], b_xn[kc]], [self.psb[pu]])
                    s = j % 2
                    self.act(sg[s], self.ps[pg], AF.Silu, [self.psb[pg]], [b_sg[s]])
                    self.tt("dve", HT[:, j, :], sg[s], self.ps[pu], ALU.mult, [b_sg[s], self.psb[pu]], [b_H[j]])
            if t + 1 < NT:
                self.dma(hres[1 - k], srcv[:, :, t0 + TS:t0 + 2 * TS], [self.db((src_key, t + 1))], b_h[1 - k])
                self.prenorm(l, gpre, hres[1 - k], b_h[1 - k], xn, b_xn, 1 - k)
            for cg in range(8 // CG):
                wb_ = di % 2
                di += 1
                self.dma(wd[wb_], wdnv[:, :, cg * CG * 128:(cg + 1) * CG * 128], [bw_dn], [b_wd[wb_]])
                for cc in range(CG):
                    c = cg * CG + cc
                    pd = 4 + (c % 2)
                    for j in range(NJ):
                        self.mm(self.ps[pd], wd[wb_][:, j, cc * 128:(cc + 1) * 128], HT[:, j, :], j == 0, j == NJ - 1,
                                [b_wd[wb_], b_H[j]], [self.psb[pd]])
                    self.cp("act", f[:, c, :], self.ps[pd], [self.psb[pd]], [b_f[c]])
            self.postnorm_res(l, gpost, f, b_f, hres[k], b_h[k], None, k)
            self.dma(dstv[:, :, t0:t0 + TS], hres[k], b_h[k], [self.db((dst_key, t))], eng="pool")

    def build(self):
        self.declare()
        self.setup_consts()
        self.setup_gc()
        self.stage_convert()
        stages = []
        for l in range(self.nl):
            src = self.xT if l == 0 else self.hT
            stages.append(("ffn1", lambda l=l, src=src: self.stage_ffn(l, 1, src, self.hT, "x" if l == 0 else "h", "h")))
            stages.append(("proj", lambda l=l: self.stage_proj(l)))
            stages.append(("attn", lambda l=l: self.stage_attn(l)))
            stages.append(("ssd", lambda l=l: self.stage_ssd(l)))
            stages.append(("merge", lambda l=l: self.stage_merge(l)))
            last = (l == self.nl - 1)
            stages.append(("ffn2", lambda l=l, last=last: self.stage_ffn(l, 2, self.hT, self.outT if last else self.hT, "h",
                                                                          "o" if last else "h")))
        for i, (nm, fn) in enumerate(stages):
            if self.stop_after is not None and i > self.stop_after:
                break
            fn()
        self.sch.barrier()
        self.sch.emit()
        return self.nc


def host_consts(S):
    NB = S // 256
    cst = np.zeros((128, 512), np.float32)
    cst[:, 0:128] = np.eye(128, dtype=np.float32)
    t = np.arange(128)
    cst[:, 128:256] = (t[:, None] <= t[None, :]).astype(np.float32)
    cst[:, 256:384] = np.where(t[:, None] <= t[None, :], 0.0, NEG)
    cst[:, 384:512] = t[None, :].astype(np.float32)
    NKT = S // 128
    selb = np.zeros((35, NKT * 128), np.float32)
    for kt in range(NKT):
        selb[kt // 2, kt * 128:(kt + 1) * 128] = -NEG
        selb[34, kt * 128:(kt + 1) * 128] = kt
    selb[32, :] = 1.0
    selb[33, :] = np.tile(t, NKT).astype(np.float32)
    slopes = 2.0 ** (-8.0 * (np.arange(8) + 1.0) / 8)
    arow = np.zeros((8, 3, 512), np.float32)
    for h in range(8):
        arow[h, 0, :] = -slopes[h] * np.arange(512)
        arow[h, 1, :] = slopes[h]
        arow[h, 2, :] = slopes[h] * 128.0
    selh = np.zeros((16, 16 * 128), np.float32)
    for h in range(16):
        selh[h, h * 128:(h + 1) * 128] = 1.0
    return {"cst": cst, "selb": selb, "arow": arow, "selh": selh}


def host_layer_inputs(inp, l):
    def colv(v):
        return np.ascontiguousarray(v.reshape(-1, 128).T)
    vec = np.zeros((128, 128), np.float32)
    for i, nm in enumerate(("ffn1_pre_g", "ffn1_post_g", "mix_pre_g", "mix_post_g", "ffn2_pre_g", "ffn2_post_g")):
        vec[:, i * 8:(i + 1) * 8] = colv(inp[nm][l])
    vec[:, 48:56] = colv(inp["ssm_norm_g"][l])
    cw = inp["conv_w"][l]
    vec[:, 56:104] = cw.T.reshape(12, 128, 4).transpose(1, 0, 2).reshape(128, 48)
    vec[:, 104:116] = colv(inp["conv_b"][l])
    vec[:, 116:124] = colv(np.repeat(inp["d_skip"][l], 64))
    rows = np.concatenate([inp["sgu_ln_g"][l], inp["sgu_ln_b"][l], inp["sgu_b"][l].reshape(-1),
                           inp["dt_bias"][l], inp["a_log"][l]])[None, :].astype(np.float32)
    sguwT = np.ascontiguousarray(inp["sgu_w"][l].transpose(2, 0, 1).reshape(128, 1024))
    d = {
        f"gu1_{l}": inp["ffn1_w_gu"][l], f"dn1_{l}": inp["ffn1_w_down"][l], f"win_{l}": inp["w_in"][l],
        f"pa_{l}": inp["p_attn"][l], f"pss_{l}": inp["p_ssm"][l], f"pc_{l}": inp["p_sgu"][l], f"wo_{l}": inp["w_out"][l],
        f"gu2_{l}": inp["ffn2_w_gu"][l], f"dn2_{l}": inp["ffn2_w_down"][l],
        f"vec_{l}": vec, f"rows_{l}": rows, f"sguwT_{l}": sguwT,
    }
    return {k: np.ascontiguousarray(v, dtype=np.float32) for k, v in d.items()}


_CACHE = {}


def kernel(**inputs):
    inp = {k: np.asarray(v) for k, v in inputs.items()}
    x = inp["x"]
    B, S, _ = x.shape
    key = (S,)
    if key not in _CACHE:
        _CACHE[key] = Prog(S).build()
    nc = _CACHE[key]
    shared = host_consts(S)
    for l in range(NL):
        shared.update(host_layer_inputs(inp, l))
    workers = [0, 1, 4, 5]
    big = [k for k, v in shared.items() if v.size >= (1 << 20)]
    zeros = dict(shared)
    for k in big:
        zeros[k] = np.zeros_like(shared[k])
    xz = np.zeros((D, S), np.float32)
    in_maps = []
    for c in range(8):
        if c in workers:
            m = dict(shared)
            m["xT"] = np.ascontiguousarray(x[workers.index(c)].T)
        else:
            m = dict(zeros)
            m["xT"] = xz
        in_maps.append(m)
    res = run_bass_kernel_spmd(nc, in_maps, core_ids=list(range(8)))
    out = np.stack([np.ascontiguousarray(np.asarray(res.results[workers[b]]["outT"]).T) for b in range(B)], axis=0)
    return out.astype(np.float32)


def stage_proj(self, l):
    self.stage_begin()
    ar = self.ar
    NT, NB, S = self.NT, self.NB, self.S
    winv = self.WB[l]["win"].rearrange("(kc p) n -> p kc n", p=128)
    bw = self.db(("wb", l, "win"))
    self.alloc_norm_tmp()
    self.alloc_gelu()
    hres = r3(ar.f32(8 * TS), 8)
    b_h = bufs(8, "ph")
    xn = r3(ar.bf16(8 * TS), 8)
    b_xn = bufs(8, "pxn")
    NWB = 4
    wblk = [r3(ar.bf16(8 * 512), 8) for _ in range(NWB)]
    b_wblk = bufs(NWB, "wblk")
    ob = [r3(ar.bf16(8 * TS), 8) for _ in range(3)]
    b_ob = bufs(3, "ob")
    gu = r3(ar.bf16(8 * TS), 8)
    b_gu = Buf("gu")
    xbc = r3(ar.f32(12 * (TS + 8)), 12)
    b_xbc = bufs(12, "xbc")
    cacc = [ar.f32(TS) for _ in range(2)]
    b_cacc = bufs(2, "cacc")
    xco = r3(ar.bf16(12 * TS), 12)
    b_xco = Buf("xco")
    vt = r3(ar.bf16(4 * 1024), 4)
    b_vt = Buf("vt")
    vgf = [ar.f32(1024) for _ in range(2)]
    b_vgf = bufs(2, "vgf")
    vgt = [ar.f32(1024) for _ in range(2)]
    b_vgt = bufs(2, "vgt")
    vgn = [ar.bf16(1024) for _ in range(2)]
    b_vgn = bufs(2, "vgn")
    rows = ar.f32(3 * 1024 + 32)
    b_rows = Buf("rows")
    self.dma(rows, self.W[l]["rows"].to_broadcast([128, 3 * 1024 + 32]), [], [b_rows])
    lng, lnb, sgub = rows[:, 0:1024], rows[:, 1024:2048], rows[:, 2048:3072]
    dtb, alog = rows[:, 3072:3088], rows[:, 3088:3104]
    abc = ar.f32(16)
    b_abc = Buf("abc")
    self.act(abc, alog, AF.Exp, [b_rows], [b_abc])
    self.tsc("dve", abc, abc, -1.0, None, ALU.mult, None, [b_abc], [b_abc])
    sg_all = ar.f32(1024)
    sgt = [sg_all[:, 0:512], sg_all[:, 512:1024]]
    b_sgt = bufs(2, "sgt")
    self.dma(sg_all, self.W[l]["sguwT"], [], b_sgt)
    wsT = r3(ar.bf16(1024), 8)
    b_wsT = Buf("wsT")
    self.tt("dve", wsT, r3(sg_all, 8), self.U_f[:, None, :].to_broadcast([128, 8, 128]), ALU.mult, b_sgt + [self.b_cf], [b_wsT])
    st = ar.f32(16)
    b_st = bufs(2, "st")
    st2 = [ar.f32(8) for _ in range(2)]
    yc = r3(ar.bf16(8 * TS), 8)
    b_yc = Buf("yc")
    dtt = r3(ar.f32(4 * 32), 4)
    b_dtt = Buf("dtt")
    dtmp = [ar.f32(16) for _ in range(2)]
    b_dtmp = bufs(2, "dtmp")
    km = ar.f32(8 * NB)
    b_km = Buf("km")

    hv = self.hT.rearrange("(c p) s -> p c s", p=128)
    QTv = self.QT.rearrange("(c p) s -> p c s", p=128)
    KTv = self.KT.rearrange("(c p) s -> p c s", p=128)
    ZSv = self.ZS.rearrange("(c p) s -> p c s", p=128)
    XCv = self.XC.rearrange("(c p) s -> p c s", p=128)
    GTv = self.GT.rearrange("(c p) s -> p c s", p=128)
    YCv = self.YC.rearrange("(c p) s -> p c s", p=128)
    Vv = self.V.rearrange("(s p) f -> p s f", p=128)
    DTv = self.DT.rearrange("(s p) c -> p s c", p=128)

    cnt = {"w": 0, "ps": 0, "ob": 0}
    import os
    PL = int(os.environ.get("PROJ_LIM", "99"))

    def load_block(c0, ncols, avoid=()):
        i = cnt["w"] % NWB
        cnt["w"] += 1
        while i in avoid:
            i = cnt["w"] % NWB
            cnt["w"] += 1
        self.dma(wblk[i][:, :, 0:ncols], winv[:, :, c0:c0 + ncols], [bw], [b_wblk[i]])
        return i

    def next_ps():
        i = cnt["ps"] % 6
        cnt["ps"] += 1
        return i

    def fm_segment(c0, nchunks, evac):
        for b0 in range(0, nchunks, 4):
            nb_ = min(4, nchunks - b0)
            wi = load_block(c0 + b0 * 128, nb_ * 128)
            for cc in range(nb_):
                p = next_ps()
                for kc in range(8):
                    self.mm(self.ps[p], wblk[wi][:, kc, cc * 128:(cc + 1) * 128], xn[:, kc, :], kc == 0, kc == 7,
                            [b_wblk[wi], b_xn[kc]], [self.psb[p]])
                evac(b0 + cc, p)

    def tm_segment(c0, ncols, evac):
        for bi, b0 in enumerate(range(0, ncols, 512)):
            w_ = min(512, ncols - b0)
            wi = load_block(c0 + b0, w_)
            for sub in range(4):
                p = next_ps()
                for kc in range(8):
                    self.mm(self.ps[p][:, 0:w_], xn[:, kc, sub * 128:(sub + 1) * 128], wblk[wi][:, kc, 0:w_], kc == 0, kc == 7,
                            [b_wblk[wi], b_xn[kc]], [self.psb[p]])
                evac(bi, sub, p, w_)

    for t in range(NT):
        t0 = t * TS
        self.dma(hres, hv[:, :, t0:t0 + TS], [self.db(("h", t))], b_h)
        self.prenorm(l, "g_mpre", hres, b_h, xn, b_xn, t % 2)
        if t == 0:
            self.memset("pool", xbc[:, :, 0:3], 0.0, b_xbc)
        else:
            self.cp("pool", xbc[:, :, 0:3], xbc[:, :, TS:TS + 3], b_xbc, b_xbc)

        if PL <= 0:
            continue
        oq = cnt["ob"] % 3
        cnt["ob"] += 1

        def ev_q(c, p, oq=oq):
            self.act(ob[oq][:, c, :], self.ps[p], AF.Copy, [self.psb[p]], [b_ob[oq]], scale=128.0 ** -0.5)
        fm_segment(OQ, 8, ev_q)
        self.dma(QTv[:, :, t0:t0 + TS], ob[oq], [b_ob[oq]], [self.db(("QT", t))], eng="act")
        if PL <= 1:
            continue
        okk = cnt["ob"] % 3
        cnt["ob"] += 1

        def ev_k(c, p, okk=okk, t=t):
            for hh in range(2):
                col = c * NB + 2 * t + hh
                self.act(ob[okk][:, c, hh * 256:(hh + 1) * 256], self.ps[p][:, hh * 256:(hh + 1) * 256], AF.Copy,
                         [self.psb[p]], [b_ob[okk], b_km], accum=km[:, col:col + 1])
        fm_segment(OK_, 8, ev_k)
        self.dma(KTv[:, :, t0:t0 + TS], ob[okk], [b_ob[okk]], [self.db(("KT", t))], eng="act")
        if PL <= 2:
            continue

        def ev_v(bi, sub, p, w_):
            self.act(vt[:, sub, bi * 512:(bi + 1) * 512], self.ps[p], AF.Copy, [self.psb[p]], [b_vt])
        tm_segment(OV, 1024, ev_v)
        self.dma(Vv[:, 4 * t:4 * t + 4, :], vt, [b_vt], [self.db(("V", t))], eng="act")
        if PL <= 3:
            continue
        oz = cnt["ob"] % 3
        cnt["ob"] += 1

        def ev_z(c, p, oz=oz):
            self.act(ob[oz][:, c, :], self.ps[p], AF.Silu, [self.psb[p]], [b_ob[oz]])
        fm_segment(OZ, 8, ev_z)
        self.dma(ZSv[:, :, t0:t0 + TS], ob[oz], [b_ob[oz]], [self.db(("ZS", t))], eng="act")
        if PL <= 4:
            continue

        def ev_x(c, p):
            self.cp("dve", xbc[:, c, 3:3 + TS], self.ps[p], [self.psb[p]], [b_xbc[c]])
            a = c % 2
            self.tsc("dve", cacc[a], xbc[:, c, 3:3 + TS], self.vcol(l, "conv_w", c * 4 + 3), self.vcol(l, "conv_b", c),
                     ALU.mult, ALU.add, [b_xbc[c], self.b_vec[l]], [b_cacc[a]])
            for k in range(3):
                self.stt("dve", cacc[a], xbc[:, c, k:k + TS], self.vcol(l, "conv_w", c * 4 + k), cacc[a], ALU.mult, ALU.add,
                         [b_xbc[c], b_cacc[a], self.b_vec[l]], [b_cacc[a]])
            self.act(xco[:, c, :], cacc[a], AF.Silu, [b_cacc[a]], [b_xco])
        fm_segment(OX, 12, ev_x)
        self.dma(XCv[:, :, t0:t0 + TS], xco, [b_xco], [self.db(("XC", t))], eng="act")
        if PL <= 5:
            continue

        def ev_dt(bi, sub, p, w_):
            a = sub % 2
            self.tt("dve", dtmp[a], self.ps[p][:, 0:16], dtb, ALU.add, [self.psb[p], b_rows], [b_dtmp[a]])
            self.act(dtmp[a], dtmp[a], AF.Exp, [b_dtmp[a]], [b_dtmp[a]])
            self.act(dtt[:, sub, 0:16], dtmp[a], AF.Ln, [b_dtmp[a]], [b_dtt], bias=1.0, scale=1.0)
            self.tt("pool", dtt[:, sub, 16:32], dtt[:, sub, 0:16], abc, ALU.mult, [b_dtt, b_abc], [b_dtt])
        tm_segment(ODT, 16, ev_dt)
        self.dma(DTv[:, 4 * t:4 * t + 4, :], dtt, [b_dtt], [self.db(("DT", t))], eng="pool")
        if PL <= 6:
            continue

        def ev_u(c, p):
            self.gelu(gu[:, c, :], self.ps[p], [self.psb[p]], [b_gu], 512)
        fm_segment(OU, 8, ev_u)
        self.gelu_flush()
        if PL <= 7:
            continue

        def ev_vg(bi, sub, p, w_):
            a = sub % 2
            self.gelu(vgf[a][:, bi * 512:(bi + 1) * 512], self.ps[p], [self.psb[p]], [b_vgf[a]], 512)
        wi0 = load_block(OVG, 512)
        wi1 = load_block(OVG + 512, 512)
        gstate = {"i": 0, "wi": None, "og": None, "hold": (wi0, wi1)}

        def emit_gate_chunks(n, t0=t0, t=t):
            for _ in range(n):
                gi_ = gstate["i"]
                if gi_ >= 24:
                    return
                gstate["i"] += 1
                gb, c = gi_ // 8, gi_ % 8
                if gi_ % 4 == 0:
                    gstate["wi"] = load_block(OG + gi_ * 128, 512, avoid=gstate["hold"])
                if c == 0:
                    gstate["og"] = cnt["ob"] % 3
                    cnt["ob"] += 1
                wi, og = gstate["wi"], gstate["og"]
                p = next_ps()
                cc = gi_ % 4
                for kc in range(8):
                    self.mm(self.ps[p], wblk[wi][:, kc, cc * 128:(cc + 1) * 128], xn[:, kc, :], kc == 0, kc == 7,
                            [b_wblk[wi], b_xn[kc]], [self.psb[p]])
                self.act(ob[og][:, c, :], self.ps[p], AF.Sigmoid, [self.psb[p]], [b_ob[og]])
                if c == 7:
                    self.dma(GTv[:, gb * 8:(gb + 1) * 8, t0:t0 + TS], ob[og], [b_ob[og]], [self.db(("GT", t))], eng="act")

        def vg_mm(sub):
            for bi, wi in ((0, wi0), (1, wi1)):
                p = next_ps()
                for kc in range(8):
                    self.mm(self.ps[p], xn[:, kc, sub * 128:(sub + 1) * 128], wblk[wi][:, kc, :], kc == 0, kc == 7,
                            [b_wblk[wi], b_xn[kc]], [self.psb[p]])
                ev_vg(bi, sub, p, 512)

        def ln(sub):
            self.gelu_flush()
            a = sub % 2
            s_ = st2[a]
            self.sch.op("dve", lambda e, a=a, s_=s_: e.tensor_reduce(out=s_[:, 0:1], in_=vgf[a], axis=AX.X, op=ALU.add),
                        [b_vgf[a]], [b_st[a]])
            self.tsc("dve", s_[:, 1:2], s_[:, 0:1], -1.0 / 1024.0, None, ALU.mult, None, [b_st[a]], [b_st[a]])
            self.act(vgt[a], vgf[a], AF.Square, [b_vgf[a], b_st[a]], [b_vgt[a], b_st[a]], bias=s_[:, 1:2], scale=1.0, accum=s_[:, 2:3])
            self.act(s_[:, 3:4], s_[:, 2:3], AF.Sqrt, [b_st[a], self.b_eps], [b_st[a]], bias=self.eps_col, scale=1.0 / 1024.0)
            self.recip(s_[:, 3:4], s_[:, 3:4], [b_st[a]], [b_st[a]])
            self.tsc("dve", vgt[a], vgf[a], s_[:, 1:2], s_[:, 3:4], ALU.add, ALU.mult, [b_vgf[a], b_st[a]], [b_vgt[a]])
            self.tt("pool", vgt[a], vgt[a], lng, ALU.mult, [b_vgt[a], b_rows], [b_vgt[a]])
            self.tt("pool", vgn[a], vgt[a], lnb, ALU.add, [b_vgt[a], b_rows], [b_vgn[a]])

        def sgu(sub):
            a = sub % 2
            for half in range(2):
                pS = 6
                for gg in range(4):
                    g = half * 4 + gg
                    self.mm(self.ps[pS][:, gg * 128:(gg + 1) * 128], vgn[a][:, g * 128:(g + 1) * 128], wsT[:, g, :], True, True,
                            [b_vgn[a], b_wsT], [self.psb[pS]])
                self.tt("dve", sgt[half], self.ps[pS], sgub[:, half * 512:(half + 1) * 512], ALU.add,
                        [self.psb[pS], b_rows], [b_sgt[half]])
                self.tt("pool", yc[:, half * 4:(half + 1) * 4, sub * 128:(sub + 1) * 128], r3(sgt[half], 4),
                        gu[:, half * 4:(half + 1) * 4, sub * 128:(sub + 1) * 128], ALU.mult, [b_sgt[half], b_gu], [b_yc])

        if PL > 8:
            for sub in range(4):
                vg_mm(sub)
                emit_gate_chunks(3)
                ln(sub)
                emit_gate_chunks(3)
                if sub >= 1:
                    sgu(sub - 1)
            gstate["hold"] = ()
            sgu(3)
            emit_gate_chunks(24)
            self.dma(YCv[:, :, t0:t0 + TS], yc, [b_yc], [self.db(("YC", t))], eng="pool")
    self.dma(self.KM, km, [b_km], [self.db("KM")], eng="pool")


def gelu(self, out, in_, reads, writes, n):
    i = self._gl["i"] % 2
    self._gl["i"] += 1
    x, u, bx, bu = self._gl["x"][i][:, 0:n], self._gl["u"][i][:, 0:n], self._gl["bx"][i], self._gl["bu"][i]
    self.cp("act", x, in_, reads, [bx])
    self.tt("pool", u, x, x, ALU.mult, [bx], [bu])
    self.tsc("pool", u, u, 0.044715, 1.0, ALU.mult, ALU.add, [bu], [bu])
    self.tt("pool", u, u, x, ALU.mult, [bu, bx], [bu])
    self.gelu_flush()

    def fin():
        self.act(u, u, AF.Sigmoid, [bu], [bu], scale=2.0 * 0.7978845608028654)
        self.tt("dve", out, u, x, ALU.mult, [bu, bx], writes)
    self._gl["pend"] = fin


def gelu_flush(self):
    f = self._gl.get("pend")
    if f is not None:
        self._gl["pend"] = None
        f()


def alloc_gelu(self):
    ar = self.ar
    self._gl = {"i": 0, "x": [ar.f32(512) for _ in range(2)], "u": [ar.f32(512) for _ in range(2)],
                "bx": bufs(2, "glx"), "bu": bufs(2, "glu")}


Prog.stage_proj = stage_proj
Prog.gelu = gelu
Prog.gelu_flush = gelu_flush
Prog.alloc_gelu = alloc_gelu


def stage_attn(self, l):
    self.stage_begin()
    ar = self.ar
    S, NB = self.S, self.NB
    NQC = S // 512
    NKT = S // 128
    GW = max(NB, 8)
    kmf = ar.f32(8 * NB)
    b_kmf = Buf("kmf")
    self.dma(kmf, self.KM, [self.db("KM")], [b_kmf])
    kmb = ar.bf16(8 * NB)
    b_kmb = Buf("kmb")
    self.cp("dve", kmb, kmf, [b_kmf], [b_kmb])
    self_f = ar.f32(512)
    selb = ar.bf16(S)
    b_self = Buf("self")
    b_selb = Buf("selb")
    for c0 in range(0, S, 512):
        self.dma(self_f[0:35, :], self.selb[0:35, c0:c0 + 512], [], [b_self])
        self.cp("dve", selb[0:35, c0:c0 + 512], self_f[0:35, :], [b_self], [b_selb])
    negtab = ar.f32(2 * GW)
    b_negtab = Buf("negtab")
    self.memset("pool", negtab[:, 0:GW], 0.0, [b_negtab])
    self.memset("pool", negtab[:, GW:2 * GW], -1e30, [b_negtab])
    CM = [[r3(ar.bf16(512), 4) for _ in range(2)] for _ in range(2)]
    b_cm = Buf("cm")
    for blk in range(2):
        for a in range(2):
            m = CM[blk][a]
            self.memset("pool", m, 0.0, [b_cm])
            for rel in range(2):
                s_ = blk * 2 + rel
                if rel < a:
                    self.memset("pool", m[:, s_, :], NEG, [b_cm])
                elif rel == a:
                    self.cp("pool", m[:, s_, :], self.negm_f, [self.b_cf], [b_cm])
    arow_f = ar.f32(512)
    b_arow = Buf("arowf")
    raug = [ar.bf16(512) for _ in range(2)]
    b_raug = bufs(2, "raug")
    for k in range(2):
        self.memset("pool", raug[k][0:35, :], 0.0, [b_raug[k]])
    irow = ar.bf16(512)
    b_irow = Buf("irow")
    KTh = [ar.bf16(S) for _ in range(2)]
    QTh = [ar.bf16(S) for _ in range(2)]
    Vh = [r3(ar.bf16(S), NKT) for _ in range(2)]
    b_kt = bufs(2, "kth")
    b_qt = bufs(2, "qth")
    b_vh = bufs(2, "vh")
    gm = r3(ar.f32(4 * GW), 4)
    b_gm = Buf("gm")
    top8 = r3(ar.f32(32), 4)
    b_top8 = Buf("top8")
    thr = ar.f32(8)
    b_thr = Buf("thr")
    bbqs = [r3(ar.bf16(4 * 32), 4) for _ in range(2)]
    b_bbqs = bufs(2, "bbq")
    b_psT = Buf("psT")
    b_psG = Buf("psG")
    PT = [ar.bf16(512) for _ in range(4)]
    b_pt = bufs(4, "pt")
    rs = [ar.f32(512) for _ in range(2)]
    b_rs = bufs(2, "rs")
    ya = [ar.bf16(512) for _ in range(2)]
    b_ya = bufs(2, "ya")
    for k in range(2):
        self.memset("pool", bbqs[k], 0.0, [b_bbqs[k]])
    psT_b = self.ps[6].bitcast(BF16)
    slopes = [2.0 ** (-8.0 * (h + 1.0) / 8) for h in range(8)]
    QTd = self.QT.rearrange("(c p) s -> p c s", p=128)
    KTd = self.KT.rearrange("(c p) s -> p c s", p=128)
    Vd = self.V.rearrange("(t p) f -> p t f", p=128)
    YAd = self.YA.rearrange("(c p) s -> p c s", p=128)
    allQ = [self.db(("QT", t)) for t in range(self.NT)]
    allK = [self.db(("KT", t)) for t in range(self.NT)]
    allV = [self.db(("V", t)) for t in range(self.NT)]
    cnt = {"pt": 0, "sc": 0}

    def gate_a(h, c, hb, k):
        pG = 6
        for s_ in range(4):
            qs = c * 512 + s_ * 128
            self.mm(self.ps[pG][:, s_ * GW:s_ * GW + NB], QTh[hb][:, qs:qs + 128], kmb[:, h * NB:(h + 1) * NB], True, True,
                    [b_qt[hb], b_kmb], [b_psG])
        for half in range(2):
            own = 2 * c + half
            self.memset("dve", gm[:, 2 * half:2 * half + 2, :], -1e30, [b_gm]) if GW > NB else None
            self.tt("dve", gm[:, 2 * half:2 * half + 2, 0:NB], r3(self.ps[pG][:, 0:4 * GW], 4)[:, 2 * half:2 * half + 2, 0:NB],
                    negtab[:, None, GW - own:GW - own + NB].to_broadcast([128, 2, NB]), ALU.add,
                    [b_psG, b_negtab], [b_gm])
        for s_ in range(4):
            self.sch.op("dve", lambda e, s_=s_: e.max(out=top8[:, s_, :], in_=gm[:, s_, :]), [b_gm], [b_top8])
        self.tsc("dve", thr[:, 0:4], top8[:, :, 2], -1e29, None, ALU.max, None, [b_top8], [b_thr])
        for s_ in range(4):
            own = 2 * c + s_ // 2
            self.tsc("dve", bbqs[k][:, s_, 0:NB], gm[:, s_, 0:NB], thr[:, s_:s_ + 1], 1.0, ALU.is_ge, ALU.subtract,
                     [b_gm, b_thr], [b_bbqs[k]])
            self.memset("dve", bbqs[k][:, s_, own:own + 1], 0.0, [b_bbqs[k]])

    def gate_b(h, c, hb, k):
        for s_ in range(4):
            self.tr(psT_b[0:32, 512 + s_ * 128:512 + (s_ + 1) * 128], bbqs[k][:, s_, :], self.ident_b, [b_bbqs[k], self.b_cb], [b_psT])
        self.cp("act", raug[k][0:32, :], psT_b[0:32, 512:1024], [b_psT], [b_raug[k]])
        self.tsc("dve", raug[k][32:33, :], irow[32:33, :], -slopes[h] * c * 512.0, None, ALU.add, None,
                 [b_irow], [b_raug[k]])

    for h in range(8):
        hb = h % 2
        self.dma(KTh[hb], KTd[:, h, :], allK, [b_kt[hb]])
        self.dma(QTh[hb], QTd[:, h, :], allQ, [b_qt[hb]])
        self.dma(Vh[hb], Vd[:, :, h * 128:(h + 1) * 128], allV, [b_vh[hb]])
        self.dma(arow_f[32:35, :], self.arow[h], [], [b_arow])
        self.cp("dve", irow[32:33, :], arow_f[32:33, :], [b_arow], [b_irow])
        for k in range(2):
            self.cp("dve", raug[k][32:35, :], arow_f[32:35, :], [b_arow], [b_raug[k]])
        gate_a(h, 0, hb, 0)
        gate_b(h, 0, hb, 0)
        SKEW = 2
        scb = [0, 1, 7]
        for c in range(NQC):
            k = c % 2
            if c + 1 < NQC:
                gate_a(h, c + 1, hb, (c + 1) % 2)
            pO, pS = 2 + k, 4 + k
            kts = [kt for kt in range(4 * c + 4) if slopes[h] * (c * 512 - (kt * 128 + 127)) <= 50.0]
            nkt = len(kts)
            pis = {}
            for step in range(nkt + SKEW):
                if step < nkt:
                    kt = kts[step]
                    n, a = kt // 2, kt % 2
                    pc = scb[cnt["sc"] % 3]
                    cnt["sc"] += 1
                    diag = n >= 2 * c
                    self.mm(self.ps[pc], KTh[hb][:, kt * 128:(kt + 1) * 128], QTh[hb][:, c * 512:(c + 1) * 512], True, False,
                            [b_kt[hb], b_qt[hb]], [self.psb[pc]])
                    self.mm(self.ps[pc], selb[0:35, kt * 128:(kt + 1) * 128], raug[k][0:35, :], False, not diag,
                            [b_selb, b_raug[k]], [self.psb[pc]])
                    if diag:
                        self.mm(self.ps[pc], self.ident_b, CM[n - 2 * c][a].rearrange("p a b -> p (a b)"), False, True,
                                [self.b_cb, b_cm], [self.psb[pc]])
                    pi = cnt["pt"] % 4
                    cnt["pt"] += 1
                    pis[kt] = pi
                    self.act(PT[pi], self.ps[pc], AF.Exp, [self.psb[pc]], [b_pt[pi]])
                if step >= SKEW:
                    j = step - SKEW
                    kt = kts[j]
                    pi = pis[kt]
                    self.mm(self.ps[pO], Vh[hb][:, kt, :], PT[pi], j == 0, j == nkt - 1, [b_vh[hb], b_pt[pi]], [self.psb[pO]])
                    self.mm(self.ps[pS], self.ones_b, PT[pi], j == 0, j == nkt - 1, [self.b_ones, b_pt[pi]], [self.psb[pS]])
            if c + 1 < NQC:
                gate_b(h, c + 1, hb, (c + 1) % 2)
            self.cp("act", rs[k], self.ps[pS], [self.psb[pS]], [b_rs[k]])
            self.recip(rs[k], rs[k], [b_rs[k]], [b_rs[k]])
            self.tt("dve", ya[k], self.ps[pO], rs[k], ALU.mult, [self.psb[pO], b_rs[k]], [b_ya[k]])
            self.dma(YAd[:, h, c * 512:(c + 1) * 512], ya[k], [b_ya[k]], [self.db(("YA", c))], eng="pool")


Prog.stage_attn = stage_attn


def stage_ssd(self, l):
    self.stage_begin()
    ar = self.ar
    S, NT = self.S, self.NT
    self.alloc_norm_tmp()
    selh = ar.f32(2048)
    b_selh = Buf("selh")
    self.dma(selh[0:16, :], self.selh, [], [b_selh])
    xc = [r3(ar.bf16(12 * TS), 12) for _ in range(2)]
    b_xc = bufs(2, "sxc")
    zs = [r3(ar.bf16(8 * TS), 8) for _ in range(2)]
    b_zs = bufs(2, "szs")
    dtt = [r3(ar.f32(4 * 32), 4) for _ in range(2)]
    b_dtt = bufs(2, "sdt")
    yg = r3(ar.f32(8 * TS), 8)
    b_yg = bufs(8, "yg")
    yo = r3(ar.bf16(8 * TS), 8)
    b_yo = Buf("yo")
    Sf = ar.f32(1024)
    Sb = ar.bf16(1024)
    b_Sf, b_Sb = Buf("Sf"), Buf("Sb")
    self.memset("pool", Sf, 0.0, [b_Sf])
    self.memset("pool", Sb, 0.0, [b_Sb])
    xdt = [r3(ar.bf16(1024), 16) for _ in range(2)]
    xw = [r3(ar.bf16(1024), 16) for _ in range(2)]
    b_xdt, b_xw = bufs(2, "xdt"), bufs(2, "xw")
    Btok = [ar.bf16(256) for _ in range(2)]
    b_btok = bufs(2, "btok")
    Asb = [ar.f32(32) for _ in range(2)]
    b_asb = bufs(2, "asb")
    acsT = [ar.f32(128) for _ in range(2)]
    b_acsT = bufs(2, "acsT")
    sm = [ar.f32(64) for _ in range(2)]
    b_sm = bufs(2, "sm")
    GTs = [r3(ar.f32(256), 2) for _ in range(2)]
    b_gts = bufs(2, "gts")
    Et = [r3(ar.f32(512), 4) for _ in range(2)]
    b_et = bufs(2, "et")
    WT = [r3(ar.bf16(512), 4) for _ in range(2)]
    b_wt = bufs(2, "wt")
    EA = [r3(ar.f32(512), 4) for _ in range(2)]
    b_ea = bufs(2, "ea")
    CwT = [r3(ar.bf16(512), 4) for _ in range(2)]
    b_cwt = bufs(2, "cwt")
    XCd = self.XC.rearrange("(c p) s -> p c s", p=128)
    ZSd = self.ZS.rearrange("(c p) s -> p c s", p=128)
    DTd = self.DT.rearrange("(s p) c -> p s c", p=128)
    YSd = self.YS.rearrange("(c p) s -> p c s", p=128)
    psX_b = self.ps[0].bitcast(BF16)
    ps1_b = self.ps[1].bitcast(BF16)
    Sbs = [Sb, ar.bf16(1024), ar.bf16(1024)]
    b_Sbs = [b_Sb, Buf("Sb1"), Buf("Sb2")]
    for i in (1, 2):
        self.memset("pool", Sbs[i], 0.0, [b_Sbs[i]])
    b_c5 = {5: self.psb[5], 7: self.psb[7]}
    b_y6 = bufs(2, "psY")
    cnt = {"hg": 0}

    def loads(t):
        tb = t % 2
        t0 = t * TS
        self.dma(xc[tb], XCd[:, :, t0:t0 + TS], [self.db(("XC", t))], [b_xc[tb]])
        self.dma(zs[tb], ZSd[:, :, t0:t0 + TS], [self.db(("ZS", t))], [b_zs[tb]])
        self.dma(dtt[tb], DTd[:, 4 * t:4 * t + 4, :], [self.db(("DT", t))], [b_dtt[tb]])

    def p1(t, q, ci):
        tb, k = t % 2, ci % 2
        cs = slice(q * 128, (q + 1) * 128)
        dt_ = dtt[tb][:, q, 0:16]
        dta_ = dtt[tb][:, q, 16:32]
        for ch in range(8):
            self.tr(psX_b[:, ch * 128:(ch + 1) * 128], xc[tb][:, ch, cs], self.ident_b, [b_xc[tb], self.b_cb], [self.psb[0]])
        for g in range(2):
            self.tr(ps1_b[:, 384 + g * 128:384 + (g + 1) * 128], xc[tb][:, 8 + g, cs], self.ident_b, [b_xc[tb], self.b_cb],
                    [self.psb[1]])
        self.mm(self.ps[1][:, 0:16], self.U_f, dta_, True, True, [self.b_cf, b_dtt[tb]], [self.psb[1]])
        self.mm(self.ps[1][:, 16:32], self.ones_f, dta_, True, True, [self.b_ones, b_dtt[tb]], [self.psb[1]])
        self.mm(self.ps[1][0:16, 32:160], dta_, self.U_f, True, True, [self.b_cf, b_dtt[tb]], [self.psb[1]])
        self.cp("act", Btok[k], ps1_b[:, 384:640], [self.psb[1]], [b_btok[k]])
        self.cp("act", Asb[k], self.ps[1][:, 0:32], [self.psb[1]], [b_asb[k]])
        self.cp("act", acsT[k][0:16, :], self.ps[1][0:16, 32:160], [self.psb[1]], [b_acsT[k]])
        nacs, dte, cd, tmp16 = sm[k][:, 0:16], sm[k][:, 16:32], sm[k][:, 32:48], sm[k][:, 48:64]
        self.tsc("dve", nacs, Asb[k][:, 0:16], -1.0, None, ALU.mult, None, [b_asb[k]], [b_sm[k]])
        self.tt("dve", tmp16, Asb[k][:, 16:32], Asb[k][:, 0:16], ALU.subtract, [b_asb[k]], [b_sm[k]])
        self.act(tmp16, tmp16, AF.Exp, [b_sm[k]], [b_sm[k]])
        self.tt("dve", dte, tmp16, dt_, ALU.mult, [b_sm[k], b_dtt[tb]], [b_sm[k]])
        self.act(cd, Asb[k][:, 16:32], AF.Exp, [b_asb[k]], [b_sm[k]])
        self.tt("dve", xdt[k], r3(psX_b, 16), dt_[:, :, None].to_broadcast([128, 16, 64]), ALU.mult,
                [self.psb[0], b_dtt[tb]], [b_xdt[k]])
        self.tt("dve", xw[k], r3(psX_b, 16), dte[:, :, None].to_broadcast([128, 16, 64]), ALU.mult,
                [self.psb[0], b_sm[k]], [b_xw[k]])
        for g in range(2):
            self.mm(self.ps[3 + g], Btok[k][:, g * 128:(g + 1) * 128], xw[k][:, g * 8:(g + 1) * 8, :].rearrange("p a b -> p (a b)"),
                    True, True, [b_btok[k], b_xw[k]], [self.psb[3 + g]])
        for g in range(2):
            self.mm(self.ps[2][:, g * 128:(g + 1) * 128], xc[tb][:, 8 + g, cs], xc[tb][:, 10 + g, cs], True, True,
                    [b_xc[tb]], [self.psb[2]])
        self.cp("act", GTs[k].rearrange("p a b -> p (a b)"), self.ps[2][:, 0:256], [self.psb[2]], [b_gts[k]])
        self.tt("dve", r3(Sf, 16), r3(Sf, 16), cd[:, :, None].to_broadcast([128, 16, 64]), ALU.mult, [b_Sf, b_sm[k]], [b_Sf])
        for g in range(2):
            self.tt("dve", Sf[:, g * 512:(g + 1) * 512], Sf[:, g * 512:(g + 1) * 512], self.ps[3 + g], ALU.add,
                    [b_Sf, self.psb[3 + g]], [b_Sf])
        self.cp("act", Sbs[ci % 3], Sf, [b_Sf], [b_Sbs[ci % 3]])

    def p2(t, q, ci):
        tb, k = t % 2, ci % 2
        cs = slice(q * 128, (q + 1) * 128)
        nacs = sm[k][:, 0:16]
        Sprev, b_Sprev = Sbs[(ci - 1) % 3], b_Sbs[(ci - 1) % 3]
        for hg in range(4):
            e = cnt["hg"] % 2
            cnt["hg"] += 1
            g = hg // 2
            pC = 5 if e == 0 else 7
            bC = b_c5[pC]
            for hh in range(4):
                h = hg * 4 + hh
                self.mm(self.ps[pC][:, hh * 128:(hh + 1) * 128], selh[0:16, h * 128:(h + 1) * 128], acsT[k][0:16, :], True, True,
                        [b_selh, b_acsT[k]], [bC])
            self.tt("dve", Et[e], r3(self.ps[pC], 4), self.negm_f[:, None, :].to_broadcast([128, 4, 128]), ALU.add,
                    [bC, self.b_cf], [b_et[e]])
            for hh in range(4):
                h = hg * 4 + hh
                self.act(Et[e][:, hh, :], Et[e][:, hh, :], AF.Exp, [b_et[e], b_sm[k]], [b_et[e]], bias=nacs[:, h:h + 1], scale=1.0)
            self.tt("dve", WT[e], Et[e], GTs[k][:, g:g + 1, :].to_broadcast([128, 4, 128]), ALU.mult,
                    [b_et[e], b_gts[k]], [b_wt[e]])
            self.act(EA[e].rearrange("p a b -> p (a b)"), self.ps[pC], AF.Exp, [bC], [b_ea[e]])
            self.tt("pool", CwT[e], EA[e], xc[tb][:, 10 + g:11 + g, cs].to_broadcast([128, 4, 128]), ALU.mult,
                    [b_ea[e], b_xc[tb]], [b_cwt[e]])
            pY = 6
            yoff = e * 256
            bY = b_y6[e]
            for hh in range(4):
                h = hg * 4 + hh
                po = (h % 2) * 64
                col = yoff + (hh // 2) * 128
                kw = {"tile_position": (0, 64)} if po else {}
                self.mm(self.ps[pY][po:po + 64, col:col + 128], xdt[k][:, h, :], WT[e][:, hh, :], True, False,
                        [b_xdt[k], b_wt[e]], [bY], **kw)
                self.mm(self.ps[pY][po:po + 64, col:col + 128], Sprev[:, h * 64:(h + 1) * 64], CwT[e][:, hh, :], False, True,
                        [b_Sprev, b_cwt[e]], [bY], **kw)
            for cc in range(2):
                cp_ = hg * 2 + cc
                self.stt("dve", yg[:, cp_, cs], xc[tb][:, cp_, cs], self.vcol(l, "dskip", cp_),
                         self.ps[pY][:, yoff + cc * 128:yoff + (cc + 1) * 128], ALU.mult, ALU.add,
                         [b_xc[tb], self.b_vec[l], bY], [b_yg[cp_]])
                self.tt("pool", yg[:, cp_, cs], yg[:, cp_, cs], zs[tb][:, cp_, cs], ALU.mult, [b_yg[cp_], b_zs[tb]], [b_yg[cp_]])
        if q == 3:
            t0 = t * TS
            self.rms_stats(yg, b_yg, self.rstd[tb], self.b_rstd[tb], self.sq, self.b_sq, 2)
            for c in range(8):
                self.stt("dve", yo[:, c, :], yg[:, c, :], self.vcol(l, "g_ssm", c), self.rstd[tb], ALU.mult, ALU.mult,
                         [b_yg[c], self.b_rstd[tb], self.b_vec[l]], [b_yo])
            self.dma(YSd[:, :, t0:t0 + TS], yo, [b_yo], [self.db(("YS", t))], eng="pool")

    chunks = [(t, q) for t in range(NT) for q in range(4)]
    for i, (t, q) in enumerate(chunks):
        if q == 0:
            loads(t)
        p1(t, q, i)
        if i >= 1:
            p2(chunks[i - 1][0], chunks[i - 1][1], i - 1)
    p2(chunks[-1][0], chunks[-1][1], len(chunks) - 1)


Prog.stage_ssd = stage_ssd


def stage_merge(self, l):
    self.stage_begin()
    ar = self.ar
    NT = self.NT
    self.alloc_norm_tmp()
    Wn = ["pa", "pss", "pc", "wo"]
    Wt = []
    b_W = []
    for nm in Wn:
        w = r3(ar.bf16(8 * 1024), 8)
        b = Buf("mw" + nm)
        self.dma(w, self.WB[l][nm].rearrange("(kc p) n -> p kc n", p=128), [self.db(("wb", l, nm))], [b])
        Wt.append(w)
        b_W.append(b)
    yt = [r3(ar.bf16(8 * TS), 8) for _ in range(3)]
    b_yt = bufs(3, "myt")
    gt = r3(ar.bf16(24 * TS), 24)
    b_gt = Buf("mgt")
    hres = r3(ar.f32(8 * TS), 8)
    b_h = bufs(8, "mh")
    f = r3(ar.f32(8 * TS), 8)
    b_f = bufs(8, "mf")
    mg = r3(ar.bf16(8 * TS), 8)
    b_mg = bufs(8, "mmg")
    tA = [ar.f32(TS) for _ in range(2)]
    tB = [ar.f32(TS) for _ in range(2)]
    tC = [ar.f32(TS) for _ in range(2)]
    b_tA, b_tB, b_tC = bufs(2, "tA"), bufs(2, "tB"), bufs(2, "tC")
    srcs = [(self.YA, "YA"), (self.YS, "YS"), (self.YC, "YC")]
    hv = self.hT.rearrange("(c p) s -> p c s", p=128)
    GTv = self.GT.rearrange("(c p) s -> p c s", p=128)
    for t in range(NT):
        t0 = t * TS
        for b in range(3):
            key = (srcs[b][1], t)
            deps = [self.db(key)] if srcs[b][1] != "YA" else [self.db(("YA", t))]
            self.dma(yt[b], srcs[b][0].rearrange("(c p) s -> p c s", p=128)[:, :, t0:t0 + TS], deps, [b_yt[b]])
        self.dma(gt, GTv[:, :, t0:t0 + TS], [self.db(("GT", t))], [b_gt])
        self.dma(hres, hv[:, :, t0:t0 + TS], [self.db(("h", t))], b_h)
        for c in range(8):
            k = c % 2
            for b in range(3):
                p = k * 3 + b
                for kc in range(8):
                    self.mm(self.ps[p], Wt[b][:, kc, c * 128:(c + 1) * 128], yt[b][:, kc, :], kc == 0, kc == 7,
                            [b_W[b], b_yt[b]], [self.psb[p]])
            self.tt("dve", tA[k], self.ps[k * 3 + 0], gt[:, c, :], ALU.mult, [self.psb[k * 3 + 0], b_gt], [b_tA[k]])
            self.tt("dve", tB[k], self.ps[k * 3 + 1], gt[:, 8 + c, :], ALU.mult, [self.psb[k * 3 + 1], b_gt], [b_tB[k]])
            self.tt("dve", tC[k], self.ps[k * 3 + 2], gt[:, 16 + c, :], ALU.mult, [self.psb[k * 3 + 2], b_gt], [b_tC[k]])
            self.tt("pool", tA[k], tA[k], tB[k], ALU.add, [b_tA[k], b_tB[k]], [b_tA[k]])
            self.tt("pool", mg[:, c, :], tA[k], tC[k], ALU.add, [b_tA[k], b_tC[k]], [b_mg[c]])
        for c in range(8):
            p = 6 + c % 2
            for kc in range(8):
                self.mm(self.ps[p], Wt[3][:, kc, c * 128:(c + 1) * 128], mg[:, kc, :], kc == 0, kc == 7,
                        [b_W[3], b_mg[kc]], [self.psb[p]])
            self.cp("act", f[:, c, :], self.ps[p], [self.psb[p]], [b_f[c]])
        self.postnorm_res(l, "g_mpost", f, b_f, hres, b_h, None, t % 2)
        self.dma(hv[:, :, t0:t0 + TS], hres, b_h, [self.db(("h", t))], eng="pool")


Prog.stage_merge = stage_merge
```
